# Optimizing a Trainium2 kernel written in Bass

```python
import math
import jax, jax.numpy as jnp
from jax import lax
import numpy as np


D_MODEL = 1024
BATCH = 4
SEQ = 4096
DEPTH = 1

D_MIX = D_MODEL
D_LRU = D_MIX // 2
D_ATTN = D_MIX - D_LRU
N_LRU_BLOCKS = 8
LRU_BLOCK = D_LRU // N_LRU_BLOCKS
CONV_WIDTH = 4
LRU_C = 8.0
N_HEADS = 8
HEAD_DIM = D_ATTN // N_HEADS
MOBA_BLOCK = 256
MOBA_TOPK = 3
Q_CHUNK = 32
REL_BUCKETS = 32
REL_MAX_DIST = 128
N_GROUPS = 4
EXPERTS_PER_GROUP = 8
N_EXPERTS = N_GROUPS * EXPERTS_PER_GROUP
EXPERT_TOPK = 2
D_EXPERT = 256
MOE_ROWS = 128
LN_EPS = 1e-5
DEEPNORM_ALPHA = (2.0 * DEPTH) ** 0.25
DEEPNORM_BETA = (8.0 * DEPTH) ** -0.25
N_MOD = 6
D_IN_PROJ = 2 * D_LRU + 3 * D_ATTN

kernel_name = 'hymba_rglru_moba_hmoe_deepnorm_layer'


def _normalize(x):
    xf = x.astype(jnp.float32)
    mu = jnp.mean(xf, axis=-1, keepdims=True)
    var = jnp.mean(jnp.square(xf - mu), axis=-1, keepdims=True)
    return (xf - mu) * lax.rsqrt(var + LN_EPS)


def layernorm(x, g, b):
    return (_normalize(x) * g + b).astype(x.dtype)


def rmsnorm(x, g):
    xf = x.astype(jnp.float32)
    y = xf * lax.rsqrt(jnp.mean(jnp.square(xf), axis=-1, keepdims=True) + LN_EPS)
    return (y * g).astype(x.dtype)


def modulate(x, shift, scale):
    return (_normalize(x) * (1.0 + scale) + shift).astype(x.dtype)


def causal_depthwise_conv(x, w, b):
    s = x.shape[1]
    xp = jnp.pad(x, ((0, 0), (CONV_WIDTH - 1, 0), (0, 0)))
    return sum(w[k] * xp[:, k:k + s] for k in range(CONV_WIDTH)) + b


def rg_lru(xc, w_a, b_a, w_x, b_x, lam):
    bsz, s, _ = xc.shape
    xb = xc.reshape(bsz, s, N_LRU_BLOCKS, LRU_BLOCK)
    r = jax.nn.sigmoid(jnp.einsum('bsni,nij->bsnj', xb, w_a) + b_a).reshape(bsz, s, D_LRU)
    i = jax.nn.sigmoid(jnp.einsum('bsni,nij->bsnj', xb, w_x) + b_x).reshape(bsz, s, D_LRU)
    log_a = -LRU_C * r.astype(jnp.float32) * jax.nn.softplus(-lam.astype(jnp.float32))
    a = jnp.exp(log_a)
    mult = jnp.sqrt(-jnp.expm1(2.0 * log_a))
    u = mult * (i * xc).astype(jnp.float32)

    def combine(left, right):
        a1, b1 = left
        a2, b2 = right
        return a1 * a2, a2 * b1 + b2

    _, h = lax.associative_scan(combine, (a, u), axis=1)
    return h.astype(xc.dtype)


def t5_bucket(rel):
    n = jnp.maximum(rel, 0)
    max_exact = REL_BUCKETS // 2
    large = max_exact + (jnp.log(jnp.maximum(n, 1).astype(jnp.float32) / max_exact)
                         / math.log(REL_MAX_DIST / max_exact)
                         * (REL_BUCKETS - max_exact)).astype(jnp.int32)
    large = jnp.minimum(large, REL_BUCKETS - 1)
    return jnp.where(n < max_exact, n, large)


def moba_attention(q, k, v, rel_bias):
    bsz, s, h, dh = q.shape
    nb = -(-s // MOBA_BLOCK)
    sp = nb * MOBA_BLOCK
    pad = ((0, 0), (0, sp - s), (0, 0), (0, 0))
    qp = jnp.pad(q, pad).transpose(0, 2, 1, 3)
    kp = jnp.pad(k, pad).transpose(0, 2, 1, 3)
    vp = jnp.pad(v, pad).transpose(0, 2, 1, 3)
    scale = HEAD_DIM ** -0.5
    k_blocks = kp.reshape(bsz, h, nb, MOBA_BLOCK, dh)
    v_blocks = vp.reshape(bsz, h, nb, MOBA_BLOCK, dh)
    k_mean = jnp.mean(k_blocks, axis=3)

    own_all = jnp.arange(sp) // MOBA_BLOCK
    past = jnp.arange(nb)[None, :] < own_all[:, None]
    gate = jnp.einsum('bhqd,bhnd->bhqn', qp, k_mean).astype(jnp.float32)
    gate = jnp.where(past, gate, -jnp.inf)
    if nb < MOBA_TOPK:
        gate = jnp.pad(gate, ((0, 0), (0, 0), (0, 0), (0, MOBA_TOPK - nb)), constant_values=-jnp.inf)
    _, sel = lax.top_k(gate, MOBA_TOPK)

    table_h = rel_bias.T
    b_idx = jnp.arange(bsz)
    h_idx = jnp.arange(h)
    kv_offsets = jnp.arange(MOBA_BLOCK)

    def chunk(ci):
        start = ci * Q_CHUNK
        qc = lax.dynamic_slice_in_dim(qp, start, Q_CHUNK, axis=2)
        selc = lax.dynamic_slice_in_dim(sel, start, Q_CHUNK, axis=2)
        qpos = start + jnp.arange(Q_CHUNK)
        own = start // MOBA_BLOCK
        valid = selc < own
        selg = jnp.minimum(selc, nb - 1)
        kg = k_blocks[b_idx[:, None, None, None], h_idx[None, :, None, None], selg]
        vg = v_blocks[b_idx[:, None, None, None], h_idx[None, :, None, None], selg]
        kpos_p = selg[..., None] * MOBA_BLOCK + kv_offsets
        s_p = jnp.einsum('bhqd,bhqnkd->bhqnk', qc, kg).astype(jnp.float32) * scale
        s_p = s_p + table_h[h_idx[None, :, None, None, None], t5_bucket(qpos[:, None, None] - kpos_p)]
        s_p = jnp.where(valid[..., None], s_p, -jnp.inf)
        ko = lax.dynamic_slice_in_dim(kp, own * MOBA_BLOCK, MOBA_BLOCK, axis=2)
        vo = lax.dynamic_slice_in_dim(vp, own * MOBA_BLOCK, MOBA_BLOCK, axis=2)
        rel_o = qpos[:, None] - (own * MOBA_BLOCK + kv_offsets)[None, :]
        s_o = jnp.einsum('bhqd,bhkd->bhqk', qc, ko).astype(jnp.float32) * scale
        s_o = s_o + table_h[:, t5_bucket(rel_o)]
        s_o = jnp.where(rel_o >= 0, s_o, -jnp.inf)
        logits = jnp.concatenate([s_p.reshape(bsz, h, Q_CHUNK, MOBA_TOPK * MOBA_BLOCK), s_o], axis=-1)
        p = jax.nn.softmax(logits, axis=-1)
        p_p = p[..., :MOBA_TOPK * MOBA_BLOCK].reshape(bsz, h, Q_CHUNK, MOBA_TOPK, MOBA_BLOCK).astype(v.dtype)
        p_o = p[..., MOBA_TOPK * MOBA_BLOCK:].astype(v.dtype)
        return (jnp.einsum('bhqnk,bhqnkd->bhqd', p_p, vg)
                + jnp.einsum('bhqk,bhkd->bhqd', p_o, vo))

    outs = lax.map(chunk, jnp.arange(sp // Q_CHUNK))
    out = outs.transpose(1, 2, 0, 3, 4).reshape(bsz, h, sp, dh)[:, :, :s]
    return out.transpose(0, 2, 1, 3).reshape(bsz, s, h * dh)


def hybrid_mixer(u, w_in, conv_w, conv_b, w_gate_a, b_gate_a, w_gate_x, b_gate_x,
                 lru_lambda, rel_bias, norm_lru_g, norm_attn_g, w_out):
    bsz, s, _ = u.shape
    proj = u @ w_in
    x_lru, y_lru, q, k, v = jnp.split(
        proj, [D_LRU, 2 * D_LRU, 2 * D_LRU + D_ATTN, 2 * D_LRU + 2 * D_ATTN], axis=-1)
    h = rg_lru(causal_depthwise_conv(x_lru, conv_w, conv_b),
               w_gate_a, b_gate_a, w_gate_x, b_gate_x, lru_lambda)
    lru_out = h * jax.nn.gelu(y_lru)
    attn_out = moba_attention(q.reshape(bsz, s, N_HEADS, HEAD_DIM),
                              k.reshape(bsz, s, N_HEADS, HEAD_DIM),
                              v.reshape(bsz, s, N_HEADS, HEAD_DIM), rel_bias)
    merged = jnp.concatenate([rmsnorm(lru_out, norm_lru_g), rmsnorm(attn_out, norm_attn_g)], axis=-1)
    return merged @ w_out


def hierarchical_moe(u, w_rg, b_rg, w_re, b_re, w1, w3, w2):
    bsz, s, d = u.shape
    nc = s // MOE_ROWS
    xt = u.reshape(bsz, nc, MOE_ROWS, d).transpose(1, 0, 2, 3).reshape(nc, bsz * MOE_ROWS, d)

    def body(xc):
        g_prob = jax.nn.softmax((xc @ w_rg + b_rg).astype(jnp.float32), axis=-1)
        g_top, g_idx = lax.top_k(g_prob, 1)
        e_logits = (xc @ w_re + b_re).astype(jnp.float32).reshape(-1, N_GROUPS, EXPERTS_PER_GROUP)
        e_in = jnp.take_along_axis(e_logits, g_idx[:, :, None], axis=1)[:, 0]
        e_top, e_loc = lax.top_k(jax.nn.softmax(e_in, axis=-1), EXPERT_TOPK)
        e_top = e_top / jnp.sum(e_top, axis=-1, keepdims=True)
        weights = g_top * e_top
        e_glob = g_idx * EXPERTS_PER_GROUP + e_loc
        gate = jnp.sum(jax.nn.one_hot(e_glob, N_EXPERTS, dtype=jnp.float32) * weights[..., None], axis=1)
        hdn = jax.nn.silu(jnp.einsum('td,edf->tef', xc, w1)) * jnp.einsum('td,edf->tef', xc, w3)
        return jnp.einsum('tef,efd->td', hdn * gate[:, :, None].astype(hdn.dtype), w2)

    ys = lax.map(body, xt)
    return ys.reshape(nc, bsz, MOE_ROWS, d).transpose(1, 0, 2, 3).reshape(bsz, s, d)


def setup_inputs(seed: int = 0) -> dict:
    key = jax.random.key(seed)
    ks = jax.random.split(key, 28)
    f32 = jnp.float32
    L = DEPTH

    def nrm(k, shape, scale):
        return jax.random.normal(k, shape, f32) * scale

    a_c = jax.random.uniform(ks[11], (L, D_LRU), f32, 0.9, 0.999)
    a_base = a_c ** (1.0 / LRU_C)
    return {
        'x': nrm(ks[0], (BATCH, SEQ, D_MODEL), 1.0),
        'c': nrm(ks[1], (BATCH, D_MODEL), 1.0),
        'w_ada': nrm(ks[2], (L, D_MODEL, N_MOD * D_MODEL), 0.5 * D_MODEL ** -0.5),
        'b_ada': nrm(ks[3], (L, N_MOD * D_MODEL), 0.02),
        'w_in': nrm(ks[4], (L, D_MODEL, D_IN_PROJ), D_MODEL ** -0.5),
        'conv_w': nrm(ks[5], (L, CONV_WIDTH, D_LRU), CONV_WIDTH ** -0.5),
        'conv_b': nrm(ks[6], (L, D_LRU), 0.02),
        'w_gate_a': nrm(ks[7], (L, N_LRU_BLOCKS, LRU_BLOCK, LRU_BLOCK), LRU_BLOCK ** -0.5),
        'b_gate_a': nrm(ks[8], (L, N_LRU_BLOCKS, LRU_BLOCK), 0.02),
        'w_gate_x': nrm(ks[9], (L, N_LRU_BLOCKS, LRU_BLOCK, LRU_BLOCK), LRU_BLOCK ** -0.5),
        'b_gate_x': nrm(ks[10], (L, N_LRU_BLOCKS, LRU_BLOCK), 0.02),
        'lru_lambda': jnp.log(a_base) - jnp.log1p(-a_base),
        'rel_bias': nrm(ks[12], (REL_BUCKETS, N_HEADS), 0.5),
        'norm_lru_g': 1.0 + nrm(ks[13], (L, D_LRU), 0.05),
        'norm_attn_g': 1.0 + nrm(ks[14], (L, D_ATTN), 0.05),
        'w_out': nrm(ks[15], (L, D_MIX, D_MODEL), DEEPNORM_BETA * D_MIX ** -0.5),
        'ln1_g': 1.0 + nrm(ks[16], (L, D_MODEL), 0.05),
        'ln1_b': nrm(ks[17], (L, D_MODEL), 0.02),
        'w_router_group': nrm(ks[18], (L, D_MODEL, N_GROUPS), D_MODEL ** -0.5),
        'b_router_group': nrm(ks[19], (L, N_GROUPS), 0.01),
        'w_router_expert': nrm(ks[20], (L, D_MODEL, N_EXPERTS), D_MODEL ** -0.5),
        'b_router_expert': nrm(ks[21], (L, N_EXPERTS), 0.01),
        'w1': nrm(ks[22], (L, N_EXPERTS, D_MODEL, D_EXPERT), D_MODEL ** -0.5),
        'w3': nrm(ks[23], (L, N_EXPERTS, D_MODEL, D_EXPERT), D_MODEL ** -0.5),
        'w2': nrm(ks[24], (L, N_EXPERTS, D_EXPERT, D_MODEL), DEEPNORM_BETA * D_EXPERT ** -0.5),
        'ln2_g': 1.0 + nrm(ks[25], (L, D_MODEL), 0.05),
        'ln2_b': nrm(ks[26], (L, D_MODEL), 0.02),
    }


def reference(x, c, w_ada, b_ada, w_in, conv_w, conv_b, w_gate_a, b_gate_a, w_gate_x, b_gate_x,
              lru_lambda, rel_bias, norm_lru_g, norm_attn_g, w_out, ln1_g, ln1_b,
              w_router_group, b_router_group, w_router_expert, b_router_expert,
              w1, w3, w2, ln2_g, ln2_b):
    bsz, _, d = x.shape
    silu_c = jax.nn.silu(c)
    for l in range(DEPTH):
        mod = (silu_c @ w_ada[l] + b_ada[l]).reshape(bsz, N_MOD, d)
        sh1, sc1, g1, sh2, sc2, g2 = [mod[:, j, None, :] for j in range(N_MOD)]
        u = modulate(x, sh1, sc1)
        mix = hybrid_mixer(u, w_in[l], conv_w[l], conv_b[l], w_gate_a[l], b_gate_a[l],
                           w_gate_x[l], b_gate_x[l], lru_lambda[l], rel_bias,
                           norm_lru_g[l], norm_attn_g[l], w_out[l])
        x = layernorm(DEEPNORM_ALPHA * x + g1 * mix, ln1_g[l], ln1_b[l])
        u2 = modulate(x, sh2, sc2)
        ffn = hierarchical_moe(u2, w_router_group[l], b_router_group[l], w_router_expert[l],
                               b_router_expert[l], w1[l], w3[l], w2[l])
        x = layernorm(DEEPNORM_ALPHA * x + g2 * ffn, ln2_g[l], ln2_b[l])
    return x
```

```python
import contextlib
import math
import numpy as np
import ml_dtypes
import concourse.bass as bass
import concourse.mybir as mybir
from concourse.bass_utils import run_bass_kernel_spmd

F32 = mybir.dt.float32
BF16 = mybir.dt.bfloat16
AF = mybir.ActivationFunctionType
ALU = mybir.AluOpType
AX = mybir.AxisListType

D = 1024
SEQ = 4096
CT = 4096
OWN0 = 2048
NOWN = 2048
NH = 8
HD = 64
BLK = 256
NBLK = 16
NE = 32
DE = 256
EPS = 1e-5
ALPHA = 2.0 ** 0.25
NEGM = -30000.0
DBG = False
import os
STOP = int(os.environ.get('MK_STOP', '9'))
NCH = int(os.environ.get('MK_NCH', '8'))
SKIP = set(os.environ.get('MK_SKIP', '').split(','))
SCHED_PH = set(os.environ.get('MK_SCHED_PH', '0,1,2,3,4,5').split(','))


class _Stop(Exception):
    pass


class Prog:
    def __init__(self, nc, n_dma_sems=24):
        self.nc = nc
        self.E = {'pe': nc.tensor, 'act': nc.scalar, 'dve': nc.vector, 'pool': nc.gpsimd, 'sp': nc.sync}
        self.sem = {}
        self.cnt = {}
        self._ctx = []
        for e in ['pe', 'act', 'dve', 'pool']:
            g = nc.semaphore('s_' + e)
            self.sem[e] = g.__enter__()
            self._ctx.append(g)
            self.cnt[e] = 0
        self.dsem = []
        for i in range(n_dma_sems):
            g = nc.semaphore('d_%d' % i)
            self.dsem.append(g.__enter__())
            self._ctx.append(g)
        self.dcnt = [0] * n_dma_sems
        self.dnext = 0
        self.dnext_sw = 0
        self.waited = {}
        self.lastw = {}
        self.reads = {}
        self.dirty = {e: False for e in self.cnt}
        self.rec = []
        self.tnow = 0.0
        self.schedule = (os.environ.get('MK_SCHED', '1') == '1')

    def _semh(self, key):
        return self.sem[key] if isinstance(key, str) else self.dsem[key]

    def _wait(self, eng, ev, same_ok):
        key, val, src = ev
        if src == eng and same_ok:
            return
        if self.waited.get((eng, key), 0) >= val:
            return
        self.E[eng].wait_ge(self._semh(key), val)
        self.waited[(eng, key)] = val

    def _deps(self, eng, reads, writes, is_dma=False):
        for r in reads:
            ev = self.lastw.get(r)
            if ev is not None:
                self._wait(eng, ev, same_ok=(eng == 'pe' and not is_dma))
        for w in writes:
            ev = self.lastw.get(w)
            if ev is not None:
                self._wait(eng, ev, same_ok=(eng == 'pe' and not is_dma))
            for ev in self.reads.get(w, ()):
                self._wait(eng, ev, same_ok=(eng == 'pe' and not is_dma))

    def _record(self, ev, reads, writes):
        for r in reads:
            lst = self.reads.setdefault(r, [])
            lst.append(ev)
            if len(lst) > 16:
                best = {}
                for k, v, s in lst:
                    if k not in best or best[k][1] < v:
                        best[k] = (k, v, s)
                self.reads[r] = list(best.values())
        for w in writes:
            self.lastw[w] = ev
            self.reads[w] = []

    def op(self, eng, fn, reads=(), writes=(), sig=True, est=0.5):
        self.rec.append(dict(kind='op', eng=eng, fn=fn, reads=tuple(reads), writes=tuple(writes), sig=sig, est=est))

    def dma(self, q, out, in_, reads=(), writes=(), **kw):
        try:
            nbytes = out.nbytes()
        except Exception:
            nbytes = 65536
        self.rec.append(dict(kind='dma', eng=q, out=out, in_=in_, kw=kw, reads=tuple(reads), writes=tuple(writes),
                             sig=True, est=2.0 + nbytes / 150e3))

    def _emit_op(self, eng, fn, reads, writes, sig):
        self._deps(eng, reads, writes)
        ins = fn()
        if sig:
            self.cnt[eng] += 1
            ins.then_inc(self.sem[eng], 1)
            ev = (eng, self.cnt[eng], eng)
            self.dirty[eng] = False
        else:
            ev = (eng, self.cnt[eng] + 1, eng)
            self.dirty[eng] = True
        self._record(ev, reads, writes)
        return ins

    def _emit_dma(self, q, out, in_, reads, writes, kw):
        half = len(self.dsem) // 2
        if q == 'pool':
            i = self.dnext_sw
            self.dnext_sw = (self.dnext_sw + 1) % half
        else:
            i = half + self.dnext
            self.dnext = (self.dnext + 1) % (len(self.dsem) - half)
        if self.dcnt[i] > 0:
            self._wait(q, (i, self.dcnt[i], 'dma'), same_ok=False)
        self._deps(q, reads, writes, is_dma=True)
        ins = self.E[q].dma_start(out=out, in_=in_, **kw)
        self.dcnt[i] += 16
        ins.then_inc(self.dsem[i], 16)
        ev = (i, self.dcnt[i], 'dma')
        self._record(ev, reads, writes)
        return ev

    def flush(self):
        rec = self.rec
        self.rec = []
        if not rec:
            return
        nodes = []
        cur_pe = None
        for r in rec:
            if r['kind'] == 'op' and r['eng'] == 'pe':
                if cur_pe is None:
                    cur_pe = dict(eng='pe', items=[], est=0.0)
                    nodes.append(cur_pe)
                cur_pe['items'].append(r)
                cur_pe['est'] += r['est']
                if r['sig']:
                    cur_pe = None
            else:
                if cur_pe is not None:
                    cur_pe['items'][-1]['sig'] = True
                    cur_pe = None
                nodes.append(dict(eng=r['eng'], items=[r], est=r['est'], isdma=(r['kind'] == 'dma')))
        if cur_pe is not None:
            cur_pe['items'][-1]['sig'] = True
            cur_pe = None
        n = len(nodes)
        lastw = {}
        readers = {}
        preds = [set() for _ in range(n)]
        for i, nd in enumerate(nodes):
            R = set(); Wr = set()
            for it in nd['items']:
                R.update(it['reads']); Wr.update(it['writes'])
            for r_ in R:
                if r_ in lastw:
                    preds[i].add(lastw[r_])
            for w_ in Wr:
                if w_ in lastw:
                    preds[i].add(lastw[w_])
                for j in readers.get(w_, ()):
                    preds[i].add(j)
            for r_ in R:
                readers.setdefault(r_, []).append(i)
            for w_ in Wr:
                lastw[w_] = i
                readers[w_] = []
            preds[i].discard(i)
        if not self.schedule:
            order = list(range(n))
        else:
            engs = ['pe', 'act', 'dve', 'pool', 'sp']
            per = {e: [] for e in engs}
            for i, nd in enumerate(nodes):
                per[nd['eng']].append(i)
            ptr = {e: 0 for e in engs}
            done = [False] * n
            fin = [0.0] * n
            free = {e: self.tnow for e in engs}
            order = []
            WINDOW = int(os.environ.get("MK_WIN", "48"))
            remaining = n
            while remaining:
                best = None
                for e in engs:
                    lst = per[e]
                    p0 = ptr[e]
                    while p0 < len(lst) and done[lst[p0]]:
                        p0 += 1
                    ptr[e] = p0
                    cnt = 0
                    k = p0
                    while k < len(lst) and cnt < WINDOW:
                        i = lst[k]
                        k += 1
                        if done[i]:
                            continue
                        cnt += 1
                        ok = True
                        t = free[e]
                        for pj in preds[i]:
                            if not done[pj]:
                                ok = False
                                break
                            if fin[pj] > t:
                                t = fin[pj]
                        if not ok:
                            continue
                        key = (t + 0.002 * (cnt - 1), i)
                        if best is None or key < best[0]:
                            best = (key, e, i, t)
                        if t <= free[e] + 1e-9:
                            break
                assert best is not None, "scheduler deadlock"
                _, e, i, t = best
                nd = nodes[i]
                done[i] = True
                remaining -= 1
                if nd.get('isdma'):
                    free[e] = t + 0.15
                    fin[i] = t + nd['est']
                else:
                    free[e] = t + nd['est']
                    fin[i] = t + nd['est'] + 0.1
                order.append((t, i))
            order.sort()
            order = [i for _, i in order]
            self.tnow = max(max(free.values()), max(fin) if fin else 0.0)
        for i in order:
            for it in nodes[i]['items']:
                if it['kind'] == 'op':
                    self._emit_op(it['eng'], it['fn'], it['reads'], it['writes'], it['sig'])
                else:
                    self._emit_dma(it['eng'], it['out'], it['in_'], it['reads'], it['writes'], it['kw'])

    def barrier(self):
        self.flush()
        for e in self.cnt:
            assert not self.dirty[e], e
        for eng in ['pe', 'act', 'dve', 'pool', 'sp']:
            for e in self.cnt:
                if self.cnt[e] > 0:
                    self._wait(eng, (e, self.cnt[e], e), same_ok=False)
            for i in range(len(self.dsem)):
                if self.dcnt[i] > 0:
                    self._wait(eng, (i, self.dcnt[i], 'dma'), same_ok=False)
        self.lastw = {}
        self.reads = {}

    def close(self):
        self.flush()
        for g in reversed(self._ctx):
            g.__exit__(None, None, None)


def _t5_bucket_np(n):
    n = np.maximum(n, 0)
    max_exact = 16
    nf = np.maximum(n, 1).astype(np.float32)
    large = max_exact + (np.log(nf / np.float32(max_exact)) / np.float32(math.log(128 / 16))
                         * np.float32(32 - max_exact)).astype(np.int32)
    large = np.minimum(large, 31)
    return np.where(n < max_exact, n, large)


def build_program():
    nc = bass.Bass("TRN2", target_bir_lowering=False)

    def din(name, shape, dt=F32):
        return nc.dram_tensor(name, list(shape), dt, kind="ExternalInput").ap()

    xctx = din("xctx", [CT, D])
    cvec = din("cvec", [128, 8])
    w_ada = din("w_ada", [D, 6 * D])
    b_ada = din("b_ada", [128, 48])
    b_ada_row = din("b_ada_row", [1, 6 * D])
    w_in = din("w_in", [D, 2560])
    convw = din("convw", [128, 16])
    convb = din("convb", [128, 4])
    WA = din("WA", [4, 128, 128])
    WX = din("WX", [4, 128, 128])
    bga = din("bga", [128, 4])
    bgx = din("bgx", [128, 4])
    lam = din("lam", [128, 4])
    relb = din("relb", [32, 8])
    glru = din("glru", [128, 4])
    gattn = din("gattn", [128, 4])
    w_out = din("w_out", [D, D])
    lnv = din("lnv", [4, D])
    w_r = din("w_r", [D, 36])
    b_r = din("b_r", [1, 36])
    if STOP >= 5:
        w1 = din("w1", [NE, D, DE])
        w3 = din("w3", [NE, D, DE])
        w2 = din("w2", [NE, DE, D])
    gmask_d = din("gmask", [128, 256])
    ownhot_d = din("ownhot", [128, 256])
    flag_d = din("flag", [128, 1])
    Roh = din("Roh", [32, 384])
    NEGr = din("NEGr", [8, 384])
    IND = din("IND", [16, CT])
    out_d = nc.dram_tensor("out", [NOWN, D], F32, kind="ExternalOutput").ap()

    xlru_s = nc.dram_tensor("xlru_s", [4, 128, CT], F32, kind="Internal").ap()
    gy_s = nc.dram_tensor("gy_s", [4, 128, NOWN], BF16, kind="Internal").ap()
    x1_s = nc.dram_tensor("x1_s", [NOWN, D], F32, kind="Internal").ap()
    q_s = nc.dram_tensor("q_s", [4, 128, NOWN], BF16, kind="Internal").ap()
    lo_s = nc.dram_tensor("lo_s", [4, 128, NOWN], BF16, kind="Internal").ap()
    a_s = nc.dram_tensor("a_s", [4, 128, NOWN], BF16, kind="Internal").ap()
    G_s = nc.dram_tensor("G_s", [8, 384], F32, kind="Internal")
    dbg = {}

    def dout(name, shape, dt=F32):
        dbg[name] = nc.dram_tensor(name, list(shape), dt, kind="ExternalOutput").ap()
        return dbg[name]

    P = Prog(nc)
    try:
      with contextlib.ExitStack() as top:
        _nm = [0]

        def sb(st, name, shape, dt):
            _nm[0] += 1
            return st.enter_context(nc.sbuf_tensor("%s_u%d" % (name, _nm[0]), list(shape), dt))

        def ps(st, name, shape, dt):
            return st.enter_context(nc.psum_tensor(name, list(shape), dt))

        E = P.E

        def _est(eng, a, kw):
            o = kw.get('out', a[0] if a else None)
            try:
                nfree = 1
                for d_ in o.shape[1:]:
                    nfree *= d_
            except Exception:
                nfree = 512
            if eng == 'pe':
                mv_ = kw.get('rhs', a[2] if len(a) > 2 else None)
                try:
                    nm = 1
                    for d_ in mv_.shape[1:]:
                        nm *= d_
                except Exception:
                    nm = 128
                return 0.06 + max(nm, 64) / 2000.0
            if eng == 'act':
                return 0.22 + nfree / 1100.0
            if eng == 'dve':
                return 0.10 + nfree / 900.0
            return 0.15 + nfree / 440.0

        def op(eng, meth, reads, writes, *args, sig=True, **kw):
            return P.op(eng, lambda: getattr(E[eng], meth)(*args, **kw), reads, writes, sig, est=_est(eng, args, kw))

        def ARGS(*a, **kw):
            return (a, kw)

        def pe_raw(meth, reads, writes, argskw=None, sig=True):
            a, kw = argskw
            return P.op('pe', lambda: getattr(nc.tensor, meth)(*a, **kw), reads, writes, sig, est=_est('pe', a, kw))

        def mm(out, lhsT, rhs, reads, writes, start, stop):
            return P.op('pe', lambda: nc.tensor.matmul(out, lhsT, rhs, start=start, stop=stop), reads, writes, sig=stop,
                        est=_est('pe', (out, lhsT, rhs), {}))

        pT0 = ps(top, "pT0", [128, 1024], BF16)
        pT1 = ps(top, "pT1", [128, 1024], BF16)
        pA = ps(top, "pA", [128, 512], F32)
        pB = ps(top, "pB", [128, 512], F32)
        pS0 = ps(top, "pS0", [128, 512], F32)
        pS1 = ps(top, "pS1", [128, 512], F32)
        pO = ps(top, "pO", [128, 512], F32)
        pM = ps(top, "pM", [128, 512], F32)

        ident = sb(top, "ident", [128, 128], BF16)
        id32 = sb(top, "id32", [128, 128], F32)
        ones32 = sb(top, "ones32", [128, 128], F32)
        onesb = sb(top, "onesb", [128, 1], BF16)
        modT = sb(top, "modT", [128, 48], F32)
        sc1p = sb(top, "sc1p", [128, 8], F32)
        sc2p = sb(top, "sc2p", [128, 8], F32)
        g1B = sb(top, "g1B", [128, D], F32)
        g2B = sb(top, "g2B", [128, D], F32)
        mhalf = sb(top, "mhalf", [128, 16], F32)
        ve = sb(top, "ve", [128, 1], F32)

        op('pool', 'memset', [], ['id32'], id32[:], 1.0)
        op('pool', 'affine_select', ['id32'], ['id32'], out=id32[:], in_=id32[:], pattern=[[-1, 128]],
           compare_op=ALU.is_equal, fill=0.0, base=0, channel_multiplier=1)
        op('dve', 'tensor_copy', ['id32'], ['ident'], ident[:], id32[:])
        op('pool', 'memset', [], ['ones32'], ones32[:], 1.0)
        op('dve', 'memset', [], ['onesb'], onesb[:], 1.0)
        op('dve', 'memset', [], ['mhalf'], mhalf[:], -0.5)

        P.flush()
        P.schedule = ('0' in SCHED_PH) and (os.environ.get('MK_SCHED', '1') == '1')
        with contextlib.ExitStack() as s0:
            csb = sb(s0, "csb", [128, 8], F32)
            modrow = sb(s0, "modrow", [1, 6 * D], F32)
            csil = sb(s0, "csil", [128, 8], BF16)
            bada = sb(s0, "bada", [128, 48], F32)
            wad = [sb(s0, "wad%d" % i, [128, 8, 512], BF16) for i in range(2)]
            P.dma('sp', csb[:], cvec, writes=['csb'])
            P.dma('sp', bada[:], b_ada, writes=['bada'])
            op('act', 'activation', ['csb'], ['csil'], out=csil[:], in_=csb[:], func=AF.Silu)
            for cc in range(12):
                w = wad[cc % 2]
                wk = 'wad%d' % (cc % 2)
                P.dma('pool', w[:], w_ada[:, cc * 512:(cc + 1) * 512].rearrange("(k p) n -> p k n", p=128),
                      writes=[wk])
                pp = pA if cc % 2 == 0 else pB
                pk = 'pA' if cc % 2 == 0 else 'pB'
                for k in range(8):
                    mm(pp[0:1, :], csil[:, k:k + 1], w[:, k, :], ['csil', wk], [pk], k == 0, k == 7)
                op('act', 'copy', [pk], ['modrow'], modrow[0:1, cc * 512:(cc + 1) * 512], pp[0:1, :])
            for ct in range(48):
                pe_raw('matmul', ['modrow', 'ones32'], ['pM'], sig=(ct == 47), argskw=ARGS(pM[:, ct:ct + 1], modrow[0:1, ct * 128:(ct + 1) * 128],
                                                    ones32[0:1, 0:1], start=True, stop=True))
            op('dve', 'tensor_tensor', ['pM', 'bada'], ['modT'], modT[:], pM[:, 0:48], bada[:], ALU.add)
            op('dve', 'tensor_scalar_add', ['modT'], ['sc1p'], sc1p[:], modT[:, 8:16], 1.0)
            op('dve', 'tensor_scalar_add', ['modT'], ['sc2p'], sc2p[:], modT[:, 32:40], 1.0)
            for (dst, dk, off) in ((g1B, 'g1B', 2 * D), (g2B, 'g2B', 5 * D)):
                for hf in range(2):
                    pe_raw('matmul', ['modrow', 'ones32'], ['pA'], argskw=ARGS(pA[:, :], ones32[0:1, :],
                                                        modrow[0:1, off + hf * 512: off + (hf + 1) * 512],
                                                        start=True, stop=True))
                    op('act', 'copy', ['pA'], [dk], dst[:, hf * 512:(hf + 1) * 512], pA[:, :])
            badarow = sb(s0, "badarow", [128, 2, D], F32)
            P.dma('sp', badarow[:, 0, :], b_ada_row[0:1, 2 * D:3 * D].rearrange("a n -> (a n)").partition_broadcast(128),
                  writes=['badarow'])
            P.dma('sp', badarow[:, 1, :], b_ada_row[0:1, 5 * D:6 * D].rearrange("a n -> (a n)").partition_broadcast(128),
                  writes=['badarow'])
            op('pool', 'tensor_tensor', ['g1B', 'badarow'], ['g1B'], g1B[:], g1B[:], badarow[:, 0, :], ALU.add)
            op('pool', 'tensor_tensor', ['g2B', 'badarow'], ['g2B'], g2B[:], g2B[:], badarow[:, 1, :], ALU.add)
            if DBG:
                P.dma('sp', dout("d_modT", [128, 48]), modT[:], reads=['modT'])
                P.dma('sp', dout("d_g1B", [128, D]), g1B[:], reads=['g1B'])
            P.barrier()
            if STOP == 0:
                raise _Stop()

        ssl = sb(top, "ssl", [128, 16], F32)
        ssa = sb(top, "ssa", [128, 16], F32)
        op('dve', 'memset', [], ['ssl'], ssl[:], 0.0)
        op('dve', 'memset', [], ['ssa'], ssa[:], 0.0)
        with contextlib.ExitStack() as s13:
            kT = [sb(s13, "kT%d" % h, [96, CT], BF16) for h in range(NH)]
            Vt = sb(s13, "Vt", [128, 32, NH * 65], BF16)
            op('pool', 'memset', [], ['Vt'], Vt[:].rearrange("p t c -> p (t c)"), 1.0)
            for h in range(NH):
                op('dve', 'memset', [], ['kTind%d' % h], kT[h][64:96, :], 0.0)
                P.dma('pool', kT[h][64:80, :], IND, writes=['kTind%d' % h])

            P.flush()
            P.schedule = ('1' in SCHED_PH) and (os.environ.get('MK_SCHED', '1') == '1')
            with contextlib.ExitStack() as s1:
                winb = sb(s1, "winb", [128, 8, 2560], BF16)
                for k in range(8):
                    P.dma('pool', winb[:, k, :], w_in[k * 128:(k + 1) * 128, :], writes=['winb%d' % k])
                xt = [sb(s1, "xt%d" % i, [128, D], F32) for i in range(2)]
                xn = [sb(s1, "xn%d" % i, [128, D], BF16) for i in range(8)]
                uTs = [sb(s1, "uT%d" % i, [128, 8, 512], BF16) for i in range(2)]
                st6 = [sb(s1, "st6_%d" % i, [128, 2, 6], F32) for i in range(4)]
                mv = [sb(s1, "mv_%d" % i, [128, 2], F32) for i in range(4)]
                rstd = [sb(s1, "rstd_%d" % i, [128, 1], F32) for i in range(4)]
                nb = [sb(s1, "nb_%d" % i, [128, 1], F32) for i in range(4)]
                ve1 = [sb(s1, "ve_%d" % i, [128, 1], F32) for i in range(4)]
                stg = [sb(s1, "stg%d" % i, [128, 512], F32) for i in range(2)]
                ysb = sb(s1, "ysb", [128, 512], F32)
                yt = sb(s1, "yt", [128, 512], F32)
                ysg = sb(s1, "ysg", [128, 512], F32)
                gyb = [sb(s1, "gyb%d" % i, [128, 512], BF16) for i in range(2)]
                winr = ['winb%d' % k for k in range(8)]
                nstg = 0

                def emit_ln(c):
                    for t in range(4):
                        T = 4 * c + t
                        xb = xt[T % 2]
                        xk = 'xt%d' % (T % 2)
                        i = T % 4
                        xi = (c % 2) * 4 + t
                        P.dma('sp', xb[:], xctx[T * 128:(T + 1) * 128, :], writes=[xk])
                        for hf in range(2):
                            op('dve', 'bn_stats', [xk], ['st6_%d' % i], out=st6[i][:, hf, :], in_=xb[:, hf * 512:(hf + 1) * 512])
                        op('dve', 'bn_aggr', ['st6_%d' % i], ['mv_%d' % i], out=mv[i][:], in_=st6[i][:].rearrange("p a b -> p (a b)"))
                        op('dve', 'tensor_scalar_add', ['mv_%d' % i], ['ve_%d' % i], ve1[i][:], mv[i][:, 1:2], EPS)
                        op('pool', 'tensor_tensor', ['ve_%d' % i, 'mhalf'], ['rstd_%d' % i], rstd[i][:], ve1[i][:], mhalf[:, 0:1], ALU.pow)
                        op('dve', 'scalar_tensor_tensor', ['mv_%d' % i, 'rstd_%d' % i], ['nb_%d' % i], out=nb[i][:], in0=mv[i][:, 0:1],
                           scalar=-1.0, in1=rstd[i][:], op0=ALU.mult, op1=ALU.mult)
                        op('act', 'activation', [xk, 'rstd_%d' % i, 'nb_%d' % i], ['xn%d' % xi], out=xn[xi][:], in_=xb[:],
                           func=AF.Identity, bias=nb[i][:], scale=rstd[i][:])

                def emit_tr(c, r):
                    uT = uTs[c % 2]
                    pt = pT0 if r % 2 == 0 else pT1
                    ptk = 'pT0' if r % 2 == 0 else 'pT1'
                    for kk in range(2):
                        k = 2 * r + kk
                        for t in range(4):
                            xi = (c % 2) * 4 + t
                            pe_raw('transpose', ['xn%d' % xi, 'ident'], [ptk], sig=(kk == 1 and t == 3), argskw=ARGS(
                                pt[:, kk * 512 + t * 128: kk * 512 + (t + 1) * 128],
                                xn[xi][:, k * 128:(k + 1) * 128], ident[:]))
                    for kk in range(2):
                        k = 2 * r + kk
                        uk = 'uT%d_%d' % (c % 2, k)
                        if r % 2 == 0:
                            op('act', 'activation', [ptk, 'sc1p', 'modT'], [uk], out=uT[:, k, :],
                               in_=pt[:, kk * 512:(kk + 1) * 512], func=AF.Identity,
                               bias=modT[:, k:k + 1], scale=sc1p[:, k:k + 1])
                        else:
                            op('dve', 'tensor_scalar', [ptk, 'sc1p', 'modT'], [uk], uT[:, k, :],
                               pt[:, kk * 512:(kk + 1) * 512], sc1p[:, k:k + 1], modT[:, k:k + 1],
                               ALU.mult, ALU.add)

                if NCH > 0:
                    emit_ln(0)
                    for r in range(4):
                        emit_tr(0, r)
                for c in range(NCH):
                    own = c >= 4
                    uT = uTs[c % 2]
                    if c + 1 < NCH:
                        emit_ln(c + 1)
                    pend_tr = list(range(4)) if c + 1 < NCH else []
                    uTr = ['uT%d_%d' % (c % 2, k) for k in range(8)]
                    cts = list(range(0, 4)) + (list(range(4, 12)) if own else []) + list(range(12, 16))
                    if 'fm' in SKIP:
                        cts = []
                    every = max(1, len(cts) // 4)
                    for ci, ct in enumerate(cts):
                        if pend_tr and ci > 0 and ci % every == 0:
                            emit_tr(c + 1, pend_tr.pop(0))
                        pp, pk = [(pA, 'pA'), (pB, 'pB'), (pO, 'pO'), (pM, 'pM')][ci % 4]
                        for k in range(8):
                            mm(pp[:, :], winb[:, k, ct * 128:(ct + 1) * 128], uT[:, k, :],
                               [winr[k], uTr[k]], [pk], k == 0, k == 7)
                        j = ct % 4
                        if ct < 4:
                            sg = stg[nstg % 2]
                            sk = 'stg%d' % (nstg % 2)
                            nstg += 1
                            op('act', 'copy', [pk], [sk], sg[:], pp[:, :])
                            P.dma('sp', xlru_s[j, :, c * 512:(c + 1) * 512], sg[:], reads=[sk], writes=['xlru_s'])
                        elif ct < 8:
                            gb = gyb[j % 2]
                            gk = 'gyb%d' % (j % 2)
                            op('act', 'copy', [pk], ['ysb'], ysb[:], pp[:, :])
                            op('pool', 'tensor_tensor', ['ysb'], ['yt'], yt[:], ysb[:], ysb[:], ALU.mult)
                            op('pool', 'tensor_scalar', ['yt'], ['yt'], yt[:], yt[:], 0.044715, 1.0, ALU.mult, ALU.add)
                            op('pool', 'tensor_tensor', ['yt', 'ysb'], ['yt'], yt[:], yt[:], ysb[:], ALU.mult)
                            op('act', 'activation', ['yt'], ['ysg'], out=ysg[:], in_=yt[:], func=AF.Sigmoid,
                               scale=1.5957691216057308)
                            op('pool', 'tensor_tensor', ['ysg', 'ysb'], [gk], gb[:], ysg[:], ysb[:], ALU.mult)
                            P.dma('sp', gy_s[j, :, (c - 4) * 512:(c - 3) * 512], gb[:], reads=[gk], writes=['gy_s'])
                        elif ct < 12:
                            oc = (c - 4) * 512
                            gb = gyb[j % 2]
                            gk = 'gyb%d' % (j % 2)
                            op('act', 'copy', [pk], [gk], gb[:], pp[:, :])
                            P.dma('sp', q_s[j, :, oc:oc + 512], gb[:], reads=[gk], writes=['q_s'])
                        else:
                            ke, km = ('act', 'copy') if j % 2 == 0 else ('dve', 'tensor_copy')
                            op(ke, km, [pk], ['kT%d' % (2 * j)], kT[2 * j][0:64, c * 512:(c + 1) * 512], pp[0:64, :])
                            op(ke, km, [pk], ['kT%d' % (2 * j + 1)],
                               kT[2 * j + 1][0:64, c * 512:(c + 1) * 512], pp[64:128, :])
                    for t in range(0 if 'v' in SKIP else 4):
                        if pend_tr:
                            emit_tr(c + 1, pend_tr.pop(0))
                        T = 4 * c + t
                        pp, pk = (pS0, 'pS0') if t % 2 == 0 else (pS1, 'pS1')
                        for k in range(8):
                            mm(pp[:, :], uT[:, k, t * 128:(t + 1) * 128], winb[:, k, 2048:2560],
                               [winr[k], uTr[k]], [pk], k == 0, k == 7)
                        op('dve' if t % 2 == 0 else 'act', 'tensor_copy' if t % 2 == 0 else 'copy', [pk], ['Vt'],
                           Vt[:, T, :].rearrange("p (h e) -> p h e", e=65)[:, :, 0:64],
                           pp[:, :].rearrange("p (h d) -> p h d", d=64))
                    while pend_tr:
                        emit_tr(c + 1, pend_tr.pop(0))
                if DBG:
                    for h in (0, 1, 7):
                        P.dma('sp', dout("d_kT%d" % h, [80, CT], BF16), kT[h][0:80, :], reads=['kT%d' % h, 'kTind%d' % h])
                    P.dma('sp', dout("d_V", [128, 32 * 520], BF16), Vt[:].rearrange("p t c -> p (t c)"), reads=['Vt'])
                P.barrier()
                if STOP == 1:
                    raise _Stop()

            P.flush()
            P.schedule = ('3' in SCHED_PH) and (os.environ.get('MK_SCHED', '1') == '1')
            with contextlib.ExitStack() as s3:
                qT = [sb(s3, "qT%d" % h, [96, NOWN], BF16) for h in range(NH)]
                for h in range(NH):
                    P.dma('sp', qT[h][0:64, :], q_s[h // 2, (h % 2) * 64:(h % 2) * 64 + 64, :], reads=['q_s'], writes=['qT%d' % h])
                    op('dve', 'memset', [], ['qTm%d' % h], qT[h][64:96, :], 0.0)
                apair = sb(s3, "apair", [128, 512], BF16)
                gmask = sb(s3, "gmask_t", [128, 16, 16], F32)
                ownhot = sb(s3, "ownhot_t", [128, 16, 16], F32)
                P.dma('sp', gmask[:].rearrange("p a b -> p (a b)"), gmask_d, writes=['gmask'])
                P.dma('sp', ownhot[:].rearrange("p a b -> p (a b)"), ownhot_d, writes=['ownhot'])
                cfar = sb(s3, "cfar", [128, 8], F32)
                biasD = sb(s3, "biasD", [128, 8, 128], F32)
                biasS = sb(s3, "biasS", [128, 8, 128], F32)
                kmf = sb(s3, "kmf", [64, NH, 16], F32)
                kmb = sb(s3, "kmb", [64, NH, 16], BF16)
                s3a = contextlib.ExitStack()
                s3a.__enter__()
                relsb = sb(s3a, "relsb", [32, 8], F32)
                Rsb = sb(s3a, "Rsb", [32, 384], F32)
                Gsb = sb(s3a, "Gsb", [8, 384], F32)
                negsb = sb(s3a, "negsb", [8, 384], F32)
                P.dma('sp', relsb[:], relb, writes=['relsb'])
                P.dma('sp', Rsb[:], Roh, writes=['Rsb'])
                P.dma('sp', negsb[:], NEGr, writes=['negsb'])
                P.dma('sp', cfar[:], relb[31:32, :].rearrange("a h -> (a h)").partition_broadcast(128), writes=['cfar'])
                mm(pA[0:8, 0:384], relsb[:, :], Rsb[:, :], ['relsb', 'Rsb'], ['pA'], True, True)
                op('dve', 'tensor_tensor', ['pA', 'negsb'], ['Gsb'], Gsb[:], pA[0:8, 0:384], negsb[:], ALU.add)
                P.dma('sp', G_s.ap(), Gsb[:], reads=['Gsb'], writes=['G_s'])
                hank = sb(s3a, "hank", [128, 16, 128], F32)
                for h in range(NH):
                    P.dma('sp', hank[:, h, :], bass.AP(G_s, h * 384 + 128, [[1, 128], [1, 128]]),
                          reads=['G_s'], writes=['hank'])
                    P.dma('sp', hank[:, 8 + h, :], bass.AP(G_s, h * 384, [[1, 128], [1, 128]]),
                          reads=['G_s'], writes=['hank'])
                for h in range(NH):
                    op('pool', 'tensor_copy', ['hank'], ['biasD'], biasD[:, h, :], hank[:, h, ::-1])
                    op('pool', 'tensor_copy', ['hank'], ['biasS'], biasS[:, h, :], hank[:, 8 + h, ::-1])
                for h in range(NH):
                    op('dve', 'tensor_reduce', ['kT%d' % h], ['kmf'], out=kmf[:, h, :],
                       in_=kT[h][0:64, :].rearrange("p (n b) -> p n b", b=BLK), axis=AX.X, op=ALU.add)
                op('dve', 'tensor_scalar_mul', ['kmf'], ['kmb'], kmb[:].rearrange("p h n -> p (h n)"),
                   kmf[:].rearrange("p h n -> p (h n)"), 1.0 / BLK)
                _sch3 = P.schedule
                s3a.__exit__(None, None, None)
                P.barrier()
                P.schedule = _sch3
                pT0f = pT0[:].bitcast(F32)
                pT1f = pT1[:].bitcast(F32)
                cw = sb(s3, "cw", [128, 16], F32)
                cb = sb(s3, "cb", [128, 4], F32)
                bA = sb(s3, "bA", [128, 4], F32)
                bX = sb(s3, "bX", [128, 4], F32)
                lamt = sb(s3, "lamt", [128, 4], F32)
                cL = sb(s3, "cL", [128, 4], F32)
                cL2 = sb(s3, "cL2", [128, 4], F32)
                flag = sb(s3, "flag_t", [128, 1], F32)
                carry = sb(s3, "carry", [128, 4], F32)
                WAb = sb(s3, "WAb", [128, 4, 128], BF16)
                WXb = sb(s3, "WXb", [128, 4, 128], BF16)
                P.dma('sp', cw[:], convw, writes=['cw'])
                P.dma('sp', cb[:], convb, writes=['cb'])
                P.dma('sp', bA[:], bga, writes=['bA'])
                P.dma('sp', bX[:], bgx, writes=['bX'])
                P.dma('sp', lamt[:], lam, writes=['lamt'])
                P.dma('sp', flag[:], flag_d, writes=['flag'])
                P.dma('pool', WAb[:], WA.rearrange("j p o -> p j o"), writes=['WAb'])
                P.dma('pool', WXb[:], WX.rearrange("j p o -> p j o"), writes=['WXb'])
                op('act', 'activation', ['lamt'], ['cL'], out=cL[:], in_=lamt[:], func=AF.Exp, scale=-1.0)
                op('act', 'activation', ['cL'], ['cL'], out=cL[:], in_=cL[:], func=AF.Ln, bias=1.0)
                op('dve', 'tensor_scalar_mul', ['cL'], ['cL2'], cL2[:], cL[:], -16.0)
                op('dve', 'tensor_scalar_mul', ['cL'], ['cL'], cL[:], cL[:], -8.0)
                op('dve', 'memset', [], ['carry'], carry[:], 0.0)
                nbA = sb(s3, "nbA", [128, 4], F32)
                nbX = sb(s3, "nbX", [128, 4], F32)
                op('dve', 'tensor_scalar_mul', ['bA'], ['nbA'], nbA[:], bA[:], -1.0)
                op('dve', 'tensor_scalar_mul', ['bX'], ['nbX'], nbX[:], bX[:], -1.0)
                W = 512
                NB2 = 2
                def mk(name, shape, dt):
                    return [sb(s3, "%s_%d" % (name, i), shape, dt) for i in range(NB2)]
                xl = mk("xl", [128, 3 + W], F32)
                xc = mk("xc", [128, W], F32)
                xcb = mk("xcb", [128, W], BF16)
                rr = mk("rr", [128, W], F32)
                ii = mk("ii", [128, W], F32)
                aa = mk("aa", [128, W], F32)
                m2 = mk("m2", [128, W], F32)
                hh = mk("hh", [128, W], F32)
                gyl = mk("gyl", [128, W], BF16)
                sq = mk("sq", [128, W], BF16)
                lop = mk("lop", [128, W], BF16)
                npc_box = [0]

                def lru_piece(j, pc):
                    b = npc_box[0] % NB2
                    npc_box[0] += 1
                    K_ = lambda n: '%s_%d' % (n, b)
                    if pc == 0:
                        op('dve', 'memset', [], [K_('xl')], xl[b][:, 0:3], 0.0)
                        P.dma('sp', xl[b][:, 3:3 + W], xlru_s[j, :, 0:W], reads=['xlru_s'], writes=[K_('xl')])
                    else:
                        P.dma('sp', xl[b][:, :], xlru_s[j, :, pc * W - 3:(pc + 1) * W], reads=['xlru_s'], writes=[K_('xl')])
                    if pc >= 4:
                        oc = (pc - 4) * W
                        P.dma('sp', gyl[b][:], gy_s[j, :, oc:oc + W], reads=['gy_s'], writes=[K_('gyl')])
                    op('dve', 'tensor_scalar', [K_('xl'), 'cw', 'cb'], [K_('xc')], xc[b][:], xl[b][:, 0:W],
                       cw[:, j * 4:j * 4 + 1], cb[:, j:j + 1], ALU.mult, ALU.add)
                    for k in range(1, 4):
                        op('dve', 'scalar_tensor_tensor', [K_('xl'), 'cw', K_('xc')], [K_('xc')], out=xc[b][:], in0=xl[b][:, k:k + W],
                           scalar=cw[:, j * 4 + k:j * 4 + k + 1], in1=xc[b][:], op0=ALU.mult, op1=ALU.add)
                    op('pool', 'tensor_copy', [K_('xc')], [K_('xcb')], xcb[b][:], xc[b][:])
                    mm(pT0f, WAb[:, j, :], xcb[b][:, :], ['WAb', K_('xcb')], ['pT0'], True, True)
                    op('act', 'activation', ['pT0', 'nbA'], [K_('rr')], out=rr[b][:, :], in_=pT0f,
                       func=AF.Exp, bias=nbA[:, j:j + 1], scale=-1.0)
                    mm(pT0f, WXb[:, j, :], xcb[b][:, :], ['WXb', K_('xcb')], ['pT0'], True, True)
                    op('act', 'activation', ['pT0', 'nbX'], [K_('ii')], out=ii[b][:, :], in_=pT0f,
                       func=AF.Exp, bias=nbX[:, j:j + 1], scale=-1.0)
                    op('act', 'activation', [K_('rr')], [K_('rr')], out=rr[b][:], in_=rr[b][:], func=AF.Ln, bias=1.0)
                    op('act', 'activation', [K_('rr')], [K_('rr')], out=rr[b][:], in_=rr[b][:], func=AF.Exp, scale=-1.0)
                    op('act', 'activation', [K_('ii')], [K_('ii')], out=ii[b][:], in_=ii[b][:], func=AF.Ln, bias=1.0)
                    op('act', 'activation', [K_('ii')], [K_('ii')], out=ii[b][:], in_=ii[b][:], func=AF.Exp, scale=-1.0)
                    op('act', 'activation', [K_('rr'), 'cL'], [K_('aa')], out=aa[b][:], in_=rr[b][:], func=AF.Exp, scale=cL[:, j:j + 1])
                    op('act', 'activation', [K_('rr'), 'cL2'], [K_('m2')], out=m2[b][:], in_=rr[b][:], func=AF.Exp, scale=cL2[:, j:j + 1])
                    op('act', 'activation', [K_('m2')], [K_('m2')], out=m2[b][:], in_=m2[b][:], func=AF.Ln, scale=-1.0, bias=1.0)
                    op('act', 'activation', [K_('m2')], [K_('m2')], out=m2[b][:], in_=m2[b][:], func=AF.Exp, scale=0.5)
                    op('pool', 'tensor_tensor', [K_('ii'), K_('xc')], [K_('ii')], ii[b][:], ii[b][:], xc[b][:], ALU.mult)
                    op('pool', 'tensor_tensor', [K_('ii'), K_('m2')], [K_('ii')], ii[b][:], ii[b][:], m2[b][:], ALU.mult)
                    if pc == 4:
                        op('dve', 'tensor_tensor', ['carry', 'flag'], ['carry'], carry[:, j:j + 1], carry[:, j:j + 1],
                           flag[:], ALU.mult)
                    op('dve', 'tensor_tensor_scan', [K_('aa'), K_('ii'), 'carry'], [K_('hh')], out=hh[b][:], data0=aa[b][:], data1=ii[b][:],
                       initial=carry[:, j:j + 1], op0=ALU.mult, op1=ALU.add)
                    op('dve', 'tensor_copy', [K_('hh')], ['carry'], carry[:, j:j + 1], hh[b][:, W - 1:W])
                    if pc >= 4:
                        oc = (pc - 4) * W
                        op('pool', 'tensor_tensor', [K_('hh'), K_('gyl')], [K_('lop')], lop[b][:], hh[b][:], gyl[b][:], ALU.mult)
                        P.dma('sp', lo_s[j, :, oc:oc + W], lop[b][:], reads=[K_('lop')], writes=['lo_s'])
                        op('pool', 'tensor_tensor', [K_('lop')], [K_('sq')], sq[b][:], lop[b][:], lop[b][:], ALU.mult)
                        for t in range(4):
                            pe_raw('matmul', [K_('sq'), 'onesb'], ['pT1'], sig=(t == 3), argskw=ARGS(pT1f[:, t:t + 1], sq[b][:, t * 128:(t + 1) * 128], onesb[:, 0:1],
                                                                start=True, stop=True))
                        t0 = (pc - 4) * 4
                        op('dve', 'tensor_tensor', ['pT1', 'ssl'], ['ssl'], ssl[:, t0:t0 + 4], ssl[:, t0:t0 + 4], pT1f[:, 0:4], ALU.add)
                lru_list = [(j, pc) for j in range(4) for pc in range(8)]
                gsb = sb(s3, "gsb", [128, NH, 16], F32)
                top8 = sb(s3, "top8", [128, NH, 8], F32)
                sel = sb(s3, "sel", [128, NH, 16], F32)
                mvb = sb(s3, "mvb", [128, NH, 16], BF16)
                for qt in range(16):
                    if qt % 2 == 0 and lru_list:
                        lru_piece(*lru_list.pop(0))
                    for h in range(NH):
                        pe_raw('matmul', ['qT%d' % h, 'kmb'], ['pM'], sig=(h == NH - 1), argskw=ARGS(pM[:, h * 16:(h + 1) * 16], qT[h][0:64, qt * 128:(qt + 1) * 128],
                                                            kmb[:, h, :], start=True, stop=True))
                    op('dve', 'tensor_tensor', ['pM', 'gmask'], ['gsb'], gsb[:],
                       pM[:, 0:128].rearrange("p (h n) -> p h n", n=16),
                       gmask[:, qt:qt + 1, :].to_broadcast([128, NH, 16]), ALU.add)
                    for h in range(NH):
                        op('dve', 'max', ['gsb'], ['top8'], out=top8[:, h, :], in_=gsb[:, h, :])
                    op('dve', 'tensor_tensor', ['gsb', 'top8'], ['sel'], sel[:], gsb[:],
                       top8[:, :, 2:3].to_broadcast([128, NH, 16]), ALU.is_ge)
                    op('dve', 'scalar_tensor_tensor', ['gsb', 'sel'], ['sel'], out=sel[:], in0=gsb[:], scalar=-1e29,
                       in1=sel[:], op0=ALU.is_gt, op1=ALU.mult)
                    op('dve', 'tensor_tensor', ['sel', 'ownhot'], ['sel'], sel[:], sel[:],
                       ownhot[:, qt:qt + 1, :].to_broadcast([128, NH, 16]), ALU.add)
                    op('dve', 'tensor_scalar', ['sel'], ['mvb'], mvb[:], sel[:], -1.0, -NEGM, ALU.add, ALU.mult)
                    for h in range(NH):
                        pe_raw('transpose', ['mvb', 'ident'], ['pT1'], sig=(h == NH - 1), argskw=ARGS(pT1[0:16, h * 128:(h + 1) * 128], mvb[:, h, :], ident[:]))
                    for h in range(NH):
                        op('act' if qt % 2 == 0 else 'dve', 'copy' if qt % 2 == 0 else 'tensor_copy', ['pT1'],
                           ['qTm%d' % h], qT[h][64:80, qt * 128:(qt + 1) * 128], pT1[0:16, h * 128:(h + 1) * 128])
                if DBG:
                    for h in (0, 1, 7):
                        P.dma('sp', dout("d_qm%d" % h, [16, NOWN], BF16), qT[h][64:80, :], reads=['qTm%d' % h])
                    P.dma('sp', dout("d_biasD", [128, 8 * 128]), biasD[:].rearrange("p h n -> p (h n)"), reads=['biasD'])
                    P.dma('sp', dout("d_biasS", [128, 8 * 128]), biasS[:].rearrange("p h n -> p (h n)"), reads=['biasS'])
                PT = [sb(s3, "PT%d" % i, [128, 512], BF16) for i in range(3)]
                tmpS = [sb(s3, "tmpS%d" % i, [128, 128], F32) for i in range(2)]
                osb = sb(s3, "osb", [65, 512], F32)
                sqa = sb(s3, "sqa", [128, 512], BF16)
                SCALE = HD ** -0.5
                npt = 0
                nts = 0
                nonlocal_nsb = [0]
                Sb = [(pS0, 'pS0'), (pS1, 'pS1'), (pA, 'pA')]
                Ob = [(pO, 'pO'), (pB, 'pB')]
                nob = 0
                for cq in range(4):
                    c = 4 + cq
                    for h in range(NH):
                        if lru_list:
                            lru_piece(*lru_list.pop(0))
                        j, s = h // 2, h % 2
                        qr = ['qT%d' % h, 'qTm%d' % h]
                        kr = ['kT%d' % h, 'kTind%d' % h]
                        nkt = 4 * c + 4
                        pOc, pOk = Ob[nob % 2]
                        nob += 1

                        def geom(kt):
                            qlo = max(kt, 4 * c)
                            n0 = (qlo - 4 * c) * 128
                            return qlo, n0

                        def issue_S(kt):
                            nonlocal_nsb[0] += 1
                            pS, pSk = Sb[nonlocal_nsb[0] % 3]
                            qlo, n0 = geom(kt)
                            mm(pS[:, n0:512], kT[h][0:96, kt * 128:(kt + 1) * 128],
                               qT[h][0:96, cq * 512 + n0: cq * 512 + 512], qr + kr, [pSk], True, True)
                            return pS, pSk

                        pendq = [issue_S(0)]
                        if nkt > 1:
                            pendq.append(issue_S(1))
                        for kt in range(nkt):
                            pS, pSk = pendq.pop(0)
                            qlo, n0 = geom(kt)
                            pt = PT[npt % 3]
                            ptk = 'PT%d' % (npt % 3)
                            npt += 1
                            col = n0
                            nearks = []
                            for qtile in range(qlo, 4 * c + 4):
                                d = qtile - kt
                                if d > 1:
                                    break
                                bt = biasD if d == 0 else biasS
                                ts_, tsk = tmpS[nts % 2], 'tmpS%d' % (nts % 2)
                                nts += 1
                                op('dve', 'scalar_tensor_tensor', [pSk, 'biasD', 'biasS'], [tsk], out=ts_[:],
                                   in0=pS[:, col:col + 128], scalar=SCALE, in1=bt[:, h, :], op0=ALU.mult, op1=ALU.add)
                                op('act', 'activation', [tsk], [ptk], out=pt[:, col:col + 128], in_=ts_[:], func=AF.Exp)
                                nearks.append(tsk)
                                col += 128
                            if col < 512:
                                op('act', 'activation', [pSk, 'cfar'] + nearks, [ptk], out=pt[:, col:512], in_=pS[:, col:512],
                                   func=AF.Exp, bias=cfar[:, h:h + 1], scale=SCALE)
                            mm(pOc[0:65, n0:512], Vt[:, kt, h * 65:(h + 1) * 65], pt[:, n0:512], ['Vt', ptk], [pOk],
                               kt == 0, kt == nkt - 1)
                            if kt + 2 < nkt:
                                pendq.append(issue_S(kt + 2))
                        op('act', 'copy', [pOk], ['osb', 'osbr'], osb[:, :], pOc[0:65, :])
                        op('dve', 'reciprocal', ['osb'], ['osbr'], osb[64:65, :], osb[64:65, :])
                        pe_raw('matmul', ['osbr', 'ones32'], ['pM'], argskw=ARGS(pM[0:64, :], ones32[64:65, 0:64], osb[64:65, :],
                                                            start=True, stop=True))
                        op('dve', 'tensor_tensor', ['osb', 'pM'], ['apair'], apair[s * 64:(s + 1) * 64, :],
                           osb[0:64, :], pM[0:64, :], ALU.mult)
                        if s == 1:
                            P.dma('sp', a_s[j, :, cq * 512:(cq + 1) * 512], apair[:], reads=['apair'], writes=['a_s'])
                            op('pool', 'tensor_tensor', ['apair'], ['sqa'], sqa[:], apair[:], apair[:], ALU.mult)
                            for t in range(4):
                                pe_raw('matmul', ['sqa', 'onesb'], ['pM'], sig=(t == 3), argskw=ARGS(pM[:, t:t + 1], sqa[:, t * 128:(t + 1) * 128], onesb[:, 0:1],
                                                                    start=True, stop=True))
                            op('dve', 'tensor_tensor', ['pM', 'ssa'], ['ssa'], ssa[:, cq * 4:cq * 4 + 4],
                               ssa[:, cq * 4:cq * 4 + 4], pM[:, 0:4], ALU.add)
                while lru_list:
                    lru_piece(*lru_list.pop(0))
                if DBG:
                    P.dma('sp', dout("d_ssa", [128, 16]), ssa[:], reads=['ssa'])
                    P.dma('sp', dout("d_ssl", [128, 16]), ssl[:], reads=['ssl'])
                P.barrier()
                if STOP == 3:
                    raise _Stop()

        P.flush()
        P.schedule = ('4' in SCHED_PH) and (os.environ.get('MK_SCHED', '1') == '1')
        lnB = sb(top, "lnB", [128, 4, D], F32)
        u2T = sb(top, "u2T", [128, 8, NOWN], BF16)
        P.dma('sp', lnB[:].rearrange("p a d -> p (a d)"),
              lnv.rearrange("a d -> (a d)").partition_broadcast(128), writes=['lnB'])
        u2r = ['u2T%d' % k for k in range(8)]
        wrb = sb(top, "wrb", [128, 8, 36], BF16)
        brB = sb(top, "brB", [128, 36], F32)
        P.dma('pool', wrb[:], w_r.rearrange("(k p) n -> p k n", p=128), writes=['wrb'])
        P.dma('sp', brB[:], b_r.rearrange("a n -> (a n)").partition_broadcast(128), writes=['brB'])
        gate = sb(top, "gate", [128, 16, NE], F32)
        lg = sb(top, "lg", [128, 36], F32)
        gmax = sb(top, "gmax", [128, 1], F32)
        ngmax = sb(top, "ngmax", [128, 1], F32)
        gex = sb(top, "gex", [128, 4], F32)
        gsum = sb(top, "gsum", [128, 1], F32)
        gtop = sb(top, "gtop", [128, 1], F32)
        goh = sb(top, "goh", [128, 4], F32)
        esel = sb(top, "esel", [128, 4, 8], F32)
        ein = sb(top, "ein", [128, 8], F32)
        et8 = sb(top, "et8", [128, 8], F32)
        nl1 = sb(top, "nl1", [128, 1], F32)
        eex = sb(top, "eex", [128, 8], F32)
        esl = sb(top, "esl", [128, 8], F32)
        eden = sb(top, "eden", [128, 1], F32)
        with contextlib.ExitStack() as s4:
            def router(tt):
                for k in range(8):
                    mm(pM[:, 0:36], u2T[:, k, tt * 128:(tt + 1) * 128], wrb[:, k, :], [u2r[k], 'wrb'], ['pM'], k == 0, k == 7)
                op('dve', 'tensor_tensor', ['pM', 'brB'], ['lg'], lg[:], pM[:, 0:36], brB[:], ALU.add)
                op('dve', 'tensor_reduce', ['lg'], ['gmax'], out=gmax[:], in_=lg[:, 0:4], axis=AX.X, op=ALU.max)
                op('dve', 'tensor_scalar_mul', ['gmax'], ['ngmax'], ngmax[:], gmax[:], -1.0)
                op('act', 'activation', ['lg', 'ngmax'], ['gex'], out=gex[:], in_=lg[:, 0:4], func=AF.Exp, bias=ngmax[:])
                op('dve', 'tensor_reduce', ['gex'], ['gsum'], out=gsum[:], in_=gex[:], axis=AX.X, op=ALU.add)
                op('dve', 'reciprocal', ['gsum'], ['gtop'], gtop[:], gsum[:])
                op('dve', 'tensor_tensor', ['lg', 'gmax'], ['goh'], goh[:], lg[:, 0:4], gmax[:].to_broadcast([128, 4]), ALU.is_ge)
                op('dve', 'tensor_tensor', ['lg', 'goh'], ['esel'], esel[:], lg[:, 4:36].rearrange("p (g e) -> p g e", e=8),
                   goh[:].unsqueeze(2).to_broadcast([128, 4, 8]), ALU.mult)
                op('dve', 'tensor_reduce', ['esel'], ['ein'], out=ein[:], in_=esel[:].rearrange("p g e -> p e g"),
                   axis=AX.X, op=ALU.add)
                op('dve', 'max', ['ein'], ['et8'], out=et8[:], in_=ein[:])
                op('dve', 'tensor_scalar_mul', ['et8'], ['nl1'], nl1[:], et8[:, 0:1], -1.0)
                op('act', 'activation', ['ein', 'nl1'], ['eex'], out=eex[:], in_=ein[:], func=AF.Exp, bias=nl1[:])
                op('dve', 'tensor_tensor', ['ein', 'et8'], ['esl'], esl[:], ein[:], et8[:, 1:2].to_broadcast([128, 8]), ALU.is_ge)
                op('dve', 'tensor_tensor', ['esl', 'eex'], ['esl'], esl[:], esl[:], eex[:], ALU.mult)
                op('dve', 'tensor_reduce', ['esl'], ['eden'], out=eden[:], in_=esl[:], axis=AX.X, op=ALU.add)
                op('dve', 'reciprocal', ['eden'], ['eden'], eden[:], eden[:])
                op('dve', 'tensor_tensor', ['eden', 'gtop'], ['eden'], eden[:], eden[:], gtop[:], ALU.mult)
                op('dve', 'tensor_scalar', ['esl', 'eden'], ['esl'], esl[:], esl[:], eden[:, 0:1], None, ALU.mult)
                op('dve', 'tensor_tensor', ['goh', 'esl'], ['gate'], gate[:, tt, :].rearrange("p (g e) -> p g e", e=8),
                   goh[:].unsqueeze(2).to_broadcast([128, 4, 8]), esl[:].unsqueeze(1).to_broadcast([128, 4, 8]), ALU.mult)
            loT = sb(s4, "loT", [128, 4, NOWN], BF16)
            aTp = sb(s4, "aTp", [128, 4, NOWN], BF16)
            for jj in range(4):
                P.dma('sp', loT[:, jj, :], lo_s[jj], reads=['lo_s'], writes=['loT%d' % jj])
                P.dma('sp', aTp[:, jj, :], a_s[jj], reads=['a_s'], writes=['aTp%d' % jj])
            if DBG:
                P.dma('sp', dout("d_loT", [128, 4 * NOWN], BF16), loT[:].rearrange("p j n -> p (j n)"),
                      reads=['loT%d' % j for j in range(4)])
                P.dma('sp', dout("d_aTp", [128, 4 * NOWN], BF16), aTp[:].rearrange("p j n -> p (j n)"),
                      reads=['aTp%d' % j for j in range(4)])
            woutb = sb(s4, "woutb", [128, 8, D], BF16)
            wo32 = [sb(s4, "wo32_%d" % i, [128, D], F32) for i in range(2)]
            gl = sb(s4, "gl", [128, 8], F32)
            P.dma('sp', gl[:, 0:4], glru, writes=['gl'])
            P.dma('sp', gl[:, 4:8], gattn, writes=['gl'])
            for k in range(8):
                wb, wk = wo32[k % 2], 'wo32_%d' % (k % 2)
                P.dma('sp', wb[:], w_out[k * 128:(k + 1) * 128, :], writes=[wk])
                op('act', 'activation', [wk, 'gl'], ['woutb%d' % k], out=woutb[:, k, :], in_=wb[:], func=AF.Identity, scale=gl[:, k:k + 1])
            NB4 = 3
            xo = [sb(s4, "xo%d" % i, [128, D], F32) for i in range(NB4)]
            mix = [sb(s4, "mix%d" % i, [128, D], F32) for i in range(NB4)]
            zz = [sb(s4, "zz%d" % i, [128, D], F32) for i in range(NB4)]
            x1 = [sb(s4, "x1_%d" % i, [128, D], F32) for i in range(NB4)]
            xn2 = [sb(s4, "xn2_%d" % i, [128, D], BF16) for i in range(NB4)]
            NST = 6
            st6 = [sb(s4, "st6b%d" % i, [128, 2, 6], F32) for i in range(NST)]
            mv = [sb(s4, "mvb%d" % i, [128, 2], F32) for i in range(NST)]
            rstd = [sb(s4, "rstdb%d" % i, [128, 1], F32) for i in range(NST)]
            nb = [sb(s4, "nbb%d" % i, [128, 1], F32) for i in range(NST)]
            ve4 = [sb(s4, "veb%d" % i, [128, 1], F32) for i in range(NST)]
            rl = sb(s4, "rl", [128, 16], F32)
            ra = sb(s4, "ra", [128, 16], F32)
            op('dve', 'tensor_scalar', ['ssl'], ['rl'], rl[:], ssl[:], 1.0 / 512, EPS, ALU.mult, ALU.add)
            op('pool', 'tensor_tensor', ['rl', 'mhalf'], ['rl'], rl[:], rl[:], mhalf[:, 0:16], ALU.pow)
            op('dve', 'tensor_scalar', ['ssa'], ['ra'], ra[:], ssa[:], 1.0 / 512, EPS, ALU.mult, ALU.add)
            op('pool', 'tensor_tensor', ['ra', 'mhalf'], ['ra'], ra[:], ra[:], mhalf[:, 0:16], ALU.pow)

            def ln_stats(src, srck, i):
                for hf in range(2):
                    op('dve', 'bn_stats', [srck], ['st6_%d' % i], out=st6[i][:, hf, :], in_=src[:, hf * 512:(hf + 1) * 512])
                op('dve', 'bn_aggr', ['st6_%d' % i], ['mv_%d' % i], out=mv[i][:], in_=st6[i][:].rearrange("p a b -> p (a b)"))
                op('dve', 'tensor_scalar_add', ['mv_%d' % i], ['ve_%d' % i], ve4[i][:], mv[i][:, 1:2], EPS)
                op('pool', 'tensor_tensor', ['ve_%d' % i, 'mhalf'], ['rstd_%d' % i], rstd[i][:], ve4[i][:], mhalf[:, 0:1], ALU.pow)
                op('dve', 'scalar_tensor_tensor', ['mv_%d' % i, 'rstd_%d' % i], ['nb_%d' % i], out=nb[i][:], in0=mv[i][:, 0:1],
                   scalar=-1.0, in1=rstd[i][:], op0=ALU.mult, op1=ALU.mult)

            for tt in range(16):
                b = tt % NB4
                sa_, sb_ = (2 * tt) % NST, (2 * tt + 1) % NST
                xok, mixk, zzk, x1k, xn2k = 'xo%d' % b, 'mix%d' % b, 'zz%d' % b, 'x1_%d' % b, 'xn2_%d' % b
                P.dma('sp', xo[b][:], xctx[OWN0 + tt * 128: OWN0 + (tt + 1) * 128, :], writes=[xok])
                for hf in range(2):
                    pa, pak, pb, pbk = (pA, 'pA', pB, 'pB') if hf == 0 else (pS0, 'pS0', pS1, 'pS1')
                    for jj in range(4):
                        mm(pa[:, :], loT[:, jj, tt * 128:(tt + 1) * 128], woutb[:, jj, hf * 512:(hf + 1) * 512],
                           ['loT%d' % jj, 'woutb%d' % jj], [pak], jj == 0, jj == 3)
                    for jj in range(4):
                        mm(pb[:, :], aTp[:, jj, tt * 128:(tt + 1) * 128], woutb[:, 4 + jj, hf * 512:(hf + 1) * 512],
                           ['aTp%d' % jj, 'woutb%d' % (4 + jj)], [pbk], jj == 0, jj == 3)
                    op('act', 'activation', [pak, 'rl'], [mixk], out=mix[b][:, hf * 512:(hf + 1) * 512], in_=pa[:, :],
                       func=AF.Identity, scale=rl[:, tt:tt + 1])
                    op('dve', 'scalar_tensor_tensor', [pbk, 'ra', mixk], [mixk], out=mix[b][:, hf * 512:(hf + 1) * 512],
                       in0=pb[:, :], scalar=ra[:, tt:tt + 1], in1=mix[b][:, hf * 512:(hf + 1) * 512], op0=ALU.mult, op1=ALU.add)
                op('pool', 'tensor_tensor', [mixk, 'g1B'], [zzk], zz[b][:], mix[b][:], g1B[:], ALU.mult)
                op('dve', 'scalar_tensor_tensor', [xok, zzk], [zzk], out=zz[b][:], in0=xo[b][:], scalar=ALPHA, in1=zz[b][:],
                   op0=ALU.mult, op1=ALU.add)
                ln_stats(zz[b], zzk, sa_)
                op('act', 'activation', [zzk, 'rstd_%d' % sa_, 'nb_%d' % sa_], [x1k], out=x1[b][:], in_=zz[b][:], func=AF.Identity,
                   bias=nb[sa_][:], scale=rstd[sa_][:])
                op('pool', 'tensor_tensor', [x1k, 'lnB'], [x1k], x1[b][:], x1[b][:], lnB[:, 0, :], ALU.mult)
                op('dve', 'tensor_tensor', [x1k, 'lnB'], [x1k], x1[b][:], x1[b][:], lnB[:, 1, :], ALU.add)
                P.dma('sp', x1_s[tt * 128:(tt + 1) * 128, :], x1[b][:], reads=[x1k], writes=['x1_s'])
                ln_stats(x1[b], x1k, sb_)
                op('act', 'activation', [x1k, 'rstd_%d' % sb_, 'nb_%d' % sb_], [xn2k], out=xn2[b][:], in_=x1[b][:], func=AF.Identity,
                   bias=nb[sb_][:], scale=rstd[sb_][:])
                pt, ptk = (pT0, 'pT0') if tt % 2 == 0 else (pT1, 'pT1')
                for k in range(8):
                    pe_raw('transpose', [xn2k, 'ident'], [ptk], sig=(k == 7), argskw=ARGS(pt[:, k * 128:(k + 1) * 128], xn2[b][:, k * 128:(k + 1) * 128], ident[:]))
                for k in range(8):
                    if tt % 2 == 0:
                        op('act', 'activation', [ptk, 'sc2p', 'modT'], ['u2T%d' % k], out=u2T[:, k, tt * 128:(tt + 1) * 128],
                           in_=pt[:, k * 128:(k + 1) * 128], func=AF.Identity, bias=modT[:, 24 + k:25 + k],
                           scale=sc2p[:, k:k + 1])
                    else:
                        op('dve', 'tensor_scalar', [ptk, 'sc2p', 'modT'], ['u2T%d' % k], u2T[:, k, tt * 128:(tt + 1) * 128],
                           pt[:, k * 128:(k + 1) * 128], sc2p[:, k:k + 1], modT[:, 24 + k:25 + k], ALU.mult, ALU.add)
                router(tt)
            if DBG:
                P.dma('sp', dout("d_u2T", [128, 8 * NOWN], BF16), u2T[:].rearrange("p k n -> p (k n)"),
                      reads=['u2T%d' % k for k in range(8)])
            P.barrier()
            if STOP == 4:
                raise _Stop()

        P.flush()
        P.schedule = ('5' in SCHED_PH) and (os.environ.get('MK_SCHED', '1') == '1')
        with contextlib.ExitStack() as s5:
            if DBG:
                P.dma('sp', dout("d_gate", [128, 16 * NE]), gate[:].rearrange("p t e -> p (t e)"), reads=['gate'])
            NS = 3
            w1b = [sb(s5, "w1b%d" % i, [128, 8, DE], BF16) for i in range(NS)]
            w3b = [sb(s5, "w3b%d" % i, [128, 8, DE], BF16) for i in range(NS)]
            w2b = [sb(s5, "w2b%d" % i, [128, 2, D], BF16) for i in range(NS)]
            yacc = sb(s5, "yacc", [128, 16, D], F32)
            ssi = [sb(s5, "ssi%d" % i, [128, 512], F32) for i in range(2)]
            hdn = [sb(s5, "hdn%d" % i, [128, 512], BF16) for i in range(4)]
            nh_ = 0
            ny = 0
            for e in range(NE):
                sl = e % NS
                P.dma('pool', w1b[sl][:], w1[e].rearrange("(k p) f -> p k f", p=128), writes=['w1b%d' % sl])
                P.dma('pool', w3b[sl][:], w3[e].rearrange("(k p) f -> p k f", p=128), writes=['w3b%d' % sl])
                P.dma('pool', w2b[sl][:], w2[e].rearrange("(c p) d -> p c d", p=128), writes=['w2b%d' % sl])
                for tc in range(4):
                    hk = []
                    for fc in range(2):
                        p1, p1k, p3, p3k = (pA, 'pA', pB, 'pB') if fc == 0 else (pS0, 'pS0', pS1, 'pS1')
                        for k in range(8):
                            mm(p1[:, :], w1b[sl][:, k, fc * 128:(fc + 1) * 128], u2T[:, k, tc * 512:(tc + 1) * 512],
                               ['w1b%d' % sl, u2r[k]], [p1k], k == 0, k == 7)
                        for k in range(8):
                            mm(p3[:, :], w3b[sl][:, k, fc * 128:(fc + 1) * 128], u2T[:, k, tc * 512:(tc + 1) * 512],
                               ['w3b%d' % sl, u2r[k]], [p3k], k == 0, k == 7)
                        si, sik = ssi[fc], 'ssi%d' % fc
                        hd, hdk = hdn[nh_ % 4], 'hdn%d' % (nh_ % 4)
                        nh_ += 1
                        op('act', 'activation', [p1k], [sik], out=si[:], in_=p1[:, :], func=AF.Silu)
                        op('dve', 'tensor_tensor', [sik, p3k], [hdk], hd[:], si[:], p3[:, :], ALU.mult)
                        hk.append((hd, hdk))
                    for t in range(4):
                        tt = tc * 4 + t
                        for hf in range(2):
                            py, pyk = (pO, 'pO') if ny % 2 == 0 else (pM, 'pM')
                            ny += 1
                            for fc in range(2):
                                mm(py[:, :], hk[fc][0][:, t * 128:(t + 1) * 128], w2b[sl][:, fc, hf * 512:(hf + 1) * 512],
                                   [hk[fc][1], 'w2b%d' % sl], [pyk], fc == 0, fc == 1)
                            if e == 0:
                                op('dve', 'tensor_scalar', [pyk, 'gate'], ['yacc%d' % tt], yacc[:, tt, hf * 512:(hf + 1) * 512],
                                   py[:, :], gate[:, tt, e:e + 1], None, ALU.mult)
                            else:
                                op('dve', 'scalar_tensor_tensor', [pyk, 'gate', 'yacc%d' % tt], ['yacc%d' % tt],
                                   out=yacc[:, tt, hf * 512:(hf + 1) * 512], in0=py[:, :], scalar=gate[:, tt, e:e + 1],
                                   in1=yacc[:, tt, hf * 512:(hf + 1) * 512], op0=ALU.mult, op1=ALU.add)
            NB5 = 3
            x1l = [sb(s5, "x1l%d" % i, [128, D], F32) for i in range(NB5)]
            zf = [sb(s5, "zf%d" % i, [128, D], F32) for i in range(NB5)]
            of = [sb(s5, "of%d" % i, [128, D], F32) for i in range(NB5)]
            st6 = [sb(s5, "st6c%d" % i, [128, 2, 6], F32) for i in range(NB5)]
            mv = [sb(s5, "mvc%d" % i, [128, 2], F32) for i in range(NB5)]
            rstd = [sb(s5, "rstdc%d" % i, [128, 1], F32) for i in range(NB5)]
            nb = [sb(s5, "nbc%d" % i, [128, 1], F32) for i in range(NB5)]
            ve5 = [sb(s5, "vec%d" % i, [128, 1], F32) for i in range(NB5)]
            for tt in range(16):
                b = tt % NB5
                x1lk, zfk, ofk = 'x1l%d' % b, 'zf%d' % b, 'of%d' % b
                P.dma('sp', x1l[b][:], x1_s[tt * 128:(tt + 1) * 128, :], reads=['x1_s'], writes=[x1lk])
                op('pool', 'tensor_tensor', ['yacc%d' % tt, 'g2B'], [zfk], zf[b][:], yacc[:, tt, :], g2B[:], ALU.mult)
                op('dve', 'scalar_tensor_tensor', [x1lk, zfk], [zfk], out=zf[b][:], in0=x1l[b][:], scalar=ALPHA, in1=zf[b][:],
                   op0=ALU.mult, op1=ALU.add)
                for hf in range(2):
                    op('dve', 'bn_stats', [zfk], ['st6f%d' % b], out=st6[b][:, hf, :], in_=zf[b][:, hf * 512:(hf + 1) * 512])
                op('dve', 'bn_aggr', ['st6f%d' % b], ['mvf%d' % b], out=mv[b][:], in_=st6[b][:].rearrange("p a b -> p (a b)"))
                op('dve', 'tensor_scalar_add', ['mvf%d' % b], ['vef%d' % b], ve5[b][:], mv[b][:, 1:2], EPS)
                op('pool', 'tensor_tensor', ['vef%d' % b, 'mhalf'], ['rstdf%d' % b], rstd[b][:], ve5[b][:], mhalf[:, 0:1], ALU.pow)
                op('dve', 'scalar_tensor_tensor', ['mvf%d' % b, 'rstdf%d' % b], ['nbf%d' % b], out=nb[b][:], in0=mv[b][:, 0:1],
                   scalar=-1.0, in1=rstd[b][:], op0=ALU.mult, op1=ALU.mult)
                op('act', 'activation', [zfk, 'rstdf%d' % b, 'nbf%d' % b], [ofk], out=of[b][:], in_=zf[b][:], func=AF.Identity,
                   bias=nb[b][:], scale=rstd[b][:])
                op('pool', 'tensor_tensor', [ofk, 'lnB'], [ofk], of[b][:], of[b][:], lnB[:, 2, :], ALU.mult)
                op('dve', 'tensor_tensor', [ofk, 'lnB'], [ofk], of[b][:], of[b][:], lnB[:, 3, :], ALU.add)
                P.dma('sp', out_d[tt * 128:(tt + 1) * 128, :], of[b][:], reads=[ofk], writes=['out'])
            P.barrier()
    except _Stop:
        P.barrier()
    P.close()
    return nc, list(dbg.keys())


def _host_inputs(inp):
    f = lambda a: np.ascontiguousarray(np.asarray(a, dtype=np.float32))
    x = f(inp['x']); c = f(inp['c'])
    per_part = lambda v: np.ascontiguousarray(v.reshape(-1, 128).T)
    shared = {
        'w_ada': f(inp['w_ada'][0]),
        'b_ada': per_part(f(inp['b_ada'][0])),
        'b_ada_row': np.ascontiguousarray(f(inp['b_ada'][0])[None, :]),
        'w_in': f(inp['w_in'][0]),
        'convw': np.ascontiguousarray(f(inp['conv_w'][0]).T.reshape(4, 128, 4).transpose(1, 0, 2).reshape(128, 16)),
        'convb': per_part(f(inp['conv_b'][0])),
        'bga': per_part(f(inp['b_gate_a'][0]).reshape(-1)),
        'bgx': per_part(f(inp['b_gate_x'][0]).reshape(-1)),
        'lam': per_part(f(inp['lru_lambda'][0])),
        'relb': f(inp['rel_bias']),
        'glru': per_part(f(inp['norm_lru_g'][0])),
        'gattn': per_part(f(inp['norm_attn_g'][0])),
        'w_out': f(inp['w_out'][0]),
        'lnv': np.ascontiguousarray(np.stack([f(inp['ln1_g'][0]), f(inp['ln1_b'][0]), f(inp['ln2_g'][0]), f(inp['ln2_b'][0])])),
        'w_r': np.ascontiguousarray(np.concatenate([f(inp['w_router_group'][0]), f(inp['w_router_expert'][0])], axis=1)),
        'b_r': np.ascontiguousarray(np.concatenate([f(inp['b_router_group'][0]), f(inp['b_router_expert'][0])])[None, :]),
        'w1': f(inp['w1'][0]), 'w3': f(inp['w3'][0]), 'w2': f(inp['w2'][0]),
    }
    for nm, src in (('WA', 'w_gate_a'), ('WX', 'w_gate_x')):
        w = f(inp[src][0])
        bd = np.zeros((4, 128, 128), np.float32)
        for j in range(4):
            for s in range(2):
                bd[j, s * 64:(s + 1) * 64, s * 64:(s + 1) * 64] = w[2 * j + s]
        shared[nm] = bd
    i = np.arange(384)
    r = 255 - i
    bk = _t5_bucket_np(r)
    Roh = np.zeros((32, 384), np.float32)
    valid = (r >= 0) & (i < 383)
    Roh[bk[valid], i[valid]] = 1.0
    NEGr = np.tile(np.where(r < 0, NEGM, 0.0).astype(np.float32)[None, :], (8, 1))
    IND = np.zeros((16, CT), np.float32)
    for n in range(16):
        IND[n, n * BLK:(n + 1) * BLK] = 1.0
    shared['Roh'] = Roh; shared['NEGr'] = np.ascontiguousarray(NEGr); shared['IND'] = IND
    maps = []
    for core in range(8):
        b, half = core // 2, core % 2
        m = dict(shared)
        if half == 1:
            m['xctx'] = np.ascontiguousarray(x[b])
        else:
            m['xctx'] = np.ascontiguousarray(np.concatenate([np.zeros((2048, D), np.float32), x[b, :2048]], axis=0))
        m['cvec'] = per_part(c[b])
        gm = np.zeros((16, 16), np.float32); oh = np.zeros((16, 16), np.float32)
        for qt in range(16):
            own = 8 + qt // 2
            for n in range(16):
                ok = (n < own) and (half == 1 or n >= 8)
                gm[qt, n] = 0.0 if ok else -1e30
            oh[qt, own] = 1.0
        m['gmask'] = np.ascontiguousarray(np.tile(gm.reshape(1, 256), (128, 1)))
        m['ownhot'] = np.ascontiguousarray(np.tile(oh.reshape(1, 256), (128, 1)))
        m['flag'] = np.full((128, 1), float(half), np.float32)
        maps.append(m)
    return maps


_NC_CACHE = {}


def kernel(**inputs):
    if 'nc' not in _NC_CACHE:
        _NC_CACHE['nc'] = build_program()
    nc, dbgnames = _NC_CACHE['nc']
    maps = _host_inputs(inputs)
    if STOP < 5:
        for m in maps:
            for k in ('w1', 'w3', 'w2'):
                m.pop(k)
    res = run_bass_kernel_spmd(nc, maps, core_ids=list(range(8)))
    out = np.zeros((4, SEQ, D), np.float32)
    for core in range(8):
        b, half = core // 2, core % 2
        out[b, half * 2048:(half + 1) * 2048] = res.results[core]['out']
    if DBG:
        kernel.dbg = [{k: res.results[core][k] for k in dbgnames} for core in range(8)]
    return out
```

```python
import contextlib
import math
import numpy as np
import ml_dtypes
import concourse.bass as bass
import concourse.mybir as mybir
from concourse.bass_utils import run_bass_kernel_spmd

F32 = mybir.dt.float32
BF16 = mybir.dt.bfloat16
AF = mybir.ActivationFunctionType
ALU = mybir.AluOpType
AX = mybir.AxisListType

D = 1024
SEQ = 4096
CT = 4096
OWN0 = 2048
NOWN = 2048
NH = 8
HD = 64
BLK = 256
NBLK = 16
NE = 32
DE = 256
EPS = 1e-5
ALPHA = 2.0 ** 0.25
NEGM = -30000.0
DBG = False
import os
STOP = int(os.environ.get('MK_STOP', '9'))
NCH = int(os.environ.get('MK_NCH', '8'))
SKIP = set(os.environ.get('MK_SKIP', '').split(','))
SCHED_PH = set(os.environ.get('MK_SCHED_PH', '0,1,2,3,4,5').split(','))


class _Stop(Exception):
    pass


class Prog:
    def __init__(self, nc, n_dma_sems=24):
        self.nc = nc
        self.E = {'pe': nc.tensor, 'act': nc.scalar, 'dve': nc.vector, 'pool': nc.gpsimd, 'sp': nc.sync}
        self.sem = {}
        self.cnt = {}
        self._ctx = []
        for e in ['pe', 'act', 'dve', 'pool']:
            g = nc.semaphore('s_' + e)
            self.sem[e] = g.__enter__()
            self._ctx.append(g)
            self.cnt[e] = 0
        self.dsem = []
        for i in range(n_dma_sems):
            g = nc.semaphore('d_%d' % i)
            self.dsem.append(g.__enter__())
            self._ctx.append(g)
        self.dcnt = [0] * n_dma_sems
        self.dnext = 0
        self.dnext_sw = 0
        self.waited = {}
        self.lastw = {}
        self.reads = {}
        self.dirty = {e: False for e in self.cnt}
        self.rec = []
        self.tnow = 0.0
        self.schedule = (os.environ.get('MK_SCHED', '1') == '1')

    def _semh(self, key):
        return self.sem[key] if isinstance(key, str) else self.dsem[key]

    def _wait(self, eng, ev, same_ok):
        key, val, src = ev
        if src == eng and same_ok:
            return
        if self.waited.get((eng, key), 0) >= val:
            return
        self.E[eng].wait_ge(self._semh(key), val)
        self.waited[(eng, key)] = val

    def _deps(self, eng, reads, writes, is_dma=False):
        for r in reads:
            ev = self.lastw.get(r)
            if ev is not None:
                self._wait(eng, ev, same_ok=(eng == 'pe' and not is_dma))
        for w in writes:
            ev = self.lastw.get(w)
            if ev is not None:
                self._wait(eng, ev, same_ok=(eng == 'pe' and not is_dma))
            for ev in self.reads.get(w, ()):
                self._wait(eng, ev, same_ok=(eng == 'pe' and not is_dma))

    def _record(self, ev, reads, writes):
        for r in reads:
            lst = self.reads.setdefault(r, [])
            lst.append(ev)
            if len(lst) > 16:
                best = {}
                for k, v, s in lst:
                    if k not in best or best[k][1] < v:
                        best[k] = (k, v, s)
                self.reads[r] = list(best.values())
        for w in writes:
            self.lastw[w] = ev
            self.reads[w] = []

    def op(self, eng, fn, reads=(), writes=(), sig=True, est=0.5):
        self.rec.append(dict(kind='op', eng=eng, fn=fn, reads=tuple(reads), writes=tuple(writes), sig=sig, est=est))

    def dma(self, q, out, in_, reads=(), writes=(), **kw):
        try:
            nbytes = out.nbytes()
        except Exception:
            nbytes = 65536
        self.rec.append(dict(kind='dma', eng=q, out=out, in_=in_, kw=kw, reads=tuple(reads), writes=tuple(writes),
                             sig=True, est=2.0 + nbytes / 150e3))

    def _emit_op(self, eng, fn, reads, writes, sig):
        self._deps(eng, reads, writes)
        ins = fn()
        if sig:
            self.cnt[eng] += 1
            ins.then_inc(self.sem[eng], 1)
            ev = (eng, self.cnt[eng], eng)
            self.dirty[eng] = False
        else:
            ev = (eng, self.cnt[eng] + 1, eng)
            self.dirty[eng] = True
        self._record(ev, reads, writes)
        return ins

    def _emit_dma(self, q, out, in_, reads, writes, kw):
        half = len(self.dsem) // 2
        if q == 'pool':
            i = self.dnext_sw
            self.dnext_sw = (self.dnext_sw + 1) % half
        else:
            i = half + self.dnext
            self.dnext = (self.dnext + 1) % (len(self.dsem) - half)
        if self.dcnt[i] > 0:
            self._wait(q, (i, self.dcnt[i], 'dma'), same_ok=False)
        self._deps(q, reads, writes, is_dma=True)
        ins = self.E[q].dma_start(out=out, in_=in_, **kw)
        self.dcnt[i] += 16
        ins.then_inc(self.dsem[i], 16)
        ev = (i, self.dcnt[i], 'dma')
        self._record(ev, reads, writes)
        return ev

    def flush(self):
        rec = self.rec
        self.rec = []
        if not rec:
            return
        nodes = []
        cur_pe = None
        for r in rec:
            if r['kind'] == 'op' and r['eng'] == 'pe':
                if cur_pe is None:
                    cur_pe = dict(eng='pe', items=[], est=0.0)
                    nodes.append(cur_pe)
                cur_pe['items'].append(r)
                cur_pe['est'] += r['est']
                if r['sig']:
                    cur_pe = None
            else:
                if cur_pe is not None:
                    cur_pe['items'][-1]['sig'] = True
                    cur_pe = None
                nodes.append(dict(eng=r['eng'], items=[r], est=r['est'], isdma=(r['kind'] == 'dma')))
        if cur_pe is not None:
            cur_pe['items'][-1]['sig'] = True
            cur_pe = None
        n = len(nodes)
        lastw = {}
        readers = {}
        preds = [set() for _ in range(n)]
        for i, nd in enumerate(nodes):
            R = set(); Wr = set()
            for it in nd['items']:
                R.update(it['reads']); Wr.update(it['writes'])
            for r_ in R:
                if r_ in lastw:
                    preds[i].add(lastw[r_])
            for w_ in Wr:
                if w_ in lastw:
                    preds[i].add(lastw[w_])
                for j in readers.get(w_, ()):
                    preds[i].add(j)
            for r_ in R:
                readers.setdefault(r_, []).append(i)
            for w_ in Wr:
                lastw[w_] = i
                readers[w_] = []
            preds[i].discard(i)
        if not self.schedule:
            order = list(range(n))
        else:
            engs = ['pe', 'act', 'dve', 'pool', 'sp']
            per = {e: [] for e in engs}
            for i, nd in enumerate(nodes):
                per[nd['eng']].append(i)
            ptr = {e: 0 for e in engs}
            done = [False] * n
            fin = [0.0] * n
            free = {e: self.tnow for e in engs}
            order = []
            WINDOW = int(os.environ.get("MK_WIN", "48"))
            remaining = n
            while remaining:
                best = None
                for e in engs:
                    lst = per[e]
                    p0 = ptr[e]
                    while p0 < len(lst) and done[lst[p0]]:
                        p0 += 1
                    ptr[e] = p0
                    cnt = 0
                    k = p0
                    while k < len(lst) and cnt < WINDOW:
                        i = lst[k]
                        k += 1
                        if done[i]:
                            continue
                        cnt += 1
                        ok = True
                        t = free[e]
                        for pj in preds[i]:
                            if not done[pj]:
                                ok = False
                                break
                            if fin[pj] > t:
                                t = fin[pj]
                        if not ok:
                            continue
                        key = (t + 0.002 * (cnt - 1), i)
                        if best is None or key < best[0]:
                            best = (key, e, i, t)
                        if t <= free[e] + 1e-9:
                            break
                assert best is not None, "scheduler deadlock"
                _, e, i, t = best
                nd = nodes[i]
                done[i] = True
                remaining -= 1
                if nd.get('isdma'):
                    free[e] = t + 0.15
                    fin[i] = t + nd['est']
                else:
                    free[e] = t + nd['est']
                    fin[i] = t + nd['est'] + 0.1
                order.append((t, i))
            order.sort()
            order = [i for _, i in order]
            self.tnow = max(max(free.values()), max(fin) if fin else 0.0)
        for i in order:
            for it in nodes[i]['items']:
                if it['kind'] == 'op':
                    self._emit_op(it['eng'], it['fn'], it['reads'], it['writes'], it['sig'])
                else:
                    self._emit_dma(it['eng'], it['out'], it['in_'], it['reads'], it['writes'], it['kw'])

    def barrier(self):
        self.flush()
        for e in self.cnt:
            assert not self.dirty[e], e
        for eng in ['pe', 'act', 'dve', 'pool', 'sp']:
            for e in self.cnt:
                if self.cnt[e] > 0:
                    self._wait(eng, (e, self.cnt[e], e), same_ok=False)
            for i in range(len(self.dsem)):
                if self.dcnt[i] > 0:
                    self._wait(eng, (i, self.dcnt[i], 'dma'), same_ok=False)
        self.lastw = {}
        self.reads = {}

    def close(self):
        self.flush()
        for g in reversed(self._ctx):
            g.__exit__(None, None, None)


def _t5_bucket_np(n):
    n = np.maximum(n, 0)
    max_exact = 16
    nf = np.maximum(n, 1).astype(np.float32)
    large = max_exact + (np.log(nf / np.float32(max_exact)) / np.float32(math.log(128 / 16))
                         * np.float32(32 - max_exact)).astype(np.int32)
    large = np.minimum(large, 31)
    return np.where(n < max_exact, n, large)


def build_program():
    nc = bass.Bass("TRN2", target_bir_lowering=False)

    def din(name, shape, dt=F32):
        return nc.dram_tensor(name, list(shape), dt, kind="ExternalInput").ap()

    xctx = din("xctx", [CT, D])
    cvec = din("cvec", [128, 8])
    w_ada = din("w_ada", [D, 6 * D])
    b_ada = din("b_ada", [128, 48])
    b_ada_row = din("b_ada_row", [1, 6 * D])
    w_in = din("w_in", [D, 2560])
    convw = din("convw", [128, 16])
    convb = din("convb", [128, 4])
    WA = din("WA", [4, 128, 128])
    WX = din("WX", [4, 128, 128])
    bga = din("bga", [128, 4])
    bgx = din("bgx", [128, 4])
    lam = din("lam", [128, 4])
    relb = din("relb", [32, 8])
    glru = din("glru", [128, 4])
    gattn = din("gattn", [128, 4])
    w_out = din("w_out", [D, D])
    lnv = din("lnv", [4, D])
    w_r = din("w_r", [D, 36])
    b_r = din("b_r", [1, 36])
    if STOP >= 5:
        w1 = din("w1", [NE, D, DE])
        w3 = din("w3", [NE, D, DE])
        w2 = din("w2", [NE, DE, D])
    gmask_d = din("gmask", [128, 256])
    ownhot_d = din("ownhot", [128, 256])
    flag_d = din("flag", [128, 1])
    Roh = din("Roh", [32, 384])
    NEGr = din("NEGr", [8, 384])
    IND = din("IND", [16, CT])
    out_d = nc.dram_tensor("out", [NOWN, D], F32, kind="ExternalOutput").ap()

    xlru_s = nc.dram_tensor("xlru_s", [4, 128, CT], F32, kind="Internal").ap()
    gy_s = nc.dram_tensor("gy_s", [4, 128, NOWN], BF16, kind="Internal").ap()
    x1_s = nc.dram_tensor("x1_s", [NOWN, D], F32, kind="Internal").ap()
    q_s = nc.dram_tensor("q_s", [4, 128, NOWN], BF16, kind="Internal").ap()
    lo_s = nc.dram_tensor("lo_s", [4, 128, NOWN], BF16, kind="Internal").ap()
    a_s = nc.dram_tensor("a_s", [4, 128, NOWN], BF16, kind="Internal").ap()
    G_s = nc.dram_tensor("G_s", [8, 384], F32, kind="Internal")
    dbg = {}

    def dout(name, shape, dt=F32):
        dbg[name] = nc.dram_tensor(name, list(shape), dt, kind="ExternalOutput").ap()
        return dbg[name]

    P = Prog(nc)
    try:
      with contextlib.ExitStack() as top:
        _nm = [0]

        def sb(st, name, shape, dt):
            _nm[0] += 1
            return st.enter_context(nc.sbuf_tensor("%s_u%d" % (name, _nm[0]), list(shape), dt))

        def ps(st, name, shape, dt):
            return st.enter_context(nc.psum_tensor(name, list(shape), dt))

        E = P.E

        def _est(eng, a, kw):
            o = kw.get('out', a[0] if a else None)
            try:
                nfree = 1
                for d_ in o.shape[1:]:
                    nfree *= d_
            except Exception:
                nfree = 512
            if eng == 'pe':
                mv_ = kw.get('rhs', a[2] if len(a) > 2 else None)
                try:
                    nm = 1
                    for d_ in mv_.shape[1:]:
                        nm *= d_
                except Exception:
                    nm = 128
                return 0.06 + max(nm, 64) / 2000.0
            if eng == 'act':
                return 0.22 + nfree / 1100.0
            if eng == 'dve':
                return 0.10 + nfree / 900.0
            return 0.15 + nfree / 440.0

        def op(eng, meth, reads, writes, *args, sig=True, **kw):
            return P.op(eng, lambda: getattr(E[eng], meth)(*args, **kw), reads, writes, sig, est=_est(eng, args, kw))

        def ARGS(*a, **kw):
            return (a, kw)

        def pe_raw(meth, reads, writes, argskw=None, sig=True):
            a, kw = argskw
            return P.op('pe', lambda: getattr(nc.tensor, meth)(*a, **kw), reads, writes, sig, est=_est('pe', a, kw))

        def mm(out, lhsT, rhs, reads, writes, start, stop):
            return P.op('pe', lambda: nc.tensor.matmul(out, lhsT, rhs, start=start, stop=stop), reads, writes, sig=stop,
                        est=_est('pe', (out, lhsT, rhs), {}))

        pT0 = ps(top, "pT0", [128, 1024], BF16)
        pT1 = ps(top, "pT1", [128, 1024], BF16)
        pA = ps(top, "pA", [128, 512], F32)
        pB = ps(top, "pB", [128, 512], F32)
        pS0 = ps(top, "pS0", [128, 512], F32)
        pS1 = ps(top, "pS1", [128, 512], F32)
        pO = ps(top, "pO", [128, 512], F32)
        pM = ps(top, "pM", [128, 512], F32)

        ident = sb(top, "ident", [128, 128], BF16)
        id32 = sb(top, "id32", [128, 128], F32)
        ones32 = sb(top, "ones32", [128, 128], F32)
        onesb = sb(top, "onesb", [128, 1], BF16)
        modT = sb(top, "modT", [128, 48], F32)
        sc1p = sb(top, "sc1p", [128, 8], F32)
        sc2p = sb(top, "sc2p", [128, 8], F32)
        g1B = sb(top, "g1B", [128, D], F32)
        g2B = sb(top, "g2B", [128, D], F32)
        mhalf = sb(top, "mhalf", [128, 16], F32)
        ve = sb(top, "ve", [128, 1], F32)

        op('pool', 'memset', [], ['id32'], id32[:], 1.0)
        op('pool', 'affine_select', ['id32'], ['id32'], out=id32[:], in_=id32[:], pattern=[[-1, 128]],
           compare_op=ALU.is_equal, fill=0.0, base=0, channel_multiplier=1)
        op('dve', 'tensor_copy', ['id32'], ['ident'], ident[:], id32[:])
        op('pool', 'memset', [], ['ones32'], ones32[:], 1.0)
        op('dve', 'memset', [], ['onesb'], onesb[:], 1.0)
        op('dve', 'memset', [], ['mhalf'], mhalf[:], -0.5)

        P.flush()
        P.schedule = ('0' in SCHED_PH) and (os.environ.get('MK_SCHED', '1') == '1')
        with contextlib.ExitStack() as s0:
            csb = sb(s0, "csb", [128, 8], F32)
            modrow = sb(s0, "modrow", [1, 6 * D], F32)
            csil = sb(s0, "csil", [128, 8], BF16)
            bada = sb(s0, "bada", [128, 48], F32)
            wad = [sb(s0, "wad%d" % i, [128, 8, 512], BF16) for i in range(2)]
            P.dma('sp', csb[:], cvec, writes=['csb'])
            P.dma('sp', bada[:], b_ada, writes=['bada'])
            op('act', 'activation', ['csb'], ['csil'], out=csil[:], in_=csb[:], func=AF.Silu)
            for cc in range(12):
                w = wad[cc % 2]
                wk = 'wad%d' % (cc % 2)
                P.dma('pool', w[:], w_ada[:, cc * 512:(cc + 1) * 512].rearrange("(k p) n -> p k n", p=128),
                      writes=[wk])
                pp = pA if cc % 2 == 0 else pB
                pk = 'pA' if cc % 2 == 0 else 'pB'
                for k in range(8):
                    mm(pp[0:1, :], csil[:, k:k + 1], w[:, k, :], ['csil', wk], [pk], k == 0, k == 7)
                op('act', 'copy', [pk], ['modrow'], modrow[0:1, cc * 512:(cc + 1) * 512], pp[0:1, :])
            for ct in range(48):
                pe_raw('matmul', ['modrow', 'ones32'], ['pM'], sig=(ct == 47), argskw=ARGS(pM[:, ct:ct + 1], modrow[0:1, ct * 128:(ct + 1) * 128],
                                                    ones32[0:1, 0:1], start=True, stop=True))
            op('dve', 'tensor_tensor', ['pM', 'bada'], ['modT'], modT[:], pM[:, 0:48], bada[:], ALU.add)
            op('dve', 'tensor_scalar_add', ['modT'], ['sc1p'], sc1p[:], modT[:, 8:16], 1.0)
            op('dve', 'tensor_scalar_add', ['modT'], ['sc2p'], sc2p[:], modT[:, 32:40], 1.0)
            for (dst, dk, off) in ((g1B, 'g1B', 2 * D), (g2B, 'g2B', 5 * D)):
                for hf in range(2):
                    pe_raw('matmul', ['modrow', 'ones32'], ['pA'], argskw=ARGS(pA[:, :], ones32[0:1, :],
                                                        modrow[0:1, off + hf * 512: off + (hf + 1) * 512],
                                                        start=True, stop=True))
                    op('act', 'copy', ['pA'], [dk], dst[:, hf * 512:(hf + 1) * 512], pA[:, :])
            badarow = sb(s0, "badarow", [128, 2, D], F32)
            P.dma('sp', badarow[:, 0, :], b_ada_row[0:1, 2 * D:3 * D].rearrange("a n -> (a n)").partition_broadcast(128),
                  writes=['badarow'])
            P.dma('sp', badarow[:, 1, :], b_ada_row[0:1, 5 * D:6 * D].rearrange("a n -> (a n)").partition_broadcast(128),
                  writes=['badarow'])
            op('pool', 'tensor_tensor', ['g1B', 'badarow'], ['g1B'], g1B[:], g1B[:], badarow[:, 0, :], ALU.add)
            op('pool', 'tensor_tensor', ['g2B', 'badarow'], ['g2B'], g2B[:], g2B[:], badarow[:, 1, :], ALU.add)
            if DBG:
                P.dma('sp', dout("d_modT", [128, 48]), modT[:], reads=['modT'])
                P.dma('sp', dout("d_g1B", [128, D]), g1B[:], reads=['g1B'])
            P.barrier()
            if STOP == 0:
                raise _Stop()

        ssl = sb(top, "ssl", [128, 16], F32)
        ssa = sb(top, "ssa", [128, 16], F32)
        op('dve', 'memset', [], ['ssl'], ssl[:], 0.0)
        op('dve', 'memset', [], ['ssa'], ssa[:], 0.0)
        with contextlib.ExitStack() as s13:
            kT = [sb(s13, "kT%d" % h, [96, CT], BF16) for h in range(NH)]
            Vt = sb(s13, "Vt", [128, 32, NH * 65], BF16)
            op('pool', 'memset', [], ['Vt'], Vt[:].rearrange("p t c -> p (t c)"), 1.0)
            for h in range(NH):
                op('dve', 'memset', [], ['kTind%d' % h], kT[h][64:96, :], 0.0)
                P.dma('pool', kT[h][64:80, :], IND, writes=['kTind%d' % h])

            P.flush()
            P.schedule = ('1' in SCHED_PH) and (os.environ.get('MK_SCHED', '1') == '1')
            with contextlib.ExitStack() as s1:
                winb = sb(s1, "winb", [128, 8, 2560], BF16)
                for k in range(8):
                    P.dma('pool', winb[:, k, :], w_in[k * 128:(k + 1) * 128, :], writes=['winb%d' % k])
                xt = [sb(s1, "xt%d" % i, [128, D], F32) for i in range(2)]
                xn = [sb(s1, "xn%d" % i, [128, D], BF16) for i in range(8)]
                uTs = [sb(s1, "uT%d" % i, [128, 8, 512], BF16) for i in range(2)]
                st6 = [sb(s1, "st6_%d" % i, [128, 2, 6], F32) for i in range(4)]
                mv = [sb(s1, "mv_%d" % i, [128, 2], F32) for i in range(4)]
                rstd = [sb(s1, "rstd_%d" % i, [128, 1], F32) for i in range(4)]
                nb = [sb(s1, "nb_%d" % i, [128, 1], F32) for i in range(4)]
                ve1 = [sb(s1, "ve_%d" % i, [128, 1], F32) for i in range(4)]
                stg = [sb(s1, "stg%d" % i, [128, 512], F32) for i in range(2)]
                ysb = sb(s1, "ysb", [128, 512], F32)
                yt = sb(s1, "yt", [128, 512], F32)
                ysg = sb(s1, "ysg", [128, 512], F32)
                gyb = [sb(s1, "gyb%d" % i, [128, 512], BF16) for i in range(2)]
                winr = ['winb%d' % k for k in range(8)]
                nstg = 0

                def emit_ln(c):
                    for t in range(4):
                        T = 4 * c + t
                        xb = xt[T % 2]
                        xk = 'xt%d' % (T % 2)
                        i = T % 4
                        xi = (c % 2) * 4 + t
                        P.dma('sp', xb[:], xctx[T * 128:(T + 1) * 128, :], writes=[xk])
                        for hf in range(2):
                            op('dve', 'bn_stats', [xk], ['st6_%d' % i], out=st6[i][:, hf, :], in_=xb[:, hf * 512:(hf + 1) * 512])
                        op('dve', 'bn_aggr', ['st6_%d' % i], ['mv_%d' % i], out=mv[i][:], in_=st6[i][:].rearrange("p a b -> p (a b)"))
                        op('dve', 'tensor_scalar_add', ['mv_%d' % i], ['ve_%d' % i], ve1[i][:], mv[i][:, 1:2], EPS)
                        op('pool', 'tensor_tensor', ['ve_%d' % i, 'mhalf'], ['rstd_%d' % i], rstd[i][:], ve1[i][:], mhalf[:, 0:1], ALU.pow)
                        op('dve', 'scalar_tensor_tensor', ['mv_%d' % i, 'rstd_%d' % i], ['nb_%d' % i], out=nb[i][:], in0=mv[i][:, 0:1],
                           scalar=-1.0, in1=rstd[i][:], op0=ALU.mult, op1=ALU.mult)
                        op('act', 'activation', [xk, 'rstd_%d' % i, 'nb_%d' % i], ['xn%d' % xi], out=xn[xi][:], in_=xb[:],
                           func=AF.Identity, bias=nb[i][:], scale=rstd[i][:])

                def emit_tr(c, r):
                    uT = uTs[c % 2]
                    pt = pT0 if r % 2 == 0 else pT1
                    ptk = 'pT0' if r % 2 == 0 else 'pT1'
                    for kk in range(2):
                        k = 2 * r + kk
                        for t in range(4):
                            xi = (c % 2) * 4 + t
                            pe_raw('transpose', ['xn%d' % xi, 'ident'], [ptk], sig=(kk == 1 and t == 3), argskw=ARGS(
                                pt[:, kk * 512 + t * 128: kk * 512 + (t + 1) * 128],
                                xn[xi][:, k * 128:(k + 1) * 128], ident[:]))
                    for kk in range(2):
                        k = 2 * r + kk
                        uk = 'uT%d_%d' % (c % 2, k)
                        if r % 2 == 0:
                            op('act', 'activation', [ptk, 'sc1p', 'modT'], [uk], out=uT[:, k, :],
                               in_=pt[:, kk * 512:(kk + 1) * 512], func=AF.Identity,
                               bias=modT[:, k:k + 1], scale=sc1p[:, k:k + 1])
                        else:
                            op('dve', 'tensor_scalar', [ptk, 'sc1p', 'modT'], [uk], uT[:, k, :],
                               pt[:, kk * 512:(kk + 1) * 512], sc1p[:, k:k + 1], modT[:, k:k + 1],
                               ALU.mult, ALU.add)

                if NCH > 0:
                    emit_ln(0)
                    for r in range(4):
                        emit_tr(0, r)
                for c in range(NCH):
                    own = c >= 4
                    uT = uTs[c % 2]
                    if c + 1 < NCH:
                        emit_ln(c + 1)
                    pend_tr = list(range(4)) if c + 1 < NCH else []
                    uTr = ['uT%d_%d' % (c % 2, k) for k in range(8)]
                    cts = list(range(0, 4)) + (list(range(4, 12)) if own else []) + list(range(12, 16))
                    if 'fm' in SKIP:
                        cts = []
                    every = max(1, len(cts) // 4)
                    for ci, ct in enumerate(cts):
                        if pend_tr and ci > 0 and ci % every == 0:
                            emit_tr(c + 1, pend_tr.pop(0))
                        pp, pk = [(pA, 'pA'), (pB, 'pB'), (pO, 'pO'), (pM, 'pM')][ci % 4]
                        for k in range(8):
                            mm(pp[:, :], winb[:, k, ct * 128:(ct + 1) * 128], uT[:, k, :],
                               [winr[k], uTr[k]], [pk], k == 0, k == 7)
                        j = ct % 4
                        if ct < 4:
                            sg = stg[nstg % 2]
                            sk = 'stg%d' % (nstg % 2)
                            nstg += 1
                            op('act', 'copy', [pk], [sk], sg[:], pp[:, :])
                            P.dma('sp', xlru_s[j, :, c * 512:(c + 1) * 512], sg[:], reads=[sk], writes=['xlru_s'])
                        elif ct < 8:
                            gb = gyb[j % 2]
                            gk = 'gyb%d' % (j % 2)
                            op('act', 'copy', [pk], ['ysb'], ysb[:], pp[:, :])
                            op('pool', 'tensor_tensor', ['ysb'], ['yt'], yt[:], ysb[:], ysb[:], ALU.mult)
                            op('pool', 'tensor_scalar', ['yt'], ['yt'], yt[:], yt[:], 0.044715, 1.0, ALU.mult, ALU.add)
                            op('pool', 'tensor_tensor', ['yt', 'ysb'], ['yt'], yt[:], yt[:], ysb[:], ALU.mult)
                            op('act', 'activation', ['yt'], ['ysg'], out=ysg[:], in_=yt[:], func=AF.Sigmoid,
                               scale=1.5957691216057308)
                            op('pool', 'tensor_tensor', ['ysg', 'ysb'], [gk], gb[:], ysg[:], ysb[:], ALU.mult)
                            P.dma('sp', gy_s[j, :, (c - 4) * 512:(c - 3) * 512], gb[:], reads=[gk], writes=['gy_s'])
                        elif ct < 12:
                            oc = (c - 4) * 512
                            gb = gyb[j % 2]
                            gk = 'gyb%d' % (j % 2)
                            op('act', 'copy', [pk], [gk], gb[:], pp[:, :])
                            P.dma('sp', q_s[j, :, oc:oc + 512], gb[:], reads=[gk], writes=['q_s'])
                        else:
                            ke, km = ('act', 'copy') if j % 2 == 0 else ('dve', 'tensor_copy')
                            op(ke, km, [pk], ['kT%d' % (2 * j)], kT[2 * j][0:64, c * 512:(c + 1) * 512], pp[0:64, :])
                            op(ke, km, [pk], ['kT%d' % (2 * j + 1)],
                               kT[2 * j + 1][0:64, c * 512:(c + 1) * 512], pp[64:128, :])
                    for t in range(0 if 'v' in SKIP else 4):
                        if pend_tr:
                            emit_tr(c + 1, pend_tr.pop(0))
                        T = 4 * c + t
                        pp, pk = (pS0, 'pS0') if t % 2 == 0 else (pS1, 'pS1')
                        for k in range(8):
                            mm(pp[:, :], uT[:, k, t * 128:(t + 1) * 128], winb[:, k, 2048:2560],
                               [winr[k], uTr[k]], [pk], k == 0, k == 7)
                        op('dve' if t % 2 == 0 else 'act', 'tensor_copy' if t % 2 == 0 else 'copy', [pk], ['Vt'],
                           Vt[:, T, :].rearrange("p (h e) -> p h e", e=65)[:, :, 0:64],
                           pp[:, :].rearrange("p (h d) -> p h d", d=64))
                    while pend_tr:
                        emit_tr(c + 1, pend_tr.pop(0))
                if DBG:
                    for h in (0, 1, 7):
                        P.dma('sp', dout("d_kT%d" % h, [80, CT], BF16), kT[h][0:80, :], reads=['kT%d' % h, 'kTind%d' % h])
                    P.dma('sp', dout("d_V", [128, 32 * 520], BF16), Vt[:].rearrange("p t c -> p (t c)"), reads=['Vt'])
                P.barrier()
                if STOP == 1:
                    raise _Stop()

            P.flush()
            P.schedule = ('3' in SCHED_PH) and (os.environ.get('MK_SCHED', '1') == '1')
            with contextlib.ExitStack() as s3:
                qT = [sb(s3, "qT%d" % h, [96, NOWN], BF16) for h in range(NH)]
                for h in range(NH):
                    P.dma('sp', qT[h][0:64, :], q_s[h // 2, (h % 2) * 64:(h % 2) * 64 + 64, :], reads=['q_s'], writes=['qT%d' % h])
                    op('dve', 'memset', [], ['qTm%d' % h], qT[h][64:96, :], 0.0)
                apair = sb(s3, "apair", [128, 512], BF16)
                gmask = sb(s3, "gmask_t", [128, 16, 16], F32)
                ownhot = sb(s3, "ownhot_t", [128, 16, 16], F32)
                P.dma('sp', gmask[:].rearrange("p a b -> p (a b)"), gmask_d, writes=['gmask'])
                P.dma('sp', ownhot[:].rearrange("p a b -> p (a b)"), ownhot_d, writes=['ownhot'])
                cfar = sb(s3, "cfar", [128, 8], F32)
                biasD = sb(s3, "biasD", [128, 8, 128], F32)
                biasS = sb(s3, "biasS", [128, 8, 128], F32)
                kmf = sb(s3, "kmf", [64, NH, 16], F32)
                kmb = sb(s3, "kmb", [64, NH, 16], BF16)
                s3a = contextlib.ExitStack()
                s3a.__enter__()
                relsb = sb(s3a, "relsb", [32, 8], F32)
                Rsb = sb(s3a, "Rsb", [32, 384], F32)
                Gsb = sb(s3a, "Gsb", [8, 384], F32)
                negsb = sb(s3a, "negsb", [8, 384], F32)
                P.dma('sp', relsb[:], relb, writes=['relsb'])
                P.dma('sp', Rsb[:], Roh, writes=['Rsb'])
                P.dma('sp', negsb[:], NEGr, writes=['negsb'])
                P.dma('sp', cfar[:], relb[31:32, :].rearrange("a h -> (a h)").partition_broadcast(128), writes=['cfar'])
                mm(pA[0:8, 0:384], relsb[:, :], Rsb[:, :], ['relsb', 'Rsb'], ['pA'], True, True)
                op('dve', 'tensor_tensor', ['pA', 'negsb'], ['Gsb'], Gsb[:], pA[0:8, 0:384], negsb[:], ALU.add)
                P.dma('sp', G_s.ap(), Gsb[:], reads=['Gsb'], writes=['G_s'])
                hank = sb(s3a, "hank", [128, 16, 128], F32)
                for h in range(NH):
                    P.dma('sp', hank[:, h, :], bass.AP(G_s, h * 384 + 128, [[1, 128], [1, 128]]),
                          reads=['G_s'], writes=['hank'])
                    P.dma('sp', hank[:, 8 + h, :], bass.AP(G_s, h * 384, [[1, 128], [1, 128]]),
                          reads=['G_s'], writes=['hank'])
                for h in range(NH):
                    op('pool', 'tensor_copy', ['hank'], ['biasD'], biasD[:, h, :], hank[:, h, ::-1])
                    op('pool', 'tensor_copy', ['hank'], ['biasS'], biasS[:, h, :], hank[:, 8 + h, ::-1])
                for h in range(NH):
                    op('dve', 'tensor_reduce', ['kT%d' % h], ['kmf'], out=kmf[:, h, :],
                       in_=kT[h][0:64, :].rearrange("p (n b) -> p n b", b=BLK), axis=AX.X, op=ALU.add)
                op('dve', 'tensor_scalar_mul', ['kmf'], ['kmb'], kmb[:].rearrange("p h n -> p (h n)"),
                   kmf[:].rearrange("p h n -> p (h n)"), 1.0 / BLK)
                _sch3 = P.schedule
                s3a.__exit__(None, None, None)
                P.barrier()
                P.schedule = _sch3
                pT0f = pT0[:].bitcast(F32)
                pT1f = pT1[:].bitcast(F32)
                cw = sb(s3, "cw", [128, 16], F32)
                cb = sb(s3, "cb", [128, 4], F32)
                bA = sb(s3, "bA", [128, 4], F32)
                bX = sb(s3, "bX", [128, 4], F32)
                lamt = sb(s3, "lamt", [128, 4], F32)
                cL = sb(s3, "cL", [128, 4], F32)
                cL2 = sb(s3, "cL2", [128, 4], F32)
                flag = sb(s3, "flag_t", [128, 1], F32)
                carry = sb(s3, "carry", [128, 4], F32)
                WAb = sb(s3, "WAb", [128, 4, 128], BF16)
                WXb = sb(s3, "WXb", [128, 4, 128], BF16)
                P.dma('sp', cw[:], convw, writes=['cw'])
                P.dma('sp', cb[:], convb, writes=['cb'])
                P.dma('sp', bA[:], bga, writes=['bA'])
                P.dma('sp', bX[:], bgx, writes=['bX'])
                P.dma('sp', lamt[:], lam, writes=['lamt'])
                P.dma('sp', flag[:], flag_d, writes=['flag'])
                P.dma('pool', WAb[:], WA.rearrange("j p o -> p j o"), writes=['WAb'])
                P.dma('pool', WXb[:], WX.rearrange("j p o -> p j o"), writes=['WXb'])
                op('act', 'activation', ['lamt'], ['cL'], out=cL[:], in_=lamt[:], func=AF.Exp, scale=-1.0)
                op('act', 'activation', ['cL'], ['cL'], out=cL[:], in_=cL[:], func=AF.Ln, bias=1.0)
                op('dve', 'tensor_scalar_mul', ['cL'], ['cL2'], cL2[:], cL[:], -16.0)
                op('dve', 'tensor_scalar_mul', ['cL'], ['cL'], cL[:], cL[:], -8.0)
                op('dve', 'memset', [], ['carry'], carry[:], 0.0)
                nbA = sb(s3, "nbA", [128, 4], F32)
                nbX = sb(s3, "nbX", [128, 4], F32)
                op('dve', 'tensor_scalar_mul', ['bA'], ['nbA'], nbA[:], bA[:], -1.0)
                op('dve', 'tensor_scalar_mul', ['bX'], ['nbX'], nbX[:], bX[:], -1.0)
                W = 512
                NB2 = 2
                def mk(name, shape, dt):
                    return [sb(s3, "%s_%d" % (name, i), shape, dt) for i in range(NB2)]
                xl = mk("xl", [128, 3 + W], F32)
                xc = mk("xc", [128, W], F32)
                xcb = mk("xcb", [128, W], BF16)
                rr = mk("rr", [128, W], F32)
                ii = mk("ii", [128, W], F32)
                aa = mk("aa", [128, W], F32)
                m2 = mk("m2", [128, W], F32)
                hh = mk("hh", [128, W], F32)
                gyl = mk("gyl", [128, W], BF16)
                sq = mk("sq", [128, W], BF16)
                lop = mk("lop", [128, W], BF16)
                npc_box = [0]

                def lru_piece(j, pc):
                    b = npc_box[0] % NB2
                    npc_box[0] += 1
                    K_ = lambda n: '%s_%d' % (n, b)
                    if pc == 0:
                        op('dve', 'memset', [], [K_('xl')], xl[b][:, 0:3], 0.0)
                        P.dma('sp', xl[b][:, 3:3 + W], xlru_s[j, :, 0:W], reads=['xlru_s'], writes=[K_('xl')])
                    else:
                        P.dma('sp', xl[b][:, :], xlru_s[j, :, pc * W - 3:(pc + 1) * W], reads=['xlru_s'], writes=[K_('xl')])
                    if pc >= 4:
                        oc = (pc - 4) * W
                        P.dma('sp', gyl[b][:], gy_s[j, :, oc:oc + W], reads=['gy_s'], writes=[K_('gyl')])
                    op('dve', 'tensor_scalar', [K_('xl'), 'cw', 'cb'], [K_('xc')], xc[b][:], xl[b][:, 0:W],
                       cw[:, j * 4:j * 4 + 1], cb[:, j:j + 1], ALU.mult, ALU.add)
                    for k in range(1, 4):
                        op('dve', 'scalar_tensor_tensor', [K_('xl'), 'cw', K_('xc')], [K_('xc')], out=xc[b][:], in0=xl[b][:, k:k + W],
                           scalar=cw[:, j * 4 + k:j * 4 + k + 1], in1=xc[b][:], op0=ALU.mult, op1=ALU.add)
                    op('pool', 'tensor_copy', [K_('xc')], [K_('xcb')], xcb[b][:], xc[b][:])
                    mm(pT0f, WAb[:, j, :], xcb[b][:, :], ['WAb', K_('xcb')], ['pT0'], True, True)
                    op('act', 'activation', ['pT0', 'nbA'], [K_('rr')], out=rr[b][:, :], in_=pT0f,
                       func=AF.Exp, bias=nbA[:, j:j + 1], scale=-1.0)
                    mm(pT0f, WXb[:, j, :], xcb[b][:, :], ['WXb', K_('xcb')], ['pT0'], True, True)
                    op('act', 'activation', ['pT0', 'nbX'], [K_('ii')], out=ii[b][:, :], in_=pT0f,
                       func=AF.Exp, bias=nbX[:, j:j + 1], scale=-1.0)
                    op('act', 'activation', [K_('rr')], [K_('rr')], out=rr[b][:], in_=rr[b][:], func=AF.Ln, bias=1.0)
                    op('act', 'activation', [K_('rr')], [K_('rr')], out=rr[b][:], in_=rr[b][:], func=AF.Exp, scale=-1.0)
                    op('act', 'activation', [K_('ii')], [K_('ii')], out=ii[b][:], in_=ii[b][:], func=AF.Ln, bias=1.0)
                    op('act', 'activation', [K_('ii')], [K_('ii')], out=ii[b][:], in_=ii[b][:], func=AF.Exp, scale=-1.0)
                    op('act', 'activation', [K_('rr'), 'cL'], [K_('aa')], out=aa[b][:], in_=rr[b][:], func=AF.Exp, scale=cL[:, j:j + 1])
                    op('act', 'activation', [K_('rr'), 'cL2'], [K_('m2')], out=m2[b][:], in_=rr[b][:], func=AF.Exp, scale=cL2[:, j:j + 1])
                    op('act', 'activation', [K_('m2')], [K_('m2')], out=m2[b][:], in_=m2[b][:], func=AF.Ln, scale=-1.0, bias=1.0)
                    op('act', 'activation', [K_('m2')], [K_('m2')], out=m2[b][:], in_=m2[b][:], func=AF.Exp, scale=0.5)
                    op('pool', 'tensor_tensor', [K_('ii'), K_('xc')], [K_('ii')], ii[b][:], ii[b][:], xc[b][:], ALU.mult)
                    op('pool', 'tensor_tensor', [K_('ii'), K_('m2')], [K_('ii')], ii[b][:], ii[b][:], m2[b][:], ALU.mult)
                    if pc == 4:
                        op('dve', 'tensor_tensor', ['carry', 'flag'], ['carry'], carry[:, j:j + 1], carry[:, j:j + 1],
                           flag[:], ALU.mult)
                    op('dve', 'tensor_tensor_scan', [K_('aa'), K_('ii'), 'carry'], [K_('hh')], out=hh[b][:], data0=aa[b][:], data1=ii[b][:],
                       initial=carry[:, j:j + 1], op0=ALU.mult, op1=ALU.add)
                    op('dve', 'tensor_copy', [K_('hh')], ['carry'], carry[:, j:j + 1], hh[b][:, W - 1:W])
                    if pc >= 4:
                        oc = (pc - 4) * W
                        op('pool', 'tensor_tensor', [K_('hh'), K_('gyl')], [K_('lop')], lop[b][:], hh[b][:], gyl[b][:], ALU.mult)
                        P.dma('sp', lo_s[j, :, oc:oc + W], lop[b][:], reads=[K_('lop')], writes=['lo_s'])
                        op('pool', 'tensor_tensor', [K_('lop')], [K_('sq')], sq[b][:], lop[b][:], lop[b][:], ALU.mult)
                        for t in range(4):
                            pe_raw('matmul', [K_('sq'), 'onesb'], ['pT1'], sig=(t == 3), argskw=ARGS(pT1f[:, t:t + 1], sq[b][:, t * 128:(t + 1) * 128], onesb[:, 0:1],
                                                                start=True, stop=True))
                        t0 = (pc - 4) * 4
                        op('dve', 'tensor_tensor', ['pT1', 'ssl'], ['ssl'], ssl[:, t0:t0 + 4], ssl[:, t0:t0 + 4], pT1f[:, 0:4], ALU.add)
                lru_list = [(j, pc) for j in range(4) for pc in range(8)]
                gsb = sb(s3, "gsb", [128, NH, 16], F32)
                top8 = sb(s3, "top8", [128, NH, 8], F32)
                sel = sb(s3, "sel", [128, NH, 16], F32)
                mvb = sb(s3, "mvb", [128, NH, 16], BF16)
                for qt in range(16):
                    if qt % 2 == 0 and lru_list:
                        lru_piece(*lru_list.pop(0))
                    for h in range(NH):
                        pe_raw('matmul', ['qT%d' % h, 'kmb'], ['pM'], sig=(h == NH - 1), argskw=ARGS(pM[:, h * 16:(h + 1) * 16], qT[h][0:64, qt * 128:(qt + 1) * 128],
                                                            kmb[:, h, :], start=True, stop=True))
                    op('dve', 'tensor_tensor', ['pM', 'gmask'], ['gsb'], gsb[:],
                       pM[:, 0:128].rearrange("p (h n) -> p h n", n=16),
                       gmask[:, qt:qt + 1, :].to_broadcast([128, NH, 16]), ALU.add)
                    for h in range(NH):
                        op('dve', 'max', ['gsb'], ['top8'], out=top8[:, h, :], in_=gsb[:, h, :])
                    op('dve', 'tensor_tensor', ['gsb', 'top8'], ['sel'], sel[:], gsb[:],
                       top8[:, :, 2:3].to_broadcast([128, NH, 16]), ALU.is_ge)
                    op('dve', 'scalar_tensor_tensor', ['gsb', 'sel'], ['sel'], out=sel[:], in0=gsb[:], scalar=-1e29,
                       in1=sel[:], op0=ALU.is_gt, op1=ALU.mult)
                    op('dve', 'tensor_tensor', ['sel', 'ownhot'], ['sel'], sel[:], sel[:],
                       ownhot[:, qt:qt + 1, :].to_broadcast([128, NH, 16]), ALU.add)
                    op('dve', 'tensor_scalar', ['sel'], ['mvb'], mvb[:], sel[:], -1.0, -NEGM, ALU.add, ALU.mult)
                    for h in range(NH):
                        pe_raw('transpose', ['mvb', 'ident'], ['pT1'], sig=(h == NH - 1), argskw=ARGS(pT1[0:16, h * 128:(h + 1) * 128], mvb[:, h, :], ident[:]))
                    for h in range(NH):
                        op('act' if qt % 2 == 0 else 'dve', 'copy' if qt % 2 == 0 else 'tensor_copy', ['pT1'],
                           ['qTm%d' % h], qT[h][64:80, qt * 128:(qt + 1) * 128], pT1[0:16, h * 128:(h + 1) * 128])
                if DBG:
                    for h in (0, 1, 7):
                        P.dma('sp', dout("d_qm%d" % h, [16, NOWN], BF16), qT[h][64:80, :], reads=['qTm%d' % h])
                    P.dma('sp', dout("d_biasD", [128, 8 * 128]), biasD[:].rearrange("p h n -> p (h n)"), reads=['biasD'])
                    P.dma('sp', dout("d_biasS", [128, 8 * 128]), biasS[:].rearrange("p h n -> p (h n)"), reads=['biasS'])
                PT = [sb(s3, "PT%d" % i, [128, 512], BF16) for i in range(3)]
                tmpS = [sb(s3, "tmpS%d" % i, [128, 128], F32) for i in range(2)]
                osb = sb(s3, "osb", [65, 512], F32)
                sqa = sb(s3, "sqa", [128, 512], BF16)
                SCALE = HD ** -0.5
                npt = 0
                nts = 0
                nonlocal_nsb = [0]
                Sb = [(pS0, 'pS0'), (pS1, 'pS1'), (pA, 'pA')]
                Ob = [(pO, 'pO'), (pB, 'pB')]
                nob = 0
                for cq in range(4):
                    c = 4 + cq
                    for h in range(NH):
                        if lru_list:
                            lru_piece(*lru_list.pop(0))
                        j, s = h // 2, h % 2
                        qr = ['qT%d' % h, 'qTm%d' % h]
                        kr = ['kT%d' % h, 'kTind%d' % h]
                        nkt = 4 * c + 4
                        pOc, pOk = Ob[nob % 2]
                        nob += 1

                        def geom(kt):
                            qlo = max(kt, 4 * c)
                            n0 = (qlo - 4 * c) * 128
                            return qlo, n0

                        def issue_S(kt):
                            nonlocal_nsb[0] += 1
                            pS, pSk = Sb[nonlocal_nsb[0] % 3]
                            qlo, n0 = geom(kt)
                            mm(pS[:, n0:512], kT[h][0:96, kt * 128:(kt + 1) * 128],
                               qT[h][0:96, cq * 512 + n0: cq * 512 + 512], qr + kr, [pSk], True, True)
                            return pS, pSk

                        pendq = [issue_S(0)]
                        if nkt > 1:
                            pendq.append(issue_S(1))
                        for kt in range(nkt):
                            pS, pSk = pendq.pop(0)
                            qlo, n0 = geom(kt)
                            pt = PT[npt % 3]
                            ptk = 'PT%d' % (npt % 3)
                            npt += 1
                            col = n0
                            nearks = []
                            for qtile in range(qlo, 4 * c + 4):
                                d = qtile - kt
                                if d > 1:
                                    break
                                bt = biasD if d == 0 else biasS
                                ts_, tsk = tmpS[nts % 2], 'tmpS%d' % (nts % 2)
                                nts += 1
                                op('dve', 'scalar_tensor_tensor', [pSk, 'biasD', 'biasS'], [tsk], out=ts_[:],
                                   in0=pS[:, col:col + 128], scalar=SCALE, in1=bt[:, h, :], op0=ALU.mult, op1=ALU.add)
                                op('act', 'activation', [tsk], [ptk], out=pt[:, col:col + 128], in_=ts_[:], func=AF.Exp)
                                nearks.append(tsk)
                                col += 128
                            if col < 512:
                                op('act', 'activation', [pSk, 'cfar'] + nearks, [ptk], out=pt[:, col:512], in_=pS[:, col:512],
                                   func=AF.Exp, bias=cfar[:, h:h + 1], scale=SCALE)
                            mm(pOc[0:65, n0:512], Vt[:, kt, h * 65:(h + 1) * 65], pt[:, n0:512], ['Vt', ptk], [pOk],
                               kt == 0, kt == nkt - 1)
                            if kt + 2 < nkt:
                                pendq.append(issue_S(kt + 2))
                        op('act', 'copy', [pOk], ['osb', 'osbr'], osb[:, :], pOc[0:65, :])
                        op('dve', 'reciprocal', ['osb'], ['osbr'], osb[64:65, :], osb[64:65, :])
                        pe_raw('matmul', ['osbr', 'ones32'], ['pM'], argskw=ARGS(pM[0:64, :], ones32[64:65, 0:64], osb[64:65, :],
                                                            start=True, stop=True))
                        op('dve', 'tensor_tensor', ['osb', 'pM'], ['apair'], apair[s * 64:(s + 1) * 64, :],
                           osb[0:64, :], pM[0:64, :], ALU.mult)
                        if s == 1:
                            P.dma('sp', a_s[j, :, cq * 512:(cq + 1) * 512], apair[:], reads=['apair'], writes=['a_s'])
                            op('pool', 'tensor_tensor', ['apair'], ['sqa'], sqa[:], apair[:], apair[:], ALU.mult)
                            for t in range(4):
                                pe_raw('matmul', ['sqa', 'onesb'], ['pM'], sig=(t == 3), argskw=ARGS(pM[:, t:t + 1], sqa[:, t * 128:(t + 1) * 128], onesb[:, 0:1],
                                                                    start=True, stop=True))
                            op('dve', 'tensor_tensor', ['pM', 'ssa'], ['ssa'], ssa[:, cq * 4:cq * 4 + 4],
                               ssa[:, cq * 4:cq * 4 + 4], pM[:, 0:4], ALU.add)
                while lru_list:
                    lru_piece(*lru_list.pop(0))
                if DBG:
                    P.dma('sp', dout("d_ssa", [128, 16]), ssa[:], reads=['ssa'])
                    P.dma('sp', dout("d_ssl", [128, 16]), ssl[:], reads=['ssl'])
                P.barrier()
                if STOP == 3:
                    raise _Stop()

        P.flush()
        P.schedule = ('4' in SCHED_PH) and (os.environ.get('MK_SCHED', '1') == '1')
        lnB = sb(top, "lnB", [128, 4, D], F32)
        u2T = sb(top, "u2T", [128, 8, NOWN], BF16)
        P.dma('sp', lnB[:].rearrange("p a d -> p (a d)"),
              lnv.rearrange("a d -> (a d)").partition_broadcast(128), writes=['lnB'])
        u2r = ['u2T%d' % k for k in range(8)]
        wrb = sb(top, "wrb", [128, 8, 36], BF16)
        brB = sb(top, "brB", [128, 36], F32)
        P.dma('pool', wrb[:], w_r.rearrange("(k p) n -> p k n", p=128), writes=['wrb'])
        P.dma('sp', brB[:], b_r.rearrange("a n -> (a n)").partition_broadcast(128), writes=['brB'])
        gate = sb(top, "gate", [128, 16, NE], F32)
        lg = sb(top, "lg", [128, 36], F32)
        gmax = sb(top, "gmax", [128, 1], F32)
        ngmax = sb(top, "ngmax", [128, 1], F32)
        gex = sb(top, "gex", [128, 4], F32)
        gsum = sb(top, "gsum", [128, 1], F32)
        gtop = sb(top, "gtop", [128, 1], F32)
        goh = sb(top, "goh", [128, 4], F32)
        esel = sb(top, "esel", [128, 4, 8], F32)
        ein = sb(top, "ein", [128, 8], F32)
        et8 = sb(top, "et8", [128, 8], F32)
        nl1 = sb(top, "nl1", [128, 1], F32)
        eex = sb(top, "eex", [128, 8], F32)
        esl = sb(top, "esl", [128, 8], F32)
        eden = sb(top, "eden", [128, 1], F32)
        with contextlib.ExitStack() as s4:
            def router(tt):
                for k in range(8):
                    mm(pM[:, 0:36], u2T[:, k, tt * 128:(tt + 1) * 128], wrb[:, k, :], [u2r[k], 'wrb'], ['pM'], k == 0, k == 7)
                op('dve', 'tensor_tensor', ['pM', 'brB'], ['lg'], lg[:], pM[:, 0:36], brB[:], ALU.add)
                op('dve', 'tensor_reduce', ['lg'], ['gmax'], out=gmax[:], in_=lg[:, 0:4], axis=AX.X, op=ALU.max)
                op('dve', 'tensor_scalar_mul', ['gmax'], ['ngmax'], ngmax[:], gmax[:], -1.0)
                op('act', 'activation', ['lg', 'ngmax'], ['gex'], out=gex[:], in_=lg[:, 0:4], func=AF.Exp, bias=ngmax[:])
                op('dve', 'tensor_reduce', ['gex'], ['gsum'], out=gsum[:], in_=gex[:], axis=AX.X, op=ALU.add)
                op('dve', 'reciprocal', ['gsum'], ['gtop'], gtop[:], gsum[:])
                op('dve', 'tensor_tensor', ['lg', 'gmax'], ['goh'], goh[:], lg[:, 0:4], gmax[:].to_broadcast([128, 4]), ALU.is_ge)
                op('dve', 'tensor_tensor', ['lg', 'goh'], ['esel'], esel[:], lg[:, 4:36].rearrange("p (g e) -> p g e", e=8),
                   goh[:].unsqueeze(2).to_broadcast([128, 4, 8]), ALU.mult)
                op('dve', 'tensor_reduce', ['esel'], ['ein'], out=ein[:], in_=esel[:].rearrange("p g e -> p e g"),
                   axis=AX.X, op=ALU.add)
                op('dve', 'max', ['ein'], ['et8'], out=et8[:], in_=ein[:])
                op('dve', 'tensor_scalar_mul', ['et8'], ['nl1'], nl1[:], et8[:, 0:1], -1.0)
                op('act', 'activation', ['ein', 'nl1'], ['eex'], out=eex[:], in_=ein[:], func=AF.Exp, bias=nl1[:])
                op('dve', 'tensor_tensor', ['ein', 'et8'], ['esl'], esl[:], ein[:], et8[:, 1:2].to_broadcast([128, 8]), ALU.is_ge)
                op('dve', 'tensor_tensor', ['esl', 'eex'], ['esl'], esl[:], esl[:], eex[:], ALU.mult)
                op('dve', 'tensor_reduce', ['esl'], ['eden'], out=eden[:], in_=esl[:], axis=AX.X, op=ALU.add)
                op('dve', 'reciprocal', ['eden'], ['eden'], eden[:], eden[:])
                op('dve', 'tensor_tensor', ['eden', 'gtop'], ['eden'], eden[:], eden[:], gtop[:], ALU.mult)
                op('dve', 'tensor_scalar', ['esl', 'eden'], ['esl'], esl[:], esl[:], eden[:, 0:1], None, ALU.mult)
                op('dve', 'tensor_tensor', ['goh', 'esl'], ['gate'], gate[:, tt, :].rearrange("p (g e) -> p g e", e=8),
                   goh[:].unsqueeze(2).to_broadcast([128, 4, 8]), esl[:].unsqueeze(1).to_broadcast([128, 4, 8]), ALU.mult)
            loT = sb(s4, "loT", [128, 4, NOWN], BF16)
            aTp = sb(s4, "aTp", [128, 4, NOWN], BF16)
            for jj in range(4):
                P.dma('sp', loT[:, jj, :], lo_s[jj], reads=['lo_s'], writes=['loT%d' % jj])
                P.dma('sp', aTp[:, jj, :], a_s[jj], reads=['a_s'], writes=['aTp%d' % jj])
            if DBG:
                P.dma('sp', dout("d_loT", [128, 4 * NOWN], BF16), loT[:].rearrange("p j n -> p (j n)"),
                      reads=['loT%d' % j for j in range(4)])
                P.dma('sp', dout("d_aTp", [128, 4 * NOWN], BF16), aTp[:].rearrange("p j n -> p (j n)"),
                      reads=['aTp%d' % j for j in range(4)])
            woutb = sb(s4, "woutb", [128, 8, D], BF16)
            wo32 = [sb(s4, "wo32_%d" % i, [128, D], F32) for i in range(2)]
            gl = sb(s4, "gl", [128, 8], F32)
            P.dma('sp', gl[:, 0:4], glru, writes=['gl'])
            P.dma('sp', gl[:, 4:8], gattn, writes=['gl'])
            for k in range(8):
                wb, wk = wo32[k % 2], 'wo32_%d' % (k % 2)
                P.dma('sp', wb[:], w_out[k * 128:(k + 1) * 128, :], writes=[wk])
                op('act', 'activation', [wk, 'gl'], ['woutb%d' % k], out=woutb[:, k, :], in_=wb[:], func=AF.Identity, scale=gl[:, k:k + 1])
            NB4 = 3
            xo = [sb(s4, "xo%d" % i, [128, D], F32) for i in range(NB4)]
            mix = [sb(s4, "mix%d" % i, [128, D], F32) for i in range(NB4)]
            zz = [sb(s4, "zz%d" % i, [128, D], F32) for i in range(NB4)]
            x1 = [sb(s4, "x1_%d" % i, [128, D], F32) for i in range(NB4)]
            xn2 = [sb(s4, "xn2_%d" % i, [128, D], BF16) for i in range(NB4)]
            NST = 6
            st6 = [sb(s4, "st6b%d" % i, [128, 2, 6], F32) for i in range(NST)]
            mv = [sb(s4, "mvb%d" % i, [128, 2], F32) for i in range(NST)]
            rstd = [sb(s4, "rstdb%d" % i, [128, 1], F32) for i in range(NST)]
            nb = [sb(s4, "nbb%d" % i, [128, 1], F32) for i in range(NST)]
            ve4 = [sb(s4, "veb%d" % i, [128, 1], F32) for i in range(NST)]
            rl = sb(s4, "rl", [128, 16], F32)
            ra = sb(s4, "ra", [128, 16], F32)
            op('dve', 'tensor_scalar', ['ssl'], ['rl'], rl[:], ssl[:], 1.0 / 512, EPS, ALU.mult, ALU.add)
            op('pool', 'tensor_tensor', ['rl', 'mhalf'], ['rl'], rl[:], rl[:], mhalf[:, 0:16], ALU.pow)
            op('dve', 'tensor_scalar', ['ssa'], ['ra'], ra[:], ssa[:], 1.0 / 512, EPS, ALU.mult, ALU.add)
            op('pool', 'tensor_tensor', ['ra', 'mhalf'], ['ra'], ra[:], ra[:], mhalf[:, 0:16], ALU.pow)

            def ln_stats(src, srck, i):
                for hf in range(2):
                    op('dve', 'bn_stats', [srck], ['st6_%d' % i], out=st6[i][:, hf, :], in_=src[:, hf * 512:(hf + 1) * 512])
                op('dve', 'bn_aggr', ['st6_%d' % i], ['mv_%d' % i], out=mv[i][:], in_=st6[i][:].rearrange("p a b -> p (a b)"))
                op('dve', 'tensor_scalar_add', ['mv_%d' % i], ['ve_%d' % i], ve4[i][:], mv[i][:, 1:2], EPS)
                op('pool', 'tensor_tensor', ['ve_%d' % i, 'mhalf'], ['rstd_%d' % i], rstd[i][:], ve4[i][:], mhalf[:, 0:1], ALU.pow)
                op('dve', 'scalar_tensor_tensor', ['mv_%d' % i, 'rstd_%d' % i], ['nb_%d' % i], out=nb[i][:], in0=mv[i][:, 0:1],
                   scalar=-1.0, in1=rstd[i][:], op0=ALU.mult, op1=ALU.mult)

            for tt in range(16):
                b = tt % NB4
                sa_, sb_ = (2 * tt) % NST, (2 * tt + 1) % NST
                xok, mixk, zzk, x1k, xn2k = 'xo%d' % b, 'mix%d' % b, 'zz%d' % b, 'x1_%d' % b, 'xn2_%d' % b
                P.dma('sp', xo[b][:], xctx[OWN0 + tt * 128: OWN0 + (tt + 1) * 128, :], writes=[xok])
                for hf in range(2):
                    pa, pak, pb, pbk = (pA, 'pA', pB, 'pB') if hf == 0 else (pS0, 'pS0', pS1, 'pS1')
                    for jj in range(4):
                        mm(pa[:, :], loT[:, jj, tt * 128:(tt + 1) * 128], woutb[:, jj, hf * 512:(hf + 1) * 512],
                           ['loT%d' % jj, 'woutb%d' % jj], [pak], jj == 0, jj == 3)
                    for jj in range(4):
                        mm(pb[:, :], aTp[:, jj, tt * 128:(tt + 1) * 128], woutb[:, 4 + jj, hf * 512:(hf + 1) * 512],
                           ['aTp%d' % jj, 'woutb%d' % (4 + jj)], [pbk], jj == 0, jj == 3)
                    op('act', 'activation', [pak, 'rl'], [mixk], out=mix[b][:, hf * 512:(hf + 1) * 512], in_=pa[:, :],
                       func=AF.Identity, scale=rl[:, tt:tt + 1])
                    op('dve', 'scalar_tensor_tensor', [pbk, 'ra', mixk], [mixk], out=mix[b][:, hf * 512:(hf + 1) * 512],
                       in0=pb[:, :], scalar=ra[:, tt:tt + 1], in1=mix[b][:, hf * 512:(hf + 1) * 512], op0=ALU.mult, op1=ALU.add)
                op('pool', 'tensor_tensor', [mixk, 'g1B'], [zzk], zz[b][:], mix[b][:], g1B[:], ALU.mult)
                op('dve', 'scalar_tensor_tensor', [xok, zzk], [zzk], out=zz[b][:], in0=xo[b][:], scalar=ALPHA, in1=zz[b][:],
                   op0=ALU.mult, op1=ALU.add)
                ln_stats(zz[b], zzk, sa_)
                op('act', 'activation', [zzk, 'rstd_%d' % sa_, 'nb_%d' % sa_], [x1k], out=x1[b][:], in_=zz[b][:], func=AF.Identity,
                   bias=nb[sa_][:], scale=rstd[sa_][:])
                op('dve', 'tensor_tensor', [x1k, 'lnB'], [x1k], x1[b][:], x1[b][:], lnB[:, 0, :], ALU.mult)
                op('dve', 'tensor_tensor', [x1k, 'lnB'], [x1k], x1[b][:], x1[b][:], lnB[:, 1, :], ALU.add)
                P.dma('sp', x1_s[tt * 128:(tt + 1) * 128, :], x1[b][:], reads=[x1k], writes=['x1_s'])
                ln_stats(x1[b], x1k, sb_)
                op('act', 'activation', [x1k, 'rstd_%d' % sb_, 'nb_%d' % sb_], [xn2k], out=xn2[b][:], in_=x1[b][:], func=AF.Identity,
                   bias=nb[sb_][:], scale=rstd[sb_][:])
                pt, ptk = (pT0, 'pT0') if tt % 2 == 0 else (pT1, 'pT1')
                for k in range(8):
                    pe_raw('transpose', [xn2k, 'ident'], [ptk], sig=(k == 7), argskw=ARGS(pt[:, k * 128:(k + 1) * 128], xn2[b][:, k * 128:(k + 1) * 128], ident[:]))
                for k in range(8):
                    if tt % 2 == 0:
                        op('act', 'activation', [ptk, 'sc2p', 'modT'], ['u2T%d' % k], out=u2T[:, k, tt * 128:(tt + 1) * 128],
                           in_=pt[:, k * 128:(k + 1) * 128], func=AF.Identity, bias=modT[:, 24 + k:25 + k],
                           scale=sc2p[:, k:k + 1])
                    else:
                        op('dve', 'tensor_scalar', [ptk, 'sc2p', 'modT'], ['u2T%d' % k], u2T[:, k, tt * 128:(tt + 1) * 128],
                           pt[:, k * 128:(k + 1) * 128], sc2p[:, k:k + 1], modT[:, 24 + k:25 + k], ALU.mult, ALU.add)
                router(tt)
            if DBG:
                P.dma('sp', dout("d_u2T", [128, 8 * NOWN], BF16), u2T[:].rearrange("p k n -> p (k n)"),
                      reads=['u2T%d' % k for k in range(8)])
            P.barrier()
            if STOP == 4:
                raise _Stop()

        P.flush()
        P.schedule = ('5' in SCHED_PH) and (os.environ.get('MK_SCHED', '1') == '1')
        with contextlib.ExitStack() as s5:
            if DBG:
                P.dma('sp', dout("d_gate", [128, 16 * NE]), gate[:].rearrange("p t e -> p (t e)"), reads=['gate'])
            NS = 3
            w1b = [sb(s5, "w1b%d" % i, [128, 8, DE], BF16) for i in range(NS)]
            w3b = [sb(s5, "w3b%d" % i, [128, 8, DE], BF16) for i in range(NS)]
            w2b = [sb(s5, "w2b%d" % i, [128, 2, D], BF16) for i in range(NS)]
            yacc = sb(s5, "yacc", [128, 16, D], F32)
            ssi = [sb(s5, "ssi%d" % i, [128, 512], F32) for i in range(2)]
            hdn = [sb(s5, "hdn%d" % i, [128, 512], BF16) for i in range(4)]
            nh_ = 0
            ny = 0
            for e in range(NE):
                sl = e % NS
                P.dma('pool', w1b[sl][:], w1[e].rearrange("(k p) f -> p k f", p=128), writes=['w1b%d' % sl])
                P.dma('pool', w3b[sl][:], w3[e].rearrange("(k p) f -> p k f", p=128), writes=['w3b%d' % sl])
                P.dma('pool', w2b[sl][:], w2[e].rearrange("(c p) d -> p c d", p=128), writes=['w2b%d' % sl])
                for tc in range(4):
                    hk = []
                    for fc in range(2):
                        p1, p1k, p3, p3k = (pA, 'pA', pB, 'pB') if fc == 0 else (pS0, 'pS0', pS1, 'pS1')
                        for k in range(8):
                            mm(p1[:, :], w1b[sl][:, k, fc * 128:(fc + 1) * 128], u2T[:, k, tc * 512:(tc + 1) * 512],
                               ['w1b%d' % sl, u2r[k]], [p1k], k == 0, k == 7)
                        for k in range(8):
                            mm(p3[:, :], w3b[sl][:, k, fc * 128:(fc + 1) * 128], u2T[:, k, tc * 512:(tc + 1) * 512],
                               ['w3b%d' % sl, u2r[k]], [p3k], k == 0, k == 7)
                        si, sik = ssi[fc], 'ssi%d' % fc
                        hd, hdk = hdn[nh_ % 4], 'hdn%d' % (nh_ % 4)
                        nh_ += 1
                        op('act', 'activation', [p1k], [sik], out=si[:], in_=p1[:, :], func=AF.Silu)
                        op('dve', 'tensor_tensor', [sik, p3k], [hdk], hd[:], si[:], p3[:, :], ALU.mult)
                        hk.append((hd, hdk))
                    for t in range(4):
                        tt = tc * 4 + t
                        for hf in range(2):
                            py, pyk = (pO, 'pO') if ny % 2 == 0 else (pM, 'pM')
                            ny += 1
                            for fc in range(2):
                                mm(py[:, :], hk[fc][0][:, t * 128:(t + 1) * 128], w2b[sl][:, fc, hf * 512:(hf + 1) * 512],
                                   [hk[fc][1], 'w2b%d' % sl], [pyk], fc == 0, fc == 1)
                            if e == 0:
                                op('dve', 'tensor_scalar', [pyk, 'gate'], ['yacc%d' % tt], yacc[:, tt, hf * 512:(hf + 1) * 512],
                                   py[:, :], gate[:, tt, e:e + 1], None, ALU.mult)
                            else:
                                op('dve', 'scalar_tensor_tensor', [pyk, 'gate', 'yacc%d' % tt], ['yacc%d' % tt],
                                   out=yacc[:, tt, hf * 512:(hf + 1) * 512], in0=py[:, :], scalar=gate[:, tt, e:e + 1],
                                   in1=yacc[:, tt, hf * 512:(hf + 1) * 512], op0=ALU.mult, op1=ALU.add)
            NB5 = 3
            x1l = [sb(s5, "x1l%d" % i, [128, D], F32) for i in range(NB5)]
            zf = [sb(s5, "zf%d" % i, [128, D], F32) for i in range(NB5)]
            of = [sb(s5, "of%d" % i, [128, D], F32) for i in range(NB5)]
            st6 = [sb(s5, "st6c%d" % i, [128, 2, 6], F32) for i in range(NB5)]
            mv = [sb(s5, "mvc%d" % i, [128, 2], F32) for i in range(NB5)]
            rstd = [sb(s5, "rstdc%d" % i, [128, 1], F32) for i in range(NB5)]
            nb = [sb(s5, "nbc%d" % i, [128, 1], F32) for i in range(NB5)]
            ve5 = [sb(s5, "vec%d" % i, [128, 1], F32) for i in range(NB5)]
            for tt in range(16):
                b = tt % NB5
                x1lk, zfk, ofk = 'x1l%d' % b, 'zf%d' % b, 'of%d' % b
                P.dma('sp', x1l[b][:], x1_s[tt * 128:(tt + 1) * 128, :], reads=['x1_s'], writes=[x1lk])
                op('pool', 'tensor_tensor', ['yacc%d' % tt, 'g2B'], [zfk], zf[b][:], yacc[:, tt, :], g2B[:], ALU.mult)
                op('dve', 'scalar_tensor_tensor', [x1lk, zfk], [zfk], out=zf[b][:], in0=x1l[b][:], scalar=ALPHA, in1=zf[b][:],
                   op0=ALU.mult, op1=ALU.add)
                for hf in range(2):
                    op('dve', 'bn_stats', [zfk], ['st6f%d' % b], out=st6[b][:, hf, :], in_=zf[b][:, hf * 512:(hf + 1) * 512])
                op('dve', 'bn_aggr', ['st6f%d' % b], ['mvf%d' % b], out=mv[b][:], in_=st6[b][:].rearrange("p a b -> p (a b)"))
                op('dve', 'tensor_scalar_add', ['mvf%d' % b], ['vef%d' % b], ve5[b][:], mv[b][:, 1:2], EPS)
                op('pool', 'tensor_tensor', ['vef%d' % b, 'mhalf'], ['rstdf%d' % b], rstd[b][:], ve5[b][:], mhalf[:, 0:1], ALU.pow)
                op('dve', 'scalar_tensor_tensor', ['mvf%d' % b, 'rstdf%d' % b], ['nbf%d' % b], out=nb[b][:], in0=mv[b][:, 0:1],
                   scalar=-1.0, in1=rstd[b][:], op0=ALU.mult, op1=ALU.mult)
                op('act', 'activation', [zfk, 'rstdf%d' % b, 'nbf%d' % b], [ofk], out=of[b][:], in_=zf[b][:], func=AF.Identity,
                   bias=nb[b][:], scale=rstd[b][:])
                op('dve', 'tensor_tensor', [ofk, 'lnB'], [ofk], of[b][:], of[b][:], lnB[:, 2, :], ALU.mult)
                op('dve', 'tensor_tensor', [ofk, 'lnB'], [ofk], of[b][:], of[b][:], lnB[:, 3, :], ALU.add)
                P.dma('sp', out_d[tt * 128:(tt + 1) * 128, :], of[b][:], reads=[ofk], writes=['out'])
            P.barrier()
    except _Stop:
        P.barrier()
    P.close()
    return nc, list(dbg.keys())


def _host_inputs(inp):
    f = lambda a: np.ascontiguousarray(np.asarray(a, dtype=np.float32))
    x = f(inp['x']); c = f(inp['c'])
    per_part = lambda v: np.ascontiguousarray(v.reshape(-1, 128).T)
    shared = {
        'w_ada': f(inp['w_ada'][0]),
        'b_ada': per_part(f(inp['b_ada'][0])),
        'b_ada_row': np.ascontiguousarray(f(inp['b_ada'][0])[None, :]),
        'w_in': f(inp['w_in'][0]),
        'convw': np.ascontiguousarray(f(inp['conv_w'][0]).T.reshape(4, 128, 4).transpose(1, 0, 2).reshape(128, 16)),
        'convb': per_part(f(inp['conv_b'][0])),
        'bga': per_part(f(inp['b_gate_a'][0]).reshape(-1)),
        'bgx': per_part(f(inp['b_gate_x'][0]).reshape(-1)),
        'lam': per_part(f(inp['lru_lambda'][0])),
        'relb': f(inp['rel_bias']),
        'glru': per_part(f(inp['norm_lru_g'][0])),
        'gattn': per_part(f(inp['norm_attn_g'][0])),
        'w_out': f(inp['w_out'][0]),
        'lnv': np.ascontiguousarray(np.stack([f(inp['ln1_g'][0]), f(inp['ln1_b'][0]), f(inp['ln2_g'][0]), f(inp['ln2_b'][0])])),
        'w_r': np.ascontiguousarray(np.concatenate([f(inp['w_router_group'][0]), f(inp['w_router_expert'][0])], axis=1)),
        'b_r': np.ascontiguousarray(np.concatenate([f(inp['b_router_group'][0]), f(inp['b_router_expert'][0])])[None, :]),
        'w1': f(inp['w1'][0]), 'w3': f(inp['w3'][0]), 'w2': f(inp['w2'][0]),
    }
    for nm, src in (('WA', 'w_gate_a'), ('WX', 'w_gate_x')):
        w = f(inp[src][0])
        bd = np.zeros((4, 128, 128), np.float32)
        for j in range(4):
            for s in range(2):
                bd[j, s * 64:(s + 1) * 64, s * 64:(s + 1) * 64] = w[2 * j + s]
        shared[nm] = bd
    i = np.arange(384)
    r = 255 - i
    bk = _t5_bucket_np(r)
    Roh = np.zeros((32, 384), np.float32)
    valid = (r >= 0) & (i < 383)
    Roh[bk[valid], i[valid]] = 1.0
    NEGr = np.tile(np.where(r < 0, NEGM, 0.0).astype(np.float32)[None, :], (8, 1))
    IND = np.zeros((16, CT), np.float32)
    for n in range(16):
        IND[n, n * BLK:(n + 1) * BLK] = 1.0
    shared['Roh'] = Roh; shared['NEGr'] = np.ascontiguousarray(NEGr); shared['IND'] = IND
    maps = []
    for core in range(8):
        b, half = core // 2, core % 2
        m = dict(shared)
        if half == 1:
            m['xctx'] = np.ascontiguousarray(x[b])
        else:
            m['xctx'] = np.ascontiguousarray(np.concatenate([np.zeros((2048, D), np.float32), x[b, :2048]], axis=0))
        m['cvec'] = per_part(c[b])
        gm = np.zeros((16, 16), np.float32); oh = np.zeros((16, 16), np.float32)
        for qt in range(16):
            own = 8 + qt // 2
            for n in range(16):
                ok = (n < own) and (half == 1 or n >= 8)
                gm[qt, n] = 0.0 if ok else -1e30
            oh[qt, own] = 1.0
        m['gmask'] = np.ascontiguousarray(np.tile(gm.reshape(1, 256), (128, 1)))
        m['ownhot'] = np.ascontiguousarray(np.tile(oh.reshape(1, 256), (128, 1)))
        m['flag'] = np.full((128, 1), float(half), np.float32)
        maps.append(m)
    return maps


_NC_CACHE = {}


def kernel(**inputs):
    if 'nc' not in _NC_CACHE:
        _NC_CACHE['nc'] = build_program()
    nc, dbgnames = _NC_CACHE['nc']
    maps = _host_inputs(inputs)
    if STOP < 5:
        for m in maps:
            for k in ('w1', 'w3', 'w2'):
                m.pop(k)
    res = run_bass_kernel_spmd(nc, maps, core_ids=list(range(8)))
    out = np.zeros((4, SEQ, D), np.float32)
    for core in range(8):
        b, half = core // 2, core % 2
        out[b, half * 2048:(half + 1) * 2048] = res.results[core]['out']
    if DBG:
        kernel.dbg = [{k: res.results[core][k] for k in dbgnames} for core in range(8)]
    return out
```

```python
import contextlib
import math
import numpy as np
import ml_dtypes
import concourse.bass as bass
import concourse.mybir as mybir
from concourse.bass_utils import run_bass_kernel_spmd

F32 = mybir.dt.float32
BF16 = mybir.dt.bfloat16
AF = mybir.ActivationFunctionType
ALU = mybir.AluOpType
AX = mybir.AxisListType

D = 1024
SEQ = 4096
CT = 4096
OWN0 = 2048
NOWN = 2048
NH = 8
HD = 64
BLK = 256
NBLK = 16
NE = 32
DE = 256
EPS = 1e-5
ALPHA = 2.0 ** 0.25
NEGM = -30000.0
DBG = False
import os
STOP = int(os.environ.get('MK_STOP', '9'))
NCH = int(os.environ.get('MK_NCH', '8'))
SKIP = set(os.environ.get('MK_SKIP', '').split(','))
SCHED_PH = set(os.environ.get('MK_SCHED_PH', '0,1,2,3,4,5').split(','))


class _Stop(Exception):
    pass


class Prog:
    def __init__(self, nc, n_dma_sems=24):
        self.nc = nc
        self.E = {'pe': nc.tensor, 'act': nc.scalar, 'dve': nc.vector, 'pool': nc.gpsimd, 'sp': nc.sync}
        self.sem = {}
        self.cnt = {}
        self._ctx = []
        for e in ['pe', 'act', 'dve', 'pool']:
            g = nc.semaphore('s_' + e)
            self.sem[e] = g.__enter__()
            self._ctx.append(g)
            self.cnt[e] = 0
        self.dsem = []
        for i in range(n_dma_sems):
            g = nc.semaphore('d_%d' % i)
            self.dsem.append(g.__enter__())
            self._ctx.append(g)
        self.dcnt = [0] * n_dma_sems
        self.dnext = 0
        self.dnext_sw = 0
        self.waited = {}
        self.lastw = {}
        self.reads = {}
        self.dirty = {e: False for e in self.cnt}
        self.rec = []
        self.tnow = 0.0
        self.schedule = (os.environ.get('MK_SCHED', '1') == '1')

    def _semh(self, key):
        return self.sem[key] if isinstance(key, str) else self.dsem[key]

    def _wait(self, eng, ev, same_ok):
        key, val, src = ev
        if src == eng and same_ok:
            return
        if self.waited.get((eng, key), 0) >= val:
            return
        self.E[eng].wait_ge(self._semh(key), val)
        self.waited[(eng, key)] = val

    def _deps(self, eng, reads, writes, is_dma=False):
        for r in reads:
            ev = self.lastw.get(r)
            if ev is not None:
                self._wait(eng, ev, same_ok=(eng == 'pe' and not is_dma))
        for w in writes:
            ev = self.lastw.get(w)
            if ev is not None:
                self._wait(eng, ev, same_ok=(eng == 'pe' and not is_dma))
            for ev in self.reads.get(w, ()):
                self._wait(eng, ev, same_ok=(eng == 'pe' and not is_dma))

    def _record(self, ev, reads, writes):
        for r in reads:
            lst = self.reads.setdefault(r, [])
            lst.append(ev)
            if len(lst) > 16:
                best = {}
                for k, v, s in lst:
                    if k not in best or best[k][1] < v:
                        best[k] = (k, v, s)
                self.reads[r] = list(best.values())
        for w in writes:
            self.lastw[w] = ev
            self.reads[w] = []

    def op(self, eng, fn, reads=(), writes=(), sig=True, est=0.5):
        self.rec.append(dict(kind='op', eng=eng, fn=fn, reads=tuple(reads), writes=tuple(writes), sig=sig, est=est))

    def dma(self, q, out, in_, reads=(), writes=(), **kw):
        try:
            nbytes = out.nbytes()
        except Exception:
            nbytes = 65536
        self.rec.append(dict(kind='dma', eng=q, out=out, in_=in_, kw=kw, reads=tuple(reads), writes=tuple(writes),
                             sig=True, est=2.0 + nbytes / 150e3))

    def _emit_op(self, eng, fn, reads, writes, sig):
        self._deps(eng, reads, writes)
        ins = fn()
        if sig:
            self.cnt[eng] += 1
            ins.then_inc(self.sem[eng], 1)
            ev = (eng, self.cnt[eng], eng)
            self.dirty[eng] = False
        else:
            ev = (eng, self.cnt[eng] + 1, eng)
            self.dirty[eng] = True
        self._record(ev, reads, writes)
        return ins

    def _emit_dma(self, q, out, in_, reads, writes, kw):
        half = len(self.dsem) // 2
        if q == 'pool':
            i = self.dnext_sw
            self.dnext_sw = (self.dnext_sw + 1) % half
        else:
            i = half + self.dnext
            self.dnext = (self.dnext + 1) % (len(self.dsem) - half)
        if self.dcnt[i] > 0:
            self._wait(q, (i, self.dcnt[i], 'dma'), same_ok=False)
        self._deps(q, reads, writes, is_dma=True)
        ins = self.E[q].dma_start(out=out, in_=in_, **kw)
        self.dcnt[i] += 16
        ins.then_inc(self.dsem[i], 16)
        ev = (i, self.dcnt[i], 'dma')
        self._record(ev, reads, writes)
        return ev

    def flush(self):
        rec = self.rec
        self.rec = []
        if not rec:
            return
        nodes = []
        cur_pe = None
        for r in rec:
            if r['kind'] == 'op' and r['eng'] == 'pe':
                if cur_pe is None:
                    cur_pe = dict(eng='pe', items=[], est=0.0)
                    nodes.append(cur_pe)
                cur_pe['items'].append(r)
                cur_pe['est'] += r['est']
                if r['sig']:
                    cur_pe = None
            else:
                if cur_pe is not None:
                    cur_pe['items'][-1]['sig'] = True
                    cur_pe = None
                nodes.append(dict(eng=r['eng'], items=[r], est=r['est'], isdma=(r['kind'] == 'dma')))
        if cur_pe is not None:
            cur_pe['items'][-1]['sig'] = True
            cur_pe = None
        n = len(nodes)
        lastw = {}
        readers = {}
        preds = [set() for _ in range(n)]
        for i, nd in enumerate(nodes):
            R = set(); Wr = set()
            for it in nd['items']:
                R.update(it['reads']); Wr.update(it['writes'])
            for r_ in R:
                if r_ in lastw:
                    preds[i].add(lastw[r_])
            for w_ in Wr:
                if w_ in lastw:
                    preds[i].add(lastw[w_])
                for j in readers.get(w_, ()):
                    preds[i].add(j)
            for r_ in R:
                readers.setdefault(r_, []).append(i)
            for w_ in Wr:
                lastw[w_] = i
                readers[w_] = []
            preds[i].discard(i)
        if not self.schedule:
            order = list(range(n))
        else:
            engs = ['pe', 'act', 'dve', 'pool', 'sp']
            per = {e: [] for e in engs}
            for i, nd in enumerate(nodes):
                per[nd['eng']].append(i)
            ptr = {e: 0 for e in engs}
            done = [False] * n
            fin = [0.0] * n
            free = {e: self.tnow for e in engs}
            order = []
            WINDOW = int(os.environ.get("MK_WIN", "48"))
            remaining = n
            while remaining:
                best = None
                for e in engs:
                    lst = per[e]
                    p0 = ptr[e]
                    while p0 < len(lst) and done[lst[p0]]:
                        p0 += 1
                    ptr[e] = p0
                    cnt = 0
                    k = p0
                    while k < len(lst) and cnt < WINDOW:
                        i = lst[k]
                        k += 1
                        if done[i]:
                            continue
                        cnt += 1
                        ok = True
                        t = free[e]
                        for pj in preds[i]:
                            if not done[pj]:
                                ok = False
                                break
                            if fin[pj] > t:
                                t = fin[pj]
                        if not ok:
                            continue
                        key = (t + 0.002 * (cnt - 1), i)
                        if best is None or key < best[0]:
                            best = (key, e, i, t)
                        if t <= free[e] + 1e-9:
                            break
                assert best is not None, "scheduler deadlock"
                _, e, i, t = best
                nd = nodes[i]
                done[i] = True
                remaining -= 1
                if nd.get('isdma'):
                    free[e] = t + 0.15
                    fin[i] = t + nd['est']
                else:
                    free[e] = t + nd['est']
                    fin[i] = t + nd['est'] + 0.1
                order.append((t, i))
            order.sort()
            order = [i for _, i in order]
            self.tnow = max(max(free.values()), max(fin) if fin else 0.0)
        for i in order:
            for it in nodes[i]['items']:
                if it['kind'] == 'op':
                    self._emit_op(it['eng'], it['fn'], it['reads'], it['writes'], it['sig'])
                else:
                    self._emit_dma(it['eng'], it['out'], it['in_'], it['reads'], it['writes'], it['kw'])

    def barrier(self):
        self.flush()
        for e in self.cnt:
            assert not self.dirty[e], e
        for eng in ['pe', 'act', 'dve', 'pool', 'sp']:
            for e in self.cnt:
                if self.cnt[e] > 0:
                    self._wait(eng, (e, self.cnt[e], e), same_ok=False)
            for i in range(len(self.dsem)):
                if self.dcnt[i] > 0:
                    self._wait(eng, (i, self.dcnt[i], 'dma'), same_ok=False)
        self.lastw = {}
        self.reads = {}

    def close(self):
        self.flush()
        for g in reversed(self._ctx):
            g.__exit__(None, None, None)


def _t5_bucket_np(n):
    n = np.maximum(n, 0)
    max_exact = 16
    nf = np.maximum(n, 1).astype(np.float32)
    large = max_exact + (np.log(nf / np.float32(max_exact)) / np.float32(math.log(128 / 16))
                         * np.float32(32 - max_exact)).astype(np.int32)
    large = np.minimum(large, 31)
    return np.where(n < max_exact, n, large)


def build_program():
    nc = bass.Bass("TRN2", target_bir_lowering=False)

    def din(name, shape, dt=F32):
        return nc.dram_tensor(name, list(shape), dt, kind="ExternalInput").ap()

    xctx = din("xctx", [CT, D])
    cvec = din("cvec", [128, 8])
    w_ada = din("w_ada", [D, 6 * D])
    b_ada = din("b_ada", [128, 48])
    b_ada_row = din("b_ada_row", [1, 6 * D])
    w_in = din("w_in", [D, 2560])
    convw = din("convw", [128, 16])
    convb = din("convb", [128, 4])
    WA = din("WA", [4, 128, 128])
    WX = din("WX", [4, 128, 128])
    bga = din("bga", [128, 4])
    bgx = din("bgx", [128, 4])
    lam = din("lam", [128, 4])
    relb = din("relb", [32, 8])
    glru = din("glru", [128, 4])
    gattn = din("gattn", [128, 4])
    w_out = din("w_out", [D, D])
    lnv = din("lnv", [4, D])
    w_r = din("w_r", [D, 36])
    b_r = din("b_r", [1, 36])
    if STOP >= 5:
        w1 = din("w1", [NE, D, DE])
        w3 = din("w3", [NE, D, DE])
        w2 = din("w2", [NE, DE, D])
    gmask_d = din("gmask", [128, 256])
    ownhot_d = din("ownhot", [128, 256])
    flag_d = din("flag", [128, 1])
    Roh = din("Roh", [32, 384])
    NEGr = din("NEGr", [8, 384])
    IND = din("IND", [16, CT])
    out_d = nc.dram_tensor("out", [NOWN, D], F32, kind="ExternalOutput").ap()

    xlru_s = nc.dram_tensor("xlru_s", [4, 128, CT], F32, kind="Internal").ap()
    gy_s = nc.dram_tensor("gy_s", [4, 128, NOWN], BF16, kind="Internal").ap()
    x1_s = nc.dram_tensor("x1_s", [NOWN, D], F32, kind="Internal").ap()
    q_s = nc.dram_tensor("q_s", [4, 128, NOWN], BF16, kind="Internal").ap()
    lo_s = nc.dram_tensor("lo_s", [4, 128, NOWN], BF16, kind="Internal").ap()
    a_s = nc.dram_tensor("a_s", [4, 128, NOWN], BF16, kind="Internal").ap()
    G_s = nc.dram_tensor("G_s", [8, 384], F32, kind="Internal")
    dbg = {}

    def dout(name, shape, dt=F32):
        dbg[name] = nc.dram_tensor(name, list(shape), dt, kind="ExternalOutput").ap()
        return dbg[name]

    P = Prog(nc)
    try:
      with contextlib.ExitStack() as top:
        _nm = [0]

        def sb(st, name, shape, dt):
            _nm[0] += 1
            return st.enter_context(nc.sbuf_tensor("%s_u%d" % (name, _nm[0]), list(shape), dt))

        def ps(st, name, shape, dt):
            return st.enter_context(nc.psum_tensor(name, list(shape), dt))

        E = P.E

        def _est(eng, a, kw):
            o = kw.get('out', a[0] if a else None)
            try:
                nfree = 1
                for d_ in o.shape[1:]:
                    nfree *= d_
            except Exception:
                nfree = 512
            if eng == 'pe':
                mv_ = kw.get('rhs', a[2] if len(a) > 2 else None)
                try:
                    nm = 1
                    for d_ in mv_.shape[1:]:
                        nm *= d_
                except Exception:
                    nm = 128
                return 0.06 + max(nm, 64) / 2000.0
            if eng == 'act':
                return 0.22 + nfree / 1100.0
            if eng == 'dve':
                return 0.10 + nfree / 900.0
            return 0.15 + nfree / 440.0

        def op(eng, meth, reads, writes, *args, sig=True, **kw):
            return P.op(eng, lambda: getattr(E[eng], meth)(*args, **kw), reads, writes, sig, est=_est(eng, args, kw))

        def ARGS(*a, **kw):
            return (a, kw)

        def pe_raw(meth, reads, writes, argskw=None, sig=True):
            a, kw = argskw
            return P.op('pe', lambda: getattr(nc.tensor, meth)(*a, **kw), reads, writes, sig, est=_est('pe', a, kw))

        def mm(out, lhsT, rhs, reads, writes, start, stop):
            return P.op('pe', lambda: nc.tensor.matmul(out, lhsT, rhs, start=start, stop=stop), reads, writes, sig=stop,
                        est=_est('pe', (out, lhsT, rhs), {}))

        pT0 = ps(top, "pT0", [128, 1024], BF16)
        pT1 = ps(top, "pT1", [128, 1024], BF16)
        pA = ps(top, "pA", [128, 512], F32)
        pB = ps(top, "pB", [128, 512], F32)
        pS0 = ps(top, "pS0", [128, 512], F32)
        pS1 = ps(top, "pS1", [128, 512], F32)
        pO = ps(top, "pO", [128, 512], F32)
        pM = ps(top, "pM", [128, 512], F32)

        ident = sb(top, "ident", [128, 128], BF16)
        id32 = sb(top, "id32", [128, 128], F32)
        ones32 = sb(top, "ones32", [128, 128], F32)
        onesb = sb(top, "onesb", [128, 1], BF16)
        modT = sb(top, "modT", [128, 48], F32)
        sc1p = sb(top, "sc1p", [128, 8], F32)
        sc2p = sb(top, "sc2p", [128, 8], F32)
        g1B = sb(top, "g1B", [128, D], F32)
        g2B = sb(top, "g2B", [128, D], F32)
        mhalf = sb(top, "mhalf", [128, 16], F32)
        ve = sb(top, "ve", [128, 1], F32)

        op('pool', 'memset', [], ['id32'], id32[:], 1.0)
        op('pool', 'affine_select', ['id32'], ['id32'], out=id32[:], in_=id32[:], pattern=[[-1, 128]],
           compare_op=ALU.is_equal, fill=0.0, base=0, channel_multiplier=1)
        op('dve', 'tensor_copy', ['id32'], ['ident'], ident[:], id32[:])
        op('pool', 'memset', [], ['ones32'], ones32[:], 1.0)
        op('dve', 'memset', [], ['onesb'], onesb[:], 1.0)
        op('dve', 'memset', [], ['mhalf'], mhalf[:], -0.5)

        P.flush()
        P.schedule = ('0' in SCHED_PH) and (os.environ.get('MK_SCHED', '1') == '1')
        with contextlib.ExitStack() as s0:
            csb = sb(s0, "csb", [128, 8], F32)
            modrow = sb(s0, "modrow", [1, 6 * D], F32)
            csil = sb(s0, "csil", [128, 8], BF16)
            bada = sb(s0, "bada", [128, 48], F32)
            wad = [sb(s0, "wad%d" % i, [128, 8, 512], BF16) for i in range(2)]
            P.dma('sp', csb[:], cvec, writes=['csb'])
            P.dma('sp', bada[:], b_ada, writes=['bada'])
            op('act', 'activation', ['csb'], ['csil'], out=csil[:], in_=csb[:], func=AF.Silu)
            for cc in range(12):
                w = wad[cc % 2]
                wk = 'wad%d' % (cc % 2)
                P.dma('pool', w[:], w_ada[:, cc * 512:(cc + 1) * 512].rearrange("(k p) n -> p k n", p=128),
                      writes=[wk])
                pp = pA if cc % 2 == 0 else pB
                pk = 'pA' if cc % 2 == 0 else 'pB'
                for k in range(8):
                    mm(pp[0:1, :], csil[:, k:k + 1], w[:, k, :], ['csil', wk], [pk], k == 0, k == 7)
                op('act', 'copy', [pk], ['modrow'], modrow[0:1, cc * 512:(cc + 1) * 512], pp[0:1, :])
            for ct in range(48):
                pe_raw('matmul', ['modrow', 'ones32'], ['pM'], sig=(ct == 47), argskw=ARGS(pM[:, ct:ct + 1], modrow[0:1, ct * 128:(ct + 1) * 128],
                                                    ones32[0:1, 0:1], start=True, stop=True))
            op('dve', 'tensor_tensor', ['pM', 'bada'], ['modT'], modT[:], pM[:, 0:48], bada[:], ALU.add)
            op('dve', 'tensor_scalar_add', ['modT'], ['sc1p'], sc1p[:], modT[:, 8:16], 1.0)
            op('dve', 'tensor_scalar_add', ['modT'], ['sc2p'], sc2p[:], modT[:, 32:40], 1.0)
            for (dst, dk, off) in ((g1B, 'g1B', 2 * D), (g2B, 'g2B', 5 * D)):
                for hf in range(2):
                    pe_raw('matmul', ['modrow', 'ones32'], ['pA'], argskw=ARGS(pA[:, :], ones32[0:1, :],
                                                        modrow[0:1, off + hf * 512: off + (hf + 1) * 512],
                                                        start=True, stop=True))
                    op('act', 'copy', ['pA'], [dk], dst[:, hf * 512:(hf + 1) * 512], pA[:, :])
            badarow = sb(s0, "badarow", [128, 2, D], F32)
            P.dma('sp', badarow[:, 0, :], b_ada_row[0:1, 2 * D:3 * D].rearrange("a n -> (a n)").partition_broadcast(128),
                  writes=['badarow'])
            P.dma('sp', badarow[:, 1, :], b_ada_row[0:1, 5 * D:6 * D].rearrange("a n -> (a n)").partition_broadcast(128),
                  writes=['badarow'])
            op('pool', 'tensor_tensor', ['g1B', 'badarow'], ['g1B'], g1B[:], g1B[:], badarow[:, 0, :], ALU.add)
            op('pool', 'tensor_tensor', ['g2B', 'badarow'], ['g2B'], g2B[:], g2B[:], badarow[:, 1, :], ALU.add)
            if DBG:
                P.dma('sp', dout("d_modT", [128, 48]), modT[:], reads=['modT'])
                P.dma('sp', dout("d_g1B", [128, D]), g1B[:], reads=['g1B'])
            P.barrier()
            if STOP == 0:
                raise _Stop()

        ssl = sb(top, "ssl", [128, 16], F32)
        ssa = sb(top, "ssa", [128, 16], F32)
        op('dve', 'memset', [], ['ssl'], ssl[:], 0.0)
        op('dve', 'memset', [], ['ssa'], ssa[:], 0.0)
        with contextlib.ExitStack() as s13:
            kT = [sb(s13, "kT%d" % h, [96, CT], BF16) for h in range(NH)]
            Vt = sb(s13, "Vt", [128, 32, NH * 65], BF16)
            op('pool', 'memset', [], ['Vt'], Vt[:].rearrange("p t c -> p (t c)"), 1.0)
            for h in range(NH):
                op('dve', 'memset', [], ['kTind%d' % h], kT[h][64:96, :], 0.0)
                P.dma('pool', kT[h][64:80, :], IND, writes=['kTind%d' % h])

            P.flush()
            P.schedule = ('1' in SCHED_PH) and (os.environ.get('MK_SCHED', '1') == '1')
            with contextlib.ExitStack() as s1:
                winb = sb(s1, "winb", [128, 8, 2560], BF16)
                for k in range(8):
                    P.dma('pool', winb[:, k, :], w_in[k * 128:(k + 1) * 128, :], writes=['winb%d' % k])
                xt = [sb(s1, "xt%d" % i, [128, D], F32) for i in range(2)]
                xn = [sb(s1, "xn%d" % i, [128, D], BF16) for i in range(8)]
                uTs = [sb(s1, "uT%d" % i, [128, 8, 512], BF16) for i in range(2)]
                st6 = [sb(s1, "st6_%d" % i, [128, 2, 6], F32) for i in range(4)]
                mv = [sb(s1, "mv_%d" % i, [128, 2], F32) for i in range(4)]
                rstd = [sb(s1, "rstd_%d" % i, [128, 1], F32) for i in range(4)]
                nb = [sb(s1, "nb_%d" % i, [128, 1], F32) for i in range(4)]
                ve1 = [sb(s1, "ve_%d" % i, [128, 1], F32) for i in range(4)]
                stg = [sb(s1, "stg%d" % i, [128, 512], F32) for i in range(2)]
                ysb = sb(s1, "ysb", [128, 512], F32)
                yt = sb(s1, "yt", [128, 512], F32)
                ysg = sb(s1, "ysg", [128, 512], F32)
                gyb = [sb(s1, "gyb%d" % i, [128, 512], BF16) for i in range(2)]
                winr = ['winb%d' % k for k in range(8)]
                nstg = 0

                def emit_ln(c):
                    for t in range(4):
                        T = 4 * c + t
                        xb = xt[T % 2]
                        xk = 'xt%d' % (T % 2)
                        i = T % 4
                        xi = (c % 2) * 4 + t
                        P.dma('sp', xb[:], xctx[T * 128:(T + 1) * 128, :], writes=[xk])
                        for hf in range(2):
                            op('dve', 'bn_stats', [xk], ['st6_%d' % i], out=st6[i][:, hf, :], in_=xb[:, hf * 512:(hf + 1) * 512])
                        op('dve', 'bn_aggr', ['st6_%d' % i], ['mv_%d' % i], out=mv[i][:], in_=st6[i][:].rearrange("p a b -> p (a b)"))
                        op('dve', 'tensor_scalar_add', ['mv_%d' % i], ['ve_%d' % i], ve1[i][:], mv[i][:, 1:2], EPS)
                        op('pool', 'tensor_tensor', ['ve_%d' % i, 'mhalf'], ['rstd_%d' % i], rstd[i][:], ve1[i][:], mhalf[:, 0:1], ALU.pow)
                        op('dve', 'scalar_tensor_tensor', ['mv_%d' % i, 'rstd_%d' % i], ['nb_%d' % i], out=nb[i][:], in0=mv[i][:, 0:1],
                           scalar=-1.0, in1=rstd[i][:], op0=ALU.mult, op1=ALU.mult)
                        op('act', 'activation', [xk, 'rstd_%d' % i, 'nb_%d' % i], ['xn%d' % xi], out=xn[xi][:], in_=xb[:],
                           func=AF.Identity, bias=nb[i][:], scale=rstd[i][:])

                def emit_tr(c, r):
                    uT = uTs[c % 2]
                    pt = pT0 if r % 2 == 0 else pT1
                    ptk = 'pT0' if r % 2 == 0 else 'pT1'
                    for kk in range(2):
                        k = 2 * r + kk
                        for t in range(4):
                            xi = (c % 2) * 4 + t
                            pe_raw('transpose', ['xn%d' % xi, 'ident'], [ptk], sig=(kk == 1 and t == 3), argskw=ARGS(
                                pt[:, kk * 512 + t * 128: kk * 512 + (t + 1) * 128],
                                xn[xi][:, k * 128:(k + 1) * 128], ident[:]))
                    for kk in range(2):
                        k = 2 * r + kk
                        uk = 'uT%d_%d' % (c % 2, k)
                        if r % 2 == 0:
                            op('act', 'activation', [ptk, 'sc1p', 'modT'], [uk], out=uT[:, k, :],
                               in_=pt[:, kk * 512:(kk + 1) * 512], func=AF.Identity,
                               bias=modT[:, k:k + 1], scale=sc1p[:, k:k + 1])
                        else:
                            op('dve', 'tensor_scalar', [ptk, 'sc1p', 'modT'], [uk], uT[:, k, :],
                               pt[:, kk * 512:(kk + 1) * 512], sc1p[:, k:k + 1], modT[:, k:k + 1],
                               ALU.mult, ALU.add)

                if NCH > 0:
                    emit_ln(0)
                    for r in range(4):
                        emit_tr(0, r)
                for c in range(NCH):
                    own = c >= 4
                    uT = uTs[c % 2]
                    if c + 1 < NCH:
                        emit_ln(c + 1)
                    pend_tr = list(range(4)) if c + 1 < NCH else []
                    uTr = ['uT%d_%d' % (c % 2, k) for k in range(8)]
                    cts = list(range(0, 4)) + (list(range(4, 12)) if own else []) + list(range(12, 16))
                    if 'fm' in SKIP:
                        cts = []
                    every = max(1, len(cts) // 4)
                    for ci, ct in enumerate(cts):
                        if pend_tr and ci > 0 and ci % every == 0:
                            emit_tr(c + 1, pend_tr.pop(0))
                        pp, pk = [(pA, 'pA'), (pB, 'pB'), (pO, 'pO'), (pM, 'pM')][ci % 4]
                        for k in range(8):
                            mm(pp[:, :], winb[:, k, ct * 128:(ct + 1) * 128], uT[:, k, :],
                               [winr[k], uTr[k]], [pk], k == 0, k == 7)
                        j = ct % 4
                        if ct < 4:
                            sg = stg[nstg % 2]
                            sk = 'stg%d' % (nstg % 2)
                            nstg += 1
                            op('act', 'copy', [pk], [sk], sg[:], pp[:, :])
                            P.dma('sp', xlru_s[j, :, c * 512:(c + 1) * 512], sg[:], reads=[sk], writes=['xlru_s'])
                        elif ct < 8:
                            gb = gyb[j % 2]
                            gk = 'gyb%d' % (j % 2)
                            op('act', 'copy', [pk], ['ysb'], ysb[:], pp[:, :])
                            op('pool', 'tensor_tensor', ['ysb'], ['yt'], yt[:], ysb[:], ysb[:], ALU.mult)
                            op('pool', 'tensor_scalar', ['yt'], ['yt'], yt[:], yt[:], 0.044715, 1.0, ALU.mult, ALU.add)
                            op('pool', 'tensor_tensor', ['yt', 'ysb'], ['yt'], yt[:], yt[:], ysb[:], ALU.mult)
                            op('act', 'activation', ['yt'], ['ysg'], out=ysg[:], in_=yt[:], func=AF.Sigmoid,
                               scale=1.5957691216057308)
                            op('pool', 'tensor_tensor', ['ysg', 'ysb'], [gk], gb[:], ysg[:], ysb[:], ALU.mult)
                            P.dma('sp', gy_s[j, :, (c - 4) * 512:(c - 3) * 512], gb[:], reads=[gk], writes=['gy_s'])
                        elif ct < 12:
                            oc = (c - 4) * 512
                            gb = gyb[j % 2]
                            gk = 'gyb%d' % (j % 2)
                            op('act', 'copy', [pk], [gk], gb[:], pp[:, :])
                            P.dma('sp', q_s[j, :, oc:oc + 512], gb[:], reads=[gk], writes=['q_s'])
                        else:
                            ke, km = ('act', 'copy') if j % 2 == 0 else ('dve', 'tensor_copy')
                            op(ke, km, [pk], ['kT%d' % (2 * j)], kT[2 * j][0:64, c * 512:(c + 1) * 512], pp[0:64, :])
                            op(ke, km, [pk], ['kT%d' % (2 * j + 1)],
                               kT[2 * j + 1][0:64, c * 512:(c + 1) * 512], pp[64:128, :])
                    for t in range(0 if 'v' in SKIP else 4):
                        if pend_tr:
                            emit_tr(c + 1, pend_tr.pop(0))
                        T = 4 * c + t
                        pp, pk = (pS0, 'pS0') if t % 2 == 0 else (pS1, 'pS1')
                        for k in range(8):
                            mm(pp[:, :], uT[:, k, t * 128:(t + 1) * 128], winb[:, k, 2048:2560],
                               [winr[k], uTr[k]], [pk], k == 0, k == 7)
                        op('dve' if t % 2 == 0 else 'act', 'tensor_copy' if t % 2 == 0 else 'copy', [pk], ['Vt'],
                           Vt[:, T, :].rearrange("p (h e) -> p h e", e=65)[:, :, 0:64],
                           pp[:, :].rearrange("p (h d) -> p h d", d=64))
                    while pend_tr:
                        emit_tr(c + 1, pend_tr.pop(0))
                if DBG:
                    for h in (0, 1, 7):
                        P.dma('sp', dout("d_kT%d" % h, [80, CT], BF16), kT[h][0:80, :], reads=['kT%d' % h, 'kTind%d' % h])
                    P.dma('sp', dout("d_V", [128, 32 * 520], BF16), Vt[:].rearrange("p t c -> p (t c)"), reads=['Vt'])
                P.barrier()
                if STOP == 1:
                    raise _Stop()

            P.flush()
            P.schedule = ('3' in SCHED_PH) and (os.environ.get('MK_SCHED', '1') == '1')
            with contextlib.ExitStack() as s3:
                qT = [sb(s3, "qT%d" % h, [96, NOWN], BF16) for h in range(NH)]
                for h in range(NH):
                    P.dma('sp', qT[h][0:64, :], q_s[h // 2, (h % 2) * 64:(h % 2) * 64 + 64, :], reads=['q_s'], writes=['qT%d' % h])
                    op('dve', 'memset', [], ['qTm%d' % h], qT[h][64:96, :], 0.0)
                apair = sb(s3, "apair", [128, 512], BF16)
                gmask = sb(s3, "gmask_t", [128, 16, 16], F32)
                ownhot = sb(s3, "ownhot_t", [128, 16, 16], F32)
                P.dma('sp', gmask[:].rearrange("p a b -> p (a b)"), gmask_d, writes=['gmask'])
                P.dma('sp', ownhot[:].rearrange("p a b -> p (a b)"), ownhot_d, writes=['ownhot'])
                cfar = sb(s3, "cfar", [128, 8], F32)
                biasD = sb(s3, "biasD", [128, 8, 128], F32)
                biasS = sb(s3, "biasS", [128, 8, 128], F32)
                kmf = sb(s3, "kmf", [64, NH, 16], F32)
                kmb = sb(s3, "kmb", [64, NH, 16], BF16)
                s3a = contextlib.ExitStack()
                s3a.__enter__()
                relsb = sb(s3a, "relsb", [32, 8], F32)
                Rsb = sb(s3a, "Rsb", [32, 384], F32)
                Gsb = sb(s3a, "Gsb", [8, 384], F32)
                negsb = sb(s3a, "negsb", [8, 384], F32)
                P.dma('sp', relsb[:], relb, writes=['relsb'])
                P.dma('sp', Rsb[:], Roh, writes=['Rsb'])
                P.dma('sp', negsb[:], NEGr, writes=['negsb'])
                P.dma('sp', cfar[:], relb[31:32, :].rearrange("a h -> (a h)").partition_broadcast(128), writes=['cfar'])
                mm(pA[0:8, 0:384], relsb[:, :], Rsb[:, :], ['relsb', 'Rsb'], ['pA'], True, True)
                op('dve', 'tensor_tensor', ['pA', 'negsb'], ['Gsb'], Gsb[:], pA[0:8, 0:384], negsb[:], ALU.add)
                P.dma('sp', G_s.ap(), Gsb[:], reads=['Gsb'], writes=['G_s'])
                hank = sb(s3a, "hank", [128, 16, 128], F32)
                for h in range(NH):
                    P.dma('sp', hank[:, h, :], bass.AP(G_s, h * 384 + 128, [[1, 128], [1, 128]]),
                          reads=['G_s'], writes=['hank'])
                    P.dma('sp', hank[:, 8 + h, :], bass.AP(G_s, h * 384, [[1, 128], [1, 128]]),
                          reads=['G_s'], writes=['hank'])
                for h in range(NH):
                    op('pool', 'tensor_copy', ['hank'], ['biasD'], biasD[:, h, :], hank[:, h, ::-1])
                    op('pool', 'tensor_copy', ['hank'], ['biasS'], biasS[:, h, :], hank[:, 8 + h, ::-1])
                for h in range(NH):
                    op('dve', 'tensor_reduce', ['kT%d' % h], ['kmf'], out=kmf[:, h, :],
                       in_=kT[h][0:64, :].rearrange("p (n b) -> p n b", b=BLK), axis=AX.X, op=ALU.add)
                op('dve', 'tensor_scalar_mul', ['kmf'], ['kmb'], kmb[:].rearrange("p h n -> p (h n)"),
                   kmf[:].rearrange("p h n -> p (h n)"), 1.0 / BLK)
                _sch3 = P.schedule
                s3a.__exit__(None, None, None)
                P.barrier()
                P.schedule = _sch3
                pT0f = pT0[:].bitcast(F32)
                pT1f = pT1[:].bitcast(F32)
                cw = sb(s3, "cw", [128, 16], F32)
                cb = sb(s3, "cb", [128, 4], F32)
                bA = sb(s3, "bA", [128, 4], F32)
                bX = sb(s3, "bX", [128, 4], F32)
                lamt = sb(s3, "lamt", [128, 4], F32)
                cL = sb(s3, "cL", [128, 4], F32)
                cL2 = sb(s3, "cL2", [128, 4], F32)
                flag = sb(s3, "flag_t", [128, 1], F32)
                carry = sb(s3, "carry", [128, 4], F32)
                WAb = sb(s3, "WAb", [128, 4, 128], BF16)
                WXb = sb(s3, "WXb", [128, 4, 128], BF16)
                P.dma('sp', cw[:], convw, writes=['cw'])
                P.dma('sp', cb[:], convb, writes=['cb'])
                P.dma('sp', bA[:], bga, writes=['bA'])
                P.dma('sp', bX[:], bgx, writes=['bX'])
                P.dma('sp', lamt[:], lam, writes=['lamt'])
                P.dma('sp', flag[:], flag_d, writes=['flag'])
                P.dma('pool', WAb[:], WA.rearrange("j p o -> p j o"), writes=['WAb'])
                P.dma('pool', WXb[:], WX.rearrange("j p o -> p j o"), writes=['WXb'])
                op('act', 'activation', ['lamt'], ['cL'], out=cL[:], in_=lamt[:], func=AF.Exp, scale=-1.0)
                op('act', 'activation', ['cL'], ['cL'], out=cL[:], in_=cL[:], func=AF.Ln, bias=1.0)
                op('dve', 'tensor_scalar_mul', ['cL'], ['cL2'], cL2[:], cL[:], -16.0)
                op('dve', 'tensor_scalar_mul', ['cL'], ['cL'], cL[:], cL[:], -8.0)
                op('dve', 'memset', [], ['carry'], carry[:], 0.0)
                nbA = sb(s3, "nbA", [128, 4], F32)
                nbX = sb(s3, "nbX", [128, 4], F32)
                op('dve', 'tensor_scalar_mul', ['bA'], ['nbA'], nbA[:], bA[:], -1.0)
                op('dve', 'tensor_scalar_mul', ['bX'], ['nbX'], nbX[:], bX[:], -1.0)
                W = 512
                NB2 = 2
                def mk(name, shape, dt):
                    return [sb(s3, "%s_%d" % (name, i), shape, dt) for i in range(NB2)]
                xl = mk("xl", [128, 3 + W], F32)
                xc = mk("xc", [128, W], F32)
                xcb = mk("xcb", [128, W], BF16)
                rr = mk("rr", [128, W], F32)
                ii = mk("ii", [128, W], F32)
                aa = mk("aa", [128, W], F32)
                m2 = mk("m2", [128, W], F32)
                hh = mk("hh", [128, W], F32)
                gyl = mk("gyl", [128, W], BF16)
                sq = mk("sq", [128, W], BF16)
                lop = mk("lop", [128, W], BF16)
                npc_box = [0]

                def lru_piece(j, pc):
                    b = npc_box[0] % NB2
                    npc_box[0] += 1
                    K_ = lambda n: '%s_%d' % (n, b)
                    if pc == 0:
                        op('dve', 'memset', [], [K_('xl')], xl[b][:, 0:3], 0.0)
                        P.dma('sp', xl[b][:, 3:3 + W], xlru_s[j, :, 0:W], reads=['xlru_s'], writes=[K_('xl')])
                    else:
                        P.dma('sp', xl[b][:, :], xlru_s[j, :, pc * W - 3:(pc + 1) * W], reads=['xlru_s'], writes=[K_('xl')])
                    if pc >= 4:
                        oc = (pc - 4) * W
                        P.dma('sp', gyl[b][:], gy_s[j, :, oc:oc + W], reads=['gy_s'], writes=[K_('gyl')])
                    op('dve', 'tensor_scalar', [K_('xl'), 'cw', 'cb'], [K_('xc')], xc[b][:], xl[b][:, 0:W],
                       cw[:, j * 4:j * 4 + 1], cb[:, j:j + 1], ALU.mult, ALU.add)
                    for k in range(1, 4):
                        op('dve', 'scalar_tensor_tensor', [K_('xl'), 'cw', K_('xc')], [K_('xc')], out=xc[b][:], in0=xl[b][:, k:k + W],
                           scalar=cw[:, j * 4 + k:j * 4 + k + 1], in1=xc[b][:], op0=ALU.mult, op1=ALU.add)
                    op('pool', 'tensor_copy', [K_('xc')], [K_('xcb')], xcb[b][:], xc[b][:])
                    mm(pT0f, WAb[:, j, :], xcb[b][:, :], ['WAb', K_('xcb')], ['pT0'], True, True)
                    op('act', 'activation', ['pT0', 'nbA'], [K_('rr')], out=rr[b][:, :], in_=pT0f,
                       func=AF.Exp, bias=nbA[:, j:j + 1], scale=-1.0)
                    mm(pT0f, WXb[:, j, :], xcb[b][:, :], ['WXb', K_('xcb')], ['pT0'], True, True)
                    op('act', 'activation', ['pT0', 'nbX'], [K_('ii')], out=ii[b][:, :], in_=pT0f,
                       func=AF.Exp, bias=nbX[:, j:j + 1], scale=-1.0)
                    op('act', 'activation', [K_('rr')], [K_('rr')], out=rr[b][:], in_=rr[b][:], func=AF.Ln, bias=1.0)
                    op('act', 'activation', [K_('rr')], [K_('rr')], out=rr[b][:], in_=rr[b][:], func=AF.Exp, scale=-1.0)
                    op('act', 'activation', [K_('ii')], [K_('ii')], out=ii[b][:], in_=ii[b][:], func=AF.Ln, bias=1.0)
                    op('act', 'activation', [K_('ii')], [K_('ii')], out=ii[b][:], in_=ii[b][:], func=AF.Exp, scale=-1.0)
                    op('act', 'activation', [K_('rr'), 'cL'], [K_('aa')], out=aa[b][:], in_=rr[b][:], func=AF.Exp, scale=cL[:, j:j + 1])
                    op('act', 'activation', [K_('rr'), 'cL2'], [K_('m2')], out=m2[b][:], in_=rr[b][:], func=AF.Exp, scale=cL2[:, j:j + 1])
                    op('act', 'activation', [K_('m2')], [K_('m2')], out=m2[b][:], in_=m2[b][:], func=AF.Ln, scale=-1.0, bias=1.0)
                    op('act', 'activation', [K_('m2')], [K_('m2')], out=m2[b][:], in_=m2[b][:], func=AF.Exp, scale=0.5)
                    op('pool', 'tensor_tensor', [K_('ii'), K_('xc')], [K_('ii')], ii[b][:], ii[b][:], xc[b][:], ALU.mult)
                    op('pool', 'tensor_tensor', [K_('ii'), K_('m2')], [K_('ii')], ii[b][:], ii[b][:], m2[b][:], ALU.mult)
                    if pc == 4:
                        op('dve', 'tensor_tensor', ['carry', 'flag'], ['carry'], carry[:, j:j + 1], carry[:, j:j + 1],
                           flag[:], ALU.mult)
                    op('dve', 'tensor_tensor_scan', [K_('aa'), K_('ii'), 'carry'], [K_('hh')], out=hh[b][:], data0=aa[b][:], data1=ii[b][:],
                       initial=carry[:, j:j + 1], op0=ALU.mult, op1=ALU.add)
                    op('dve', 'tensor_copy', [K_('hh')], ['carry'], carry[:, j:j + 1], hh[b][:, W - 1:W])
                    if pc >= 4:
                        oc = (pc - 4) * W
                        op('pool', 'tensor_tensor', [K_('hh'), K_('gyl')], [K_('lop')], lop[b][:], hh[b][:], gyl[b][:], ALU.mult)
                        P.dma('sp', lo_s[j, :, oc:oc + W], lop[b][:], reads=[K_('lop')], writes=['lo_s'])
                        op('pool', 'tensor_tensor', [K_('lop')], [K_('sq')], sq[b][:], lop[b][:], lop[b][:], ALU.mult)
                        for t in range(4):
                            pe_raw('matmul', [K_('sq'), 'onesb'], ['pT1'], sig=(t == 3), argskw=ARGS(pT1f[:, t:t + 1], sq[b][:, t * 128:(t + 1) * 128], onesb[:, 0:1],
                                                                start=True, stop=True))
                        t0 = (pc - 4) * 4
                        op('dve', 'tensor_tensor', ['pT1', 'ssl'], ['ssl'], ssl[:, t0:t0 + 4], ssl[:, t0:t0 + 4], pT1f[:, 0:4], ALU.add)
                lru_list = [(j, pc) for j in range(4) for pc in range(8)]
                gsb = sb(s3, "gsb", [128, NH, 16], F32)
                top8 = sb(s3, "top8", [128, NH, 8], F32)
                sel = sb(s3, "sel", [128, NH, 16], F32)
                mvb = sb(s3, "mvb", [128, NH, 16], BF16)
                for qt in range(16):
                    for h in range(NH):
                        pe_raw('matmul', ['qT%d' % h, 'kmb'], ['pM'], sig=(h == NH - 1), argskw=ARGS(pM[:, h * 16:(h + 1) * 16], qT[h][0:64, qt * 128:(qt + 1) * 128],
                                                            kmb[:, h, :], start=True, stop=True))
                    op('dve', 'tensor_tensor', ['pM', 'gmask'], ['gsb'], gsb[:],
                       pM[:, 0:128].rearrange("p (h n) -> p h n", n=16),
                       gmask[:, qt:qt + 1, :].to_broadcast([128, NH, 16]), ALU.add)
                    for h in range(NH):
                        op('dve', 'max', ['gsb'], ['top8'], out=top8[:, h, :], in_=gsb[:, h, :])
                    op('dve', 'tensor_tensor', ['gsb', 'top8'], ['sel'], sel[:], gsb[:],
                       top8[:, :, 2:3].to_broadcast([128, NH, 16]), ALU.is_ge)
                    op('dve', 'scalar_tensor_tensor', ['gsb', 'sel'], ['sel'], out=sel[:], in0=gsb[:], scalar=-1e29,
                       in1=sel[:], op0=ALU.is_gt, op1=ALU.mult)
                    op('dve', 'tensor_tensor', ['sel', 'ownhot'], ['sel'], sel[:], sel[:],
                       ownhot[:, qt:qt + 1, :].to_broadcast([128, NH, 16]), ALU.add)
                    op('dve', 'tensor_scalar', ['sel'], ['mvb'], mvb[:], sel[:], -1.0, -NEGM, ALU.add, ALU.mult)
                    for h in range(NH):
                        pe_raw('transpose', ['mvb', 'ident'], ['pT1'], sig=(h == NH - 1), argskw=ARGS(pT1[0:16, h * 128:(h + 1) * 128], mvb[:, h, :], ident[:]))
                    for h in range(NH):
                        op('act' if qt % 2 == 0 else 'dve', 'copy' if qt % 2 == 0 else 'tensor_copy', ['pT1'],
                           ['qTm%d' % h], qT[h][64:80, qt * 128:(qt + 1) * 128], pT1[0:16, h * 128:(h + 1) * 128])
                if DBG:
                    for h in (0, 1, 7):
                        P.dma('sp', dout("d_qm%d" % h, [16, NOWN], BF16), qT[h][64:80, :], reads=['qTm%d' % h])
                    P.dma('sp', dout("d_biasD", [128, 8 * 128]), biasD[:].rearrange("p h n -> p (h n)"), reads=['biasD'])
                    P.dma('sp', dout("d_biasS", [128, 8 * 128]), biasS[:].rearrange("p h n -> p (h n)"), reads=['biasS'])
                PT = [sb(s3, "PT%d" % i, [128, 512], BF16) for i in range(3)]
                tmpS = [sb(s3, "tmpS%d" % i, [128, 128], F32) for i in range(2)]
                osb = sb(s3, "osb", [65, 512], F32)
                sqa = sb(s3, "sqa", [128, 512], BF16)
                SCALE = HD ** -0.5
                npt = 0
                nts = 0
                nonlocal_nsb = [0]
                Sb = [(pS0, 'pS0'), (pS1, 'pS1'), (pA, 'pA')]
                Ob = [(pO, 'pO'), (pB, 'pB')]
                nob = 0
                for cq in range(4):
                    c = 4 + cq
                    for h in range(NH):
                        if lru_list:
                            lru_piece(*lru_list.pop(0))
                        j, s = h // 2, h % 2
                        qr = ['qT%d' % h, 'qTm%d' % h]
                        kr = ['kT%d' % h, 'kTind%d' % h]
                        nkt = 4 * c + 4
                        pOc, pOk = Ob[nob % 2]
                        nob += 1

                        def geom(kt):
                            qlo = max(kt, 4 * c)
                            n0 = (qlo - 4 * c) * 128
                            return qlo, n0

                        def issue_S(kt):
                            nonlocal_nsb[0] += 1
                            pS, pSk = Sb[nonlocal_nsb[0] % 3]
                            qlo, n0 = geom(kt)
                            mm(pS[:, n0:512], kT[h][0:96, kt * 128:(kt + 1) * 128],
                               qT[h][0:96, cq * 512 + n0: cq * 512 + 512], qr + kr, [pSk], True, True)
                            return pS, pSk

                        pendq = [issue_S(0)]
                        if nkt > 1:
                            pendq.append(issue_S(1))
                        for kt in range(nkt):
                            pS, pSk = pendq.pop(0)
                            qlo, n0 = geom(kt)
                            pt = PT[npt % 3]
                            ptk = 'PT%d' % (npt % 3)
                            npt += 1
                            col = n0
                            nearks = []
                            for qtile in range(qlo, 4 * c + 4):
                                d = qtile - kt
                                if d > 1:
                                    break
                                bt = biasD if d == 0 else biasS
                                ts_, tsk = tmpS[nts % 2], 'tmpS%d' % (nts % 2)
                                nts += 1
                                op('dve', 'scalar_tensor_tensor', [pSk, 'biasD', 'biasS'], [tsk], out=ts_[:],
                                   in0=pS[:, col:col + 128], scalar=SCALE, in1=bt[:, h, :], op0=ALU.mult, op1=ALU.add)
                                op('act', 'activation', [tsk], [ptk], out=pt[:, col:col + 128], in_=ts_[:], func=AF.Exp)
                                nearks.append(tsk)
                                col += 128
                            if col < 512:
                                op('act', 'activation', [pSk, 'cfar'] + nearks, [ptk], out=pt[:, col:512], in_=pS[:, col:512],
                                   func=AF.Exp, bias=cfar[:, h:h + 1], scale=SCALE)
                            mm(pOc[0:65, n0:512], Vt[:, kt, h * 65:(h + 1) * 65], pt[:, n0:512], ['Vt', ptk], [pOk],
                               kt == 0, kt == nkt - 1)
                            if kt + 2 < nkt:
                                pendq.append(issue_S(kt + 2))
                        op('dve', 'tensor_copy', [pOk], ['osb', 'osbr'], osb[:, :], pOc[0:65, :])
                        op('dve', 'reciprocal', ['osb'], ['osbr'], osb[64:65, :], osb[64:65, :])
                        pe_raw('matmul', ['osbr', 'ones32'], ['pM'], argskw=ARGS(pM[0:64, :], ones32[64:65, 0:64], osb[64:65, :],
                                                            start=True, stop=True))
                        op('dve', 'tensor_tensor', ['osb', 'pM'], ['apair'], apair[s * 64:(s + 1) * 64, :],
                           osb[0:64, :], pM[0:64, :], ALU.mult)
                        if s == 1:
                            P.dma('sp', a_s[j, :, cq * 512:(cq + 1) * 512], apair[:], reads=['apair'], writes=['a_s'])
                            op('pool', 'tensor_tensor', ['apair'], ['sqa'], sqa[:], apair[:], apair[:], ALU.mult)
                            for t in range(4):
                                pe_raw('matmul', ['sqa', 'onesb'], ['pM'], sig=(t == 3), argskw=ARGS(pM[:, t:t + 1], sqa[:, t * 128:(t + 1) * 128], onesb[:, 0:1],
                                                                    start=True, stop=True))
                            op('dve', 'tensor_tensor', ['pM', 'ssa'], ['ssa'], ssa[:, cq * 4:cq * 4 + 4],
                               ssa[:, cq * 4:cq * 4 + 4], pM[:, 0:4], ALU.add)
                while lru_list:
                    lru_piece(*lru_list.pop(0))
                if DBG:
                    P.dma('sp', dout("d_ssa", [128, 16]), ssa[:], reads=['ssa'])
                    P.dma('sp', dout("d_ssl", [128, 16]), ssl[:], reads=['ssl'])
                P.barrier()
                if STOP == 3:
                    raise _Stop()

        P.flush()
        P.schedule = ('4' in SCHED_PH) and (os.environ.get('MK_SCHED', '1') == '1')
        lnB = sb(top, "lnB", [128, 4, D], F32)
        u2T = sb(top, "u2T", [128, 8, NOWN], BF16)
        P.dma('sp', lnB[:].rearrange("p a d -> p (a d)"),
              lnv.rearrange("a d -> (a d)").partition_broadcast(128), writes=['lnB'])
        u2r = ['u2T%d' % k for k in range(8)]
        wrb = sb(top, "wrb", [128, 8, 36], BF16)
        brB = sb(top, "brB", [128, 36], F32)
        P.dma('pool', wrb[:], w_r.rearrange("(k p) n -> p k n", p=128), writes=['wrb'])
        P.dma('sp', brB[:], b_r.rearrange("a n -> (a n)").partition_broadcast(128), writes=['brB'])
        gate = sb(top, "gate", [128, 16, NE], F32)
        lg = sb(top, "lg", [128, 36], F32)
        gmax = sb(top, "gmax", [128, 1], F32)
        ngmax = sb(top, "ngmax", [128, 1], F32)
        gex = sb(top, "gex", [128, 4], F32)
        gsum = sb(top, "gsum", [128, 1], F32)
        gtop = sb(top, "gtop", [128, 1], F32)
        goh = sb(top, "goh", [128, 4], F32)
        esel = sb(top, "esel", [128, 4, 8], F32)
        ein = sb(top, "ein", [128, 8], F32)
        et8 = sb(top, "et8", [128, 8], F32)
        nl1 = sb(top, "nl1", [128, 1], F32)
        eex = sb(top, "eex", [128, 8], F32)
        esl = sb(top, "esl", [128, 8], F32)
        eden = sb(top, "eden", [128, 1], F32)
        with contextlib.ExitStack() as s4:
            def router(tt):
                for k in range(8):
                    mm(pM[:, 0:36], u2T[:, k, tt * 128:(tt + 1) * 128], wrb[:, k, :], [u2r[k], 'wrb'], ['pM'], k == 0, k == 7)
                op('dve', 'tensor_tensor', ['pM', 'brB'], ['lg'], lg[:], pM[:, 0:36], brB[:], ALU.add)
                op('dve', 'tensor_reduce', ['lg'], ['gmax'], out=gmax[:], in_=lg[:, 0:4], axis=AX.X, op=ALU.max)
                op('dve', 'tensor_scalar_mul', ['gmax'], ['ngmax'], ngmax[:], gmax[:], -1.0)
                op('act', 'activation', ['lg', 'ngmax'], ['gex'], out=gex[:], in_=lg[:, 0:4], func=AF.Exp, bias=ngmax[:])
                op('dve', 'tensor_reduce', ['gex'], ['gsum'], out=gsum[:], in_=gex[:], axis=AX.X, op=ALU.add)
                op('dve', 'reciprocal', ['gsum'], ['gtop'], gtop[:], gsum[:])
                op('dve', 'tensor_tensor', ['lg', 'gmax'], ['goh'], goh[:], lg[:, 0:4], gmax[:].to_broadcast([128, 4]), ALU.is_ge)
                op('dve', 'tensor_tensor', ['lg', 'goh'], ['esel'], esel[:], lg[:, 4:36].rearrange("p (g e) -> p g e", e=8),
                   goh[:].unsqueeze(2).to_broadcast([128, 4, 8]), ALU.mult)
                op('dve', 'tensor_reduce', ['esel'], ['ein'], out=ein[:], in_=esel[:].rearrange("p g e -> p e g"),
                   axis=AX.X, op=ALU.add)
                op('dve', 'max', ['ein'], ['et8'], out=et8[:], in_=ein[:])
                op('dve', 'tensor_scalar_mul', ['et8'], ['nl1'], nl1[:], et8[:, 0:1], -1.0)
                op('act', 'activation', ['ein', 'nl1'], ['eex'], out=eex[:], in_=ein[:], func=AF.Exp, bias=nl1[:])
                op('dve', 'tensor_tensor', ['ein', 'et8'], ['esl'], esl[:], ein[:], et8[:, 1:2].to_broadcast([128, 8]), ALU.is_ge)
                op('dve', 'tensor_tensor', ['esl', 'eex'], ['esl'], esl[:], esl[:], eex[:], ALU.mult)
                op('dve', 'tensor_reduce', ['esl'], ['eden'], out=eden[:], in_=esl[:], axis=AX.X, op=ALU.add)
                op('dve', 'reciprocal', ['eden'], ['eden'], eden[:], eden[:])
                op('dve', 'tensor_tensor', ['eden', 'gtop'], ['eden'], eden[:], eden[:], gtop[:], ALU.mult)
                op('dve', 'tensor_scalar', ['esl', 'eden'], ['esl'], esl[:], esl[:], eden[:, 0:1], None, ALU.mult)
                op('dve', 'tensor_tensor', ['goh', 'esl'], ['gate'], gate[:, tt, :].rearrange("p (g e) -> p g e", e=8),
                   goh[:].unsqueeze(2).to_broadcast([128, 4, 8]), esl[:].unsqueeze(1).to_broadcast([128, 4, 8]), ALU.mult)
            loT = sb(s4, "loT", [128, 4, NOWN], BF16)
            aTp = sb(s4, "aTp", [128, 4, NOWN], BF16)
            for jj in range(4):
                P.dma('sp', loT[:, jj, :], lo_s[jj], reads=['lo_s'], writes=['loT%d' % jj])
                P.dma('sp', aTp[:, jj, :], a_s[jj], reads=['a_s'], writes=['aTp%d' % jj])
            if DBG:
                P.dma('sp', dout("d_loT", [128, 4 * NOWN], BF16), loT[:].rearrange("p j n -> p (j n)"),
                      reads=['loT%d' % j for j in range(4)])
                P.dma('sp', dout("d_aTp", [128, 4 * NOWN], BF16), aTp[:].rearrange("p j n -> p (j n)"),
                      reads=['aTp%d' % j for j in range(4)])
            woutb = sb(s4, "woutb", [128, 8, D], BF16)
            wo32 = [sb(s4, "wo32_%d" % i, [128, D], F32) for i in range(2)]
            gl = sb(s4, "gl", [128, 8], F32)
            P.dma('sp', gl[:, 0:4], glru, writes=['gl'])
            P.dma('sp', gl[:, 4:8], gattn, writes=['gl'])
            for k in range(8):
                wb, wk = wo32[k % 2], 'wo32_%d' % (k % 2)
                P.dma('sp', wb[:], w_out[k * 128:(k + 1) * 128, :], writes=[wk])
                op('act', 'activation', [wk, 'gl'], ['woutb%d' % k], out=woutb[:, k, :], in_=wb[:], func=AF.Identity, scale=gl[:, k:k + 1])
            NB4 = 3
            xo = [sb(s4, "xo%d" % i, [128, D], F32) for i in range(NB4)]
            mix = [sb(s4, "mix%d" % i, [128, D], F32) for i in range(NB4)]
            zz = [sb(s4, "zz%d" % i, [128, D], F32) for i in range(NB4)]
            x1 = [sb(s4, "x1_%d" % i, [128, D], F32) for i in range(NB4)]
            xn2 = [sb(s4, "xn2_%d" % i, [128, D], BF16) for i in range(NB4)]
            NST = 6
            st6 = [sb(s4, "st6b%d" % i, [128, 2, 6], F32) for i in range(NST)]
            mv = [sb(s4, "mvb%d" % i, [128, 2], F32) for i in range(NST)]
            rstd = [sb(s4, "rstdb%d" % i, [128, 1], F32) for i in range(NST)]
            nb = [sb(s4, "nbb%d" % i, [128, 1], F32) for i in range(NST)]
            ve4 = [sb(s4, "veb%d" % i, [128, 1], F32) for i in range(NST)]
            rl = sb(s4, "rl", [128, 16], F32)
            ra = sb(s4, "ra", [128, 16], F32)
            op('dve', 'tensor_scalar', ['ssl'], ['rl'], rl[:], ssl[:], 1.0 / 512, EPS, ALU.mult, ALU.add)
            op('pool', 'tensor_tensor', ['rl', 'mhalf'], ['rl'], rl[:], rl[:], mhalf[:, 0:16], ALU.pow)
            op('dve', 'tensor_scalar', ['ssa'], ['ra'], ra[:], ssa[:], 1.0 / 512, EPS, ALU.mult, ALU.add)
            op('pool', 'tensor_tensor', ['ra', 'mhalf'], ['ra'], ra[:], ra[:], mhalf[:, 0:16], ALU.pow)

            def ln_stats(src, srck, i):
                for hf in range(2):
                    op('dve', 'bn_stats', [srck], ['st6_%d' % i], out=st6[i][:, hf, :], in_=src[:, hf * 512:(hf + 1) * 512])
                op('dve', 'bn_aggr', ['st6_%d' % i], ['mv_%d' % i], out=mv[i][:], in_=st6[i][:].rearrange("p a b -> p (a b)"))
                op('dve', 'tensor_scalar_add', ['mv_%d' % i], ['ve_%d' % i], ve4[i][:], mv[i][:, 1:2], EPS)
                op('pool', 'tensor_tensor', ['ve_%d' % i, 'mhalf'], ['rstd_%d' % i], rstd[i][:], ve4[i][:], mhalf[:, 0:1], ALU.pow)
                op('dve', 'scalar_tensor_tensor', ['mv_%d' % i, 'rstd_%d' % i], ['nb_%d' % i], out=nb[i][:], in0=mv[i][:, 0:1],
                   scalar=-1.0, in1=rstd[i][:], op0=ALU.mult, op1=ALU.mult)

            for tt in range(16):
                b = tt % NB4
                sa_, sb_ = (2 * tt) % NST, (2 * tt + 1) % NST
                xok, mixk, zzk, x1k, xn2k = 'xo%d' % b, 'mix%d' % b, 'zz%d' % b, 'x1_%d' % b, 'xn2_%d' % b
                P.dma('sp', xo[b][:], xctx[OWN0 + tt * 128: OWN0 + (tt + 1) * 128, :], writes=[xok])
                for hf in range(2):
                    pa, pak, pb, pbk = (pA, 'pA', pB, 'pB') if hf == 0 else (pS0, 'pS0', pS1, 'pS1')
                    for jj in range(4):
                        mm(pa[:, :], loT[:, jj, tt * 128:(tt + 1) * 128], woutb[:, jj, hf * 512:(hf + 1) * 512],
                           ['loT%d' % jj, 'woutb%d' % jj], [pak], jj == 0, jj == 3)
                    for jj in range(4):
                        mm(pb[:, :], aTp[:, jj, tt * 128:(tt + 1) * 128], woutb[:, 4 + jj, hf * 512:(hf + 1) * 512],
                           ['aTp%d' % jj, 'woutb%d' % (4 + jj)], [pbk], jj == 0, jj == 3)
                    op('act', 'activation', [pak, 'rl'], [mixk], out=mix[b][:, hf * 512:(hf + 1) * 512], in_=pa[:, :],
                       func=AF.Identity, scale=rl[:, tt:tt + 1])
                    op('dve', 'scalar_tensor_tensor', [pbk, 'ra', mixk], [mixk], out=mix[b][:, hf * 512:(hf + 1) * 512],
                       in0=pb[:, :], scalar=ra[:, tt:tt + 1], in1=mix[b][:, hf * 512:(hf + 1) * 512], op0=ALU.mult, op1=ALU.add)
                op('pool', 'tensor_tensor', [mixk, 'g1B'], [zzk], zz[b][:], mix[b][:], g1B[:], ALU.mult)
                op('dve', 'scalar_tensor_tensor', [xok, zzk], [zzk], out=zz[b][:], in0=xo[b][:], scalar=ALPHA, in1=zz[b][:],
                   op0=ALU.mult, op1=ALU.add)
                ln_stats(zz[b], zzk, sa_)
                op('act', 'activation', [zzk, 'rstd_%d' % sa_, 'nb_%d' % sa_], [x1k], out=x1[b][:], in_=zz[b][:], func=AF.Identity,
                   bias=nb[sa_][:], scale=rstd[sa_][:])
                op('dve', 'tensor_tensor', [x1k, 'lnB'], [x1k], x1[b][:], x1[b][:], lnB[:, 0, :], ALU.mult)
                op('dve', 'tensor_tensor', [x1k, 'lnB'], [x1k], x1[b][:], x1[b][:], lnB[:, 1, :], ALU.add)
                P.dma('sp', x1_s[tt * 128:(tt + 1) * 128, :], x1[b][:], reads=[x1k], writes=['x1_s'])
                ln_stats(x1[b], x1k, sb_)
                op('act', 'activation', [x1k, 'rstd_%d' % sb_, 'nb_%d' % sb_], [xn2k], out=xn2[b][:], in_=x1[b][:], func=AF.Identity,
                   bias=nb[sb_][:], scale=rstd[sb_][:])
                pt, ptk = (pT0, 'pT0') if tt % 2 == 0 else (pT1, 'pT1')
                for k in range(8):
                    pe_raw('transpose', [xn2k, 'ident'], [ptk], sig=(k == 7), argskw=ARGS(pt[:, k * 128:(k + 1) * 128], xn2[b][:, k * 128:(k + 1) * 128], ident[:]))
                for k in range(8):
                    if tt % 2 == 0:
                        op('act', 'activation', [ptk, 'sc2p', 'modT'], ['u2T%d' % k], out=u2T[:, k, tt * 128:(tt + 1) * 128],
                           in_=pt[:, k * 128:(k + 1) * 128], func=AF.Identity, bias=modT[:, 24 + k:25 + k],
                           scale=sc2p[:, k:k + 1])
                    else:
                        op('dve', 'tensor_scalar', [ptk, 'sc2p', 'modT'], ['u2T%d' % k], u2T[:, k, tt * 128:(tt + 1) * 128],
                           pt[:, k * 128:(k + 1) * 128], sc2p[:, k:k + 1], modT[:, 24 + k:25 + k], ALU.mult, ALU.add)
                router(tt)
            if DBG:
                P.dma('sp', dout("d_u2T", [128, 8 * NOWN], BF16), u2T[:].rearrange("p k n -> p (k n)"),
                      reads=['u2T%d' % k for k in range(8)])
            P.barrier()
            if STOP == 4:
                raise _Stop()

        P.flush()
        P.schedule = ('5' in SCHED_PH) and (os.environ.get('MK_SCHED', '1') == '1')
        with contextlib.ExitStack() as s5:
            if DBG:
                P.dma('sp', dout("d_gate", [128, 16 * NE]), gate[:].rearrange("p t e -> p (t e)"), reads=['gate'])
            NS = 3
            w1b = [sb(s5, "w1b%d" % i, [128, 8, DE], BF16) for i in range(NS)]
            w3b = [sb(s5, "w3b%d" % i, [128, 8, DE], BF16) for i in range(NS)]
            w2b = [sb(s5, "w2b%d" % i, [128, 2, D], BF16) for i in range(NS)]
            yacc = sb(s5, "yacc", [128, 16, D], F32)
            ssi = [sb(s5, "ssi%d" % i, [128, 512], F32) for i in range(2)]
            hdn = [sb(s5, "hdn%d" % i, [128, 512], BF16) for i in range(4)]
            nh_ = 0
            ny = 0
            for e in range(NE):
                sl = e % NS
                P.dma('pool', w1b[sl][:], w1[e].rearrange("(k p) f -> p k f", p=128), writes=['w1b%d' % sl])
                P.dma('pool', w3b[sl][:], w3[e].rearrange("(k p) f -> p k f", p=128), writes=['w3b%d' % sl])
                P.dma('pool', w2b[sl][:], w2[e].rearrange("(c p) d -> p c d", p=128), writes=['w2b%d' % sl])
                for tc in range(4):
                    hk = []
                    for fc in range(2):
                        p1, p1k, p3, p3k = (pA, 'pA', pB, 'pB') if fc == 0 else (pS0, 'pS0', pS1, 'pS1')
                        for k in range(8):
                            mm(p1[:, :], w1b[sl][:, k, fc * 128:(fc + 1) * 128], u2T[:, k, tc * 512:(tc + 1) * 512],
                               ['w1b%d' % sl, u2r[k]], [p1k], k == 0, k == 7)
                        for k in range(8):
                            mm(p3[:, :], w3b[sl][:, k, fc * 128:(fc + 1) * 128], u2T[:, k, tc * 512:(tc + 1) * 512],
                               ['w3b%d' % sl, u2r[k]], [p3k], k == 0, k == 7)
                        si, sik = ssi[fc], 'ssi%d' % fc
                        hd, hdk = hdn[nh_ % 4], 'hdn%d' % (nh_ % 4)
                        nh_ += 1
                        op('act', 'activation', [p1k], [sik], out=si[:], in_=p1[:, :], func=AF.Silu)
                        op('dve', 'tensor_tensor', [sik, p3k], [hdk], hd[:], si[:], p3[:, :], ALU.mult)
                        hk.append((hd, hdk))
                    for t in range(4):
                        tt = tc * 4 + t
                        for hf in range(2):
                            py, pyk = (pO, 'pO') if ny % 2 == 0 else (pM, 'pM')
                            ny += 1
                            for fc in range(2):
                                mm(py[:, :], hk[fc][0][:, t * 128:(t + 1) * 128], w2b[sl][:, fc, hf * 512:(hf + 1) * 512],
                                   [hk[fc][1], 'w2b%d' % sl], [pyk], fc == 0, fc == 1)
                            if e == 0:
                                op('dve', 'tensor_scalar', [pyk, 'gate'], ['yacc%d' % tt], yacc[:, tt, hf * 512:(hf + 1) * 512],
                                   py[:, :], gate[:, tt, e:e + 1], None, ALU.mult)
                            else:
                                op('dve', 'scalar_tensor_tensor', [pyk, 'gate', 'yacc%d' % tt], ['yacc%d' % tt],
                                   out=yacc[:, tt, hf * 512:(hf + 1) * 512], in0=py[:, :], scalar=gate[:, tt, e:e + 1],
                                   in1=yacc[:, tt, hf * 512:(hf + 1) * 512], op0=ALU.mult, op1=ALU.add)
            NB5 = 3
            x1l = [sb(s5, "x1l%d" % i, [128, D], F32) for i in range(NB5)]
            zf = [sb(s5, "zf%d" % i, [128, D], F32) for i in range(NB5)]
            of = [sb(s5, "of%d" % i, [128, D], F32) for i in range(NB5)]
            st6 = [sb(s5, "st6c%d" % i, [128, 2, 6], F32) for i in range(NB5)]
            mv = [sb(s5, "mvc%d" % i, [128, 2], F32) for i in range(NB5)]
            rstd = [sb(s5, "rstdc%d" % i, [128, 1], F32) for i in range(NB5)]
            nb = [sb(s5, "nbc%d" % i, [128, 1], F32) for i in range(NB5)]
            ve5 = [sb(s5, "vec%d" % i, [128, 1], F32) for i in range(NB5)]
            for tt in range(16):
                b = tt % NB5
                x1lk, zfk, ofk = 'x1l%d' % b, 'zf%d' % b, 'of%d' % b
                P.dma('sp', x1l[b][:], x1_s[tt * 128:(tt + 1) * 128, :], reads=['x1_s'], writes=[x1lk])
                op('pool', 'tensor_tensor', ['yacc%d' % tt, 'g2B'], [zfk], zf[b][:], yacc[:, tt, :], g2B[:], ALU.mult)
                op('dve', 'scalar_tensor_tensor', [x1lk, zfk], [zfk], out=zf[b][:], in0=x1l[b][:], scalar=ALPHA, in1=zf[b][:],
                   op0=ALU.mult, op1=ALU.add)
                for hf in range(2):
                    op('dve', 'bn_stats', [zfk], ['st6f%d' % b], out=st6[b][:, hf, :], in_=zf[b][:, hf * 512:(hf + 1) * 512])
                op('dve', 'bn_aggr', ['st6f%d' % b], ['mvf%d' % b], out=mv[b][:], in_=st6[b][:].rearrange("p a b -> p (a b)"))
                op('dve', 'tensor_scalar_add', ['mvf%d' % b], ['vef%d' % b], ve5[b][:], mv[b][:, 1:2], EPS)
                op('pool', 'tensor_tensor', ['vef%d' % b, 'mhalf'], ['rstdf%d' % b], rstd[b][:], ve5[b][:], mhalf[:, 0:1], ALU.pow)
                op('dve', 'scalar_tensor_tensor', ['mvf%d' % b, 'rstdf%d' % b], ['nbf%d' % b], out=nb[b][:], in0=mv[b][:, 0:1],
                   scalar=-1.0, in1=rstd[b][:], op0=ALU.mult, op1=ALU.mult)
                op('act', 'activation', [zfk, 'rstdf%d' % b, 'nbf%d' % b], [ofk], out=of[b][:], in_=zf[b][:], func=AF.Identity,
                   bias=nb[b][:], scale=rstd[b][:])
                op('dve', 'tensor_tensor', [ofk, 'lnB'], [ofk], of[b][:], of[b][:], lnB[:, 2, :], ALU.mult)
                op('dve', 'tensor_tensor', [ofk, 'lnB'], [ofk], of[b][:], of[b][:], lnB[:, 3, :], ALU.add)
                P.dma('sp', out_d[tt * 128:(tt + 1) * 128, :], of[b][:], reads=[ofk], writes=['out'])
            P.barrier()
    except _Stop:
        P.barrier()
    P.close()
    return nc, list(dbg.keys())


def _host_inputs(inp):
    f = lambda a: np.ascontiguousarray(np.asarray(a, dtype=np.float32))
    x = f(inp['x']); c = f(inp['c'])
    per_part = lambda v: np.ascontiguousarray(v.reshape(-1, 128).T)
    shared = {
        'w_ada': f(inp['w_ada'][0]),
        'b_ada': per_part(f(inp['b_ada'][0])),
        'b_ada_row': np.ascontiguousarray(f(inp['b_ada'][0])[None, :]),
        'w_in': f(inp['w_in'][0]),
        'convw': np.ascontiguousarray(f(inp['conv_w'][0]).T.reshape(4, 128, 4).transpose(1, 0, 2).reshape(128, 16)),
        'convb': per_part(f(inp['conv_b'][0])),
        'bga': per_part(f(inp['b_gate_a'][0]).reshape(-1)),
        'bgx': per_part(f(inp['b_gate_x'][0]).reshape(-1)),
        'lam': per_part(f(inp['lru_lambda'][0])),
        'relb': f(inp['rel_bias']),
        'glru': per_part(f(inp['norm_lru_g'][0])),
        'gattn': per_part(f(inp['norm_attn_g'][0])),
        'w_out': f(inp['w_out'][0]),
        'lnv': np.ascontiguousarray(np.stack([f(inp['ln1_g'][0]), f(inp['ln1_b'][0]), f(inp['ln2_g'][0]), f(inp['ln2_b'][0])])),
        'w_r': np.ascontiguousarray(np.concatenate([f(inp['w_router_group'][0]), f(inp['w_router_expert'][0])], axis=1)),
        'b_r': np.ascontiguousarray(np.concatenate([f(inp['b_router_group'][0]), f(inp['b_router_expert'][0])])[None, :]),
        'w1': f(inp['w1'][0]), 'w3': f(inp['w3'][0]), 'w2': f(inp['w2'][0]),
    }
    for nm, src in (('WA', 'w_gate_a'), ('WX', 'w_gate_x')):
        w = f(inp[src][0])
        bd = np.zeros((4, 128, 128), np.float32)
        for j in range(4):
            for s in range(2):
                bd[j, s * 64:(s + 1) * 64, s * 64:(s + 1) * 64] = w[2 * j + s]
        shared[nm] = bd
    i = np.arange(384)
    r = 255 - i
    bk = _t5_bucket_np(r)
    Roh = np.zeros((32, 384), np.float32)
    valid = (r >= 0) & (i < 383)
    Roh[bk[valid], i[valid]] = 1.0
    NEGr = np.tile(np.where(r < 0, NEGM, 0.0).astype(np.float32)[None, :], (8, 1))
    IND = np.zeros((16, CT), np.float32)
    for n in range(16):
        IND[n, n * BLK:(n + 1) * BLK] = 1.0
    shared['Roh'] = Roh; shared['NEGr'] = np.ascontiguousarray(NEGr); shared['IND'] = IND
    maps = []
    for core in range(8):
        b, half = core // 2, core % 2
        m = dict(shared)
        if half == 1:
            m['xctx'] = np.ascontiguousarray(x[b])
        else:
            m['xctx'] = np.ascontiguousarray(np.concatenate([np.zeros((2048, D), np.float32), x[b, :2048]], axis=0))
        m['cvec'] = per_part(c[b])
        gm = np.zeros((16, 16), np.float32); oh = np.zeros((16, 16), np.float32)
        for qt in range(16):
            own = 8 + qt // 2
            for n in range(16):
                ok = (n < own) and (half == 1 or n >= 8)
                gm[qt, n] = 0.0 if ok else -1e30
            oh[qt, own] = 1.0
        m['gmask'] = np.ascontiguousarray(np.tile(gm.reshape(1, 256), (128, 1)))
        m['ownhot'] = np.ascontiguousarray(np.tile(oh.reshape(1, 256), (128, 1)))
        m['flag'] = np.full((128, 1), float(half), np.float32)
        maps.append(m)
    return maps


_NC_CACHE = {}


def kernel(**inputs):
    if 'nc' not in _NC_CACHE:
        _NC_CACHE['nc'] = build_program()
    nc, dbgnames = _NC_CACHE['nc']
    maps = _host_inputs(inputs)
    if STOP < 5:
        for m in maps:
            for k in ('w1', 'w3', 'w2'):
                m.pop(k)
    res = run_bass_kernel_spmd(nc, maps, core_ids=list(range(8)))
    out = np.zeros((4, SEQ, D), np.float32)
    for core in range(8):
        b, half = core // 2, core % 2
        out[b, half * 2048:(half + 1) * 2048] = res.results[core]['out']
    if DBG:
        kernel.dbg = [{k: res.results[core][k] for k in dbgnames} for core in range(8)]
    return out
```

```python
import contextlib
import math
import numpy as np
import ml_dtypes
import concourse.bass as bass
import concourse.mybir as mybir
from concourse.bass_utils import run_bass_kernel_spmd

F32 = mybir.dt.float32
BF16 = mybir.dt.bfloat16
AF = mybir.ActivationFunctionType
ALU = mybir.AluOpType
AX = mybir.AxisListType

D = 1024
SEQ = 4096
CT = 4096
OWN0 = 2048
NOWN = 2048
NH = 8
HD = 64
BLK = 256
NBLK = 16
NE = 32
DE = 256
EPS = 1e-5
ALPHA = 2.0 ** 0.25
NEGM = -30000.0
DBG = False
import os
STOP = int(os.environ.get('MK_STOP', '9'))
NCH = int(os.environ.get('MK_NCH', '8'))
SKIP = set(os.environ.get('MK_SKIP', '').split(','))
SCHED_PH = set(os.environ.get('MK_SCHED_PH', '0,1,2,3,4,5').split(','))


class _Stop(Exception):
    pass


class Prog:
    def __init__(self, nc, n_dma_sems=24):
        self.nc = nc
        self.E = {'pe': nc.tensor, 'act': nc.scalar, 'dve': nc.vector, 'pool': nc.gpsimd, 'sp': nc.sync}
        self.sem = {}
        self.cnt = {}
        self._ctx = []
        for e in ['pe', 'act', 'dve', 'pool']:
            g = nc.semaphore('s_' + e)
            self.sem[e] = g.__enter__()
            self._ctx.append(g)
            self.cnt[e] = 0
        self.dsem = []
        for i in range(n_dma_sems):
            g = nc.semaphore('d_%d' % i)
            self.dsem.append(g.__enter__())
            self._ctx.append(g)
        self.dcnt = [0] * n_dma_sems
        self.dnext = 0
        self.dnext_sw = 0
        self.waited = {}
        self.lastw = {}
        self.reads = {}
        self.dirty = {e: False for e in self.cnt}
        self.rec = []
        self.tnow = 0.0
        self.schedule = (os.environ.get('MK_SCHED', '1') == '1')

    def _semh(self, key):
        return self.sem[key] if isinstance(key, str) else self.dsem[key]

    def _wait(self, eng, ev, same_ok):
        key, val, src = ev
        if src == eng and same_ok:
            return
        if self.waited.get((eng, key), 0) >= val:
            return
        self.E[eng].wait_ge(self._semh(key), val)
        self.waited[(eng, key)] = val

    def _deps(self, eng, reads, writes, is_dma=False):
        for r in reads:
            ev = self.lastw.get(r)
            if ev is not None:
                self._wait(eng, ev, same_ok=(eng == 'pe' and not is_dma))
        for w in writes:
            ev = self.lastw.get(w)
            if ev is not None:
                self._wait(eng, ev, same_ok=(eng == 'pe' and not is_dma))
            for ev in self.reads.get(w, ()):
                self._wait(eng, ev, same_ok=(eng == 'pe' and not is_dma))

    def _record(self, ev, reads, writes):
        for r in reads:
            lst = self.reads.setdefault(r, [])
            lst.append(ev)
            if len(lst) > 16:
                best = {}
                for k, v, s in lst:
                    if k not in best or best[k][1] < v:
                        best[k] = (k, v, s)
                self.reads[r] = list(best.values())
        for w in writes:
            self.lastw[w] = ev
            self.reads[w] = []

    def op(self, eng, fn, reads=(), writes=(), sig=True, est=0.5):
        self.rec.append(dict(kind='op', eng=eng, fn=fn, reads=tuple(reads), writes=tuple(writes), sig=sig, est=est))

    def dma(self, q, out, in_, reads=(), writes=(), **kw):
        try:
            nbytes = out.nbytes()
        except Exception:
            nbytes = 65536
        self.rec.append(dict(kind='dma', eng=q, out=out, in_=in_, kw=kw, reads=tuple(reads), writes=tuple(writes),
                             sig=True, est=2.0 + nbytes / 150e3))

    def _emit_op(self, eng, fn, reads, writes, sig):
        self._deps(eng, reads, writes)
        ins = fn()
        if sig:
            self.cnt[eng] += 1
            ins.then_inc(self.sem[eng], 1)
            ev = (eng, self.cnt[eng], eng)
            self.dirty[eng] = False
        else:
            ev = (eng, self.cnt[eng] + 1, eng)
            self.dirty[eng] = True
        self._record(ev, reads, writes)
        return ins

    def _emit_dma(self, q, out, in_, reads, writes, kw):
        half = len(self.dsem) // 2
        if q == 'pool':
            i = self.dnext_sw
            self.dnext_sw = (self.dnext_sw + 1) % half
        else:
            i = half + self.dnext
            self.dnext = (self.dnext + 1) % (len(self.dsem) - half)
        if self.dcnt[i] > 0:
            self._wait(q, (i, self.dcnt[i], 'dma'), same_ok=False)
        self._deps(q, reads, writes, is_dma=True)
        ins = self.E[q].dma_start(out=out, in_=in_, **kw)
        self.dcnt[i] += 16
        ins.then_inc(self.dsem[i], 16)
        ev = (i, self.dcnt[i], 'dma')
        self._record(ev, reads, writes)
        return ev

    def flush(self):
        rec = self.rec
        self.rec = []
        if not rec:
            return
        nodes = []
        cur_pe = None
        for r in rec:
            if r['kind'] == 'op' and r['eng'] == 'pe':
                if cur_pe is None:
                    cur_pe = dict(eng='pe', items=[], est=0.0)
                    nodes.append(cur_pe)
                cur_pe['items'].append(r)
                cur_pe['est'] += r['est']
                if r['sig']:
                    cur_pe = None
            else:
                if cur_pe is not None:
                    cur_pe['items'][-1]['sig'] = True
                    cur_pe = None
                nodes.append(dict(eng=r['eng'], items=[r], est=r['est'], isdma=(r['kind'] == 'dma')))
        if cur_pe is not None:
            cur_pe['items'][-1]['sig'] = True
            cur_pe = None
        n = len(nodes)
        lastw = {}
        readers = {}
        preds = [set() for _ in range(n)]
        for i, nd in enumerate(nodes):
            R = set(); Wr = set()
            for it in nd['items']:
                R.update(it['reads']); Wr.update(it['writes'])
            for r_ in R:
                if r_ in lastw:
                    preds[i].add(lastw[r_])
            for w_ in Wr:
                if w_ in lastw:
                    preds[i].add(lastw[w_])
                for j in readers.get(w_, ()):
                    preds[i].add(j)
            for r_ in R:
                readers.setdefault(r_, []).append(i)
            for w_ in Wr:
                lastw[w_] = i
                readers[w_] = []
            preds[i].discard(i)
        if not self.schedule:
            order = list(range(n))
        else:
            engs = ['pe', 'act', 'dve', 'pool', 'sp']
            per = {e: [] for e in engs}
            for i, nd in enumerate(nodes):
                per[nd['eng']].append(i)
            ptr = {e: 0 for e in engs}
            done = [False] * n
            fin = [0.0] * n
            free = {e: self.tnow for e in engs}
            order = []
            WINDOW = int(os.environ.get("MK_WIN", "192"))
            remaining = n
            while remaining:
                best = None
                for e in engs:
                    lst = per[e]
                    p0 = ptr[e]
                    while p0 < len(lst) and done[lst[p0]]:
                        p0 += 1
                    ptr[e] = p0
                    cnt = 0
                    k = p0
                    while k < len(lst) and cnt < WINDOW:
                        i = lst[k]
                        k += 1
                        if done[i]:
                            continue
                        cnt += 1
                        ok = True
                        t = free[e]
                        for pj in preds[i]:
                            if not done[pj]:
                                ok = False
                                break
                            if fin[pj] > t:
                                t = fin[pj]
                        if not ok:
                            continue
                        key = (t + 0.002 * (cnt - 1), i)
                        if best is None or key < best[0]:
                            best = (key, e, i, t)
                        if t <= free[e] + 1e-9:
                            break
                assert best is not None, "scheduler deadlock"
                _, e, i, t = best
                nd = nodes[i]
                done[i] = True
                remaining -= 1
                if nd.get('isdma'):
                    free[e] = t + 0.15
                    fin[i] = t + nd['est']
                else:
                    free[e] = t + nd['est']
                    fin[i] = t + nd['est'] + 0.1
                order.append((t, i))
            order.sort()
            order = [i for _, i in order]
            self.tnow = max(max(free.values()), max(fin) if fin else 0.0)
        for i in order:
            for it in nodes[i]['items']:
                if it['kind'] == 'op':
                    self._emit_op(it['eng'], it['fn'], it['reads'], it['writes'], it['sig'])
                else:
                    self._emit_dma(it['eng'], it['out'], it['in_'], it['reads'], it['writes'], it['kw'])

    def barrier(self):
        self.flush()
        for e in self.cnt:
            assert not self.dirty[e], e
        for eng in ['pe', 'act', 'dve', 'pool', 'sp']:
            for e in self.cnt:
                if self.cnt[e] > 0:
                    self._wait(eng, (e, self.cnt[e], e), same_ok=False)
            for i in range(len(self.dsem)):
                if self.dcnt[i] > 0:
                    self._wait(eng, (i, self.dcnt[i], 'dma'), same_ok=False)
        self.lastw = {}
        self.reads = {}

    def close(self):
        self.flush()
        for g in reversed(self._ctx):
            g.__exit__(None, None, None)


def _t5_bucket_np(n):
    n = np.maximum(n, 0)
    max_exact = 16
    nf = np.maximum(n, 1).astype(np.float32)
    large = max_exact + (np.log(nf / np.float32(max_exact)) / np.float32(math.log(128 / 16))
                         * np.float32(32 - max_exact)).astype(np.int32)
    large = np.minimum(large, 31)
    return np.where(n < max_exact, n, large)


def build_program():
    nc = bass.Bass("TRN2", target_bir_lowering=False)

    def din(name, shape, dt=F32):
        return nc.dram_tensor(name, list(shape), dt, kind="ExternalInput").ap()

    xctx = din("xctx", [CT, D])
    cvec = din("cvec", [128, 8])
    w_ada = din("w_ada", [D, 6 * D])
    b_ada = din("b_ada", [128, 48])
    b_ada_row = din("b_ada_row", [1, 6 * D])
    w_in = din("w_in", [D, 2560])
    convw = din("convw", [128, 16])
    convb = din("convb", [128, 4])
    WA = din("WA", [4, 128, 128])
    WX = din("WX", [4, 128, 128])
    bga = din("bga", [128, 4])
    bgx = din("bgx", [128, 4])
    lam = din("lam", [128, 4])
    relb = din("relb", [32, 8])
    glru = din("glru", [128, 4])
    gattn = din("gattn", [128, 4])
    w_out = din("w_out", [D, D])
    lnv = din("lnv", [4, D])
    w_r = din("w_r", [D, 36])
    b_r = din("b_r", [1, 36])
    if STOP >= 5:
        w1 = din("w1", [NE, D, DE])
        w3 = din("w3", [NE, D, DE])
        w2 = din("w2", [NE, DE, D])
    gmask_d = din("gmask", [128, 256])
    ownhot_d = din("ownhot", [128, 256])
    flag_d = din("flag", [128, 1])
    Roh = din("Roh", [32, 384])
    NEGr = din("NEGr", [8, 384])
    IND = din("IND", [16, CT])
    out_d = nc.dram_tensor("out", [NOWN, D], F32, kind="ExternalOutput").ap()

    xlru_s = nc.dram_tensor("xlru_s", [4, 128, CT], F32, kind="Internal").ap()
    gy_s = nc.dram_tensor("gy_s", [4, 128, NOWN], BF16, kind="Internal").ap()
    x1_s = nc.dram_tensor("x1_s", [NOWN, D], F32, kind="Internal").ap()
    q_s = nc.dram_tensor("q_s", [4, 128, NOWN], BF16, kind="Internal").ap()
    lo_s = nc.dram_tensor("lo_s", [4, 128, NOWN], BF16, kind="Internal").ap()
    a_s = nc.dram_tensor("a_s", [4, 128, NOWN], BF16, kind="Internal").ap()
    G_s = nc.dram_tensor("G_s", [8, 384], F32, kind="Internal")
    dbg = {}

    def dout(name, shape, dt=F32):
        dbg[name] = nc.dram_tensor(name, list(shape), dt, kind="ExternalOutput").ap()
        return dbg[name]

    P = Prog(nc)
    try:
      with contextlib.ExitStack() as top:
        _nm = [0]

        def sb(st, name, shape, dt):
            _nm[0] += 1
            return st.enter_context(nc.sbuf_tensor("%s_u%d" % (name, _nm[0]), list(shape), dt))

        def ps(st, name, shape, dt):
            return st.enter_context(nc.psum_tensor(name, list(shape), dt))

        E = P.E

        def _est(eng, a, kw):
            o = kw.get('out', a[0] if a else None)
            try:
                nfree = 1
                for d_ in o.shape[1:]:
                    nfree *= d_
            except Exception:
                nfree = 512
            if eng == 'pe':
                mv_ = kw.get('rhs', a[2] if len(a) > 2 else None)
                try:
                    nm = 1
                    for d_ in mv_.shape[1:]:
                        nm *= d_
                except Exception:
                    nm = 128
                return 0.06 + max(nm, 64) / 2000.0
            if eng == 'act':
                return 0.22 + nfree / 1100.0
            if eng == 'dve':
                return 0.10 + nfree / 900.0
            return 0.15 + nfree / 440.0

        def op(eng, meth, reads, writes, *args, sig=True, **kw):
            return P.op(eng, lambda: getattr(E[eng], meth)(*args, **kw), reads, writes, sig, est=_est(eng, args, kw))

        def ARGS(*a, **kw):
            return (a, kw)

        def pe_raw(meth, reads, writes, argskw=None, sig=True):
            a, kw = argskw
            return P.op('pe', lambda: getattr(nc.tensor, meth)(*a, **kw), reads, writes, sig, est=_est('pe', a, kw))

        def mm(out, lhsT, rhs, reads, writes, start, stop):
            return P.op('pe', lambda: nc.tensor.matmul(out, lhsT, rhs, start=start, stop=stop), reads, writes, sig=stop,
                        est=_est('pe', (out, lhsT, rhs), {}))

        pT0 = ps(top, "pT0", [128, 1024], BF16)
        pT1 = ps(top, "pT1", [128, 1024], BF16)
        pA = ps(top, "pA", [128, 512], F32)
        pB = ps(top, "pB", [128, 512], F32)
        pS0 = ps(top, "pS0", [128, 512], F32)
        pS1 = ps(top, "pS1", [128, 512], F32)
        pO = ps(top, "pO", [128, 512], F32)
        pM = ps(top, "pM", [128, 512], F32)

        ident = sb(top, "ident", [128, 128], BF16)
        id32 = sb(top, "id32", [128, 128], F32)
        ones32 = sb(top, "ones32", [128, 128], F32)
        onesb = sb(top, "onesb", [128, 1], BF16)
        modT = sb(top, "modT", [128, 48], F32)
        sc1p = sb(top, "sc1p", [128, 8], F32)
        sc2p = sb(top, "sc2p", [128, 8], F32)
        g1B = sb(top, "g1B", [128, D], F32)
        g2B = sb(top, "g2B", [128, D], F32)
        mhalf = sb(top, "mhalf", [128, 16], F32)
        ve = sb(top, "ve", [128, 1], F32)

        op('pool', 'memset', [], ['id32'], id32[:], 1.0)
        op('pool', 'affine_select', ['id32'], ['id32'], out=id32[:], in_=id32[:], pattern=[[-1, 128]],
           compare_op=ALU.is_equal, fill=0.0, base=0, channel_multiplier=1)
        op('dve', 'tensor_copy', ['id32'], ['ident'], ident[:], id32[:])
        op('pool', 'memset', [], ['ones32'], ones32[:], 1.0)
        op('dve', 'memset', [], ['onesb'], onesb[:], 1.0)
        op('dve', 'memset', [], ['mhalf'], mhalf[:], -0.5)

        P.flush()
        P.schedule = ('0' in SCHED_PH) and (os.environ.get('MK_SCHED', '1') == '1')
        with contextlib.ExitStack() as s0:
            csb = sb(s0, "csb", [128, 8], F32)
            modrow = sb(s0, "modrow", [1, 6 * D], F32)
            csil = sb(s0, "csil", [128, 8], BF16)
            bada = sb(s0, "bada", [128, 48], F32)
            wad = [sb(s0, "wad%d" % i, [128, 8, 512], BF16) for i in range(2)]
            P.dma('sp', csb[:], cvec, writes=['csb'])
            P.dma('sp', bada[:], b_ada, writes=['bada'])
            op('act', 'activation', ['csb'], ['csil'], out=csil[:], in_=csb[:], func=AF.Silu)
            for cc in range(12):
                w = wad[cc % 2]
                wk = 'wad%d' % (cc % 2)
                P.dma('pool', w[:], w_ada[:, cc * 512:(cc + 1) * 512].rearrange("(k p) n -> p k n", p=128),
                      writes=[wk])
                pp = pA if cc % 2 == 0 else pB
                pk = 'pA' if cc % 2 == 0 else 'pB'
                for k in range(8):
                    mm(pp[0:1, :], csil[:, k:k + 1], w[:, k, :], ['csil', wk], [pk], k == 0, k == 7)
                op('act', 'copy', [pk], ['modrow'], modrow[0:1, cc * 512:(cc + 1) * 512], pp[0:1, :])
            for ct in range(48):
                pe_raw('matmul', ['modrow', 'ones32'], ['pM'], sig=(ct == 47), argskw=ARGS(pM[:, ct:ct + 1], modrow[0:1, ct * 128:(ct + 1) * 128],
                                                    ones32[0:1, 0:1], start=True, stop=True))
            op('dve', 'tensor_tensor', ['pM', 'bada'], ['modT'], modT[:], pM[:, 0:48], bada[:], ALU.add)
            op('dve', 'tensor_scalar_add', ['modT'], ['sc1p'], sc1p[:], modT[:, 8:16], 1.0)
            op('dve', 'tensor_scalar_add', ['modT'], ['sc2p'], sc2p[:], modT[:, 32:40], 1.0)
            for (dst, dk, off) in ((g1B, 'g1B', 2 * D), (g2B, 'g2B', 5 * D)):
                for hf in range(2):
                    pe_raw('matmul', ['modrow', 'ones32'], ['pA'], argskw=ARGS(pA[:, :], ones32[0:1, :],
                                                        modrow[0:1, off + hf * 512: off + (hf + 1) * 512],
                                                        start=True, stop=True))
                    op('act', 'copy', ['pA'], [dk], dst[:, hf * 512:(hf + 1) * 512], pA[:, :])
            badarow = sb(s0, "badarow", [128, 2, D], F32)
            P.dma('sp', badarow[:, 0, :], b_ada_row[0:1, 2 * D:3 * D].rearrange("a n -> (a n)").partition_broadcast(128),
                  writes=['badarow'])
            P.dma('sp', badarow[:, 1, :], b_ada_row[0:1, 5 * D:6 * D].rearrange("a n -> (a n)").partition_broadcast(128),
                  writes=['badarow'])
            op('pool', 'tensor_tensor', ['g1B', 'badarow'], ['g1B'], g1B[:], g1B[:], badarow[:, 0, :], ALU.add)
            op('pool', 'tensor_tensor', ['g2B', 'badarow'], ['g2B'], g2B[:], g2B[:], badarow[:, 1, :], ALU.add)
            if DBG:
                P.dma('sp', dout("d_modT", [128, 48]), modT[:], reads=['modT'])
                P.dma('sp', dout("d_g1B", [128, D]), g1B[:], reads=['g1B'])
            P.barrier()
            if STOP == 0:
                raise _Stop()

        ssl = sb(top, "ssl", [128, 16], F32)
        ssa = sb(top, "ssa", [128, 16], F32)
        op('dve', 'memset', [], ['ssl'], ssl[:], 0.0)
        op('dve', 'memset', [], ['ssa'], ssa[:], 0.0)
        with contextlib.ExitStack() as s13:
            kT = [sb(s13, "kT%d" % h, [96, CT], BF16) for h in range(NH)]
            Vt = sb(s13, "Vt", [128, 32, NH * 65], BF16)
            op('pool', 'memset', [], ['Vt'], Vt[:].rearrange("p t c -> p (t c)"), 1.0)
            for h in range(NH):
                op('dve', 'memset', [], ['kTind%d' % h], kT[h][64:96, :], 0.0)
                P.dma('pool', kT[h][64:80, :], IND, writes=['kTind%d' % h])

            P.flush()
            P.schedule = ('1' in SCHED_PH) and (os.environ.get('MK_SCHED', '1') == '1')
            with contextlib.ExitStack() as s1:
                winb = sb(s1, "winb", [128, 8, 2560], BF16)
                for k in range(8):
                    P.dma('pool', winb[:, k, :], w_in[k * 128:(k + 1) * 128, :], writes=['winb%d' % k])
                xt = [sb(s1, "xt%d" % i, [128, D], F32) for i in range(2)]
                xn = [sb(s1, "xn%d" % i, [128, D], BF16) for i in range(8)]
                uTs = [sb(s1, "uT%d" % i, [128, 8, 512], BF16) for i in range(2)]
                st6 = [sb(s1, "st6_%d" % i, [128, 2, 6], F32) for i in range(4)]
                mv = [sb(s1, "mv_%d" % i, [128, 2], F32) for i in range(4)]
                rstd = [sb(s1, "rstd_%d" % i, [128, 1], F32) for i in range(4)]
                nb = [sb(s1, "nb_%d" % i, [128, 1], F32) for i in range(4)]
                ve1 = [sb(s1, "ve_%d" % i, [128, 1], F32) for i in range(4)]
                stg = [sb(s1, "stg%d" % i, [128, 512], F32) for i in range(2)]
                ysb = sb(s1, "ysb", [128, 512], F32)
                yt = sb(s1, "yt", [128, 512], F32)
                ysg = sb(s1, "ysg", [128, 512], F32)
                gyb = [sb(s1, "gyb%d" % i, [128, 512], BF16) for i in range(2)]
                winr = ['winb%d' % k for k in range(8)]
                nstg = 0

                def emit_ln(c):
                    for t in range(4):
                        T = 4 * c + t
                        xb = xt[T % 2]
                        xk = 'xt%d' % (T % 2)
                        i = T % 4
                        xi = (c % 2) * 4 + t
                        P.dma('sp', xb[:], xctx[T * 128:(T + 1) * 128, :], writes=[xk])
                        for hf in range(2):
                            op('dve', 'bn_stats', [xk], ['st6_%d' % i], out=st6[i][:, hf, :], in_=xb[:, hf * 512:(hf + 1) * 512])
                        op('dve', 'bn_aggr', ['st6_%d' % i], ['mv_%d' % i], out=mv[i][:], in_=st6[i][:].rearrange("p a b -> p (a b)"))
                        op('dve', 'tensor_scalar_add', ['mv_%d' % i], ['ve_%d' % i], ve1[i][:], mv[i][:, 1:2], EPS)
                        op('pool', 'tensor_tensor', ['ve_%d' % i, 'mhalf'], ['rstd_%d' % i], rstd[i][:], ve1[i][:], mhalf[:, 0:1], ALU.pow)
                        op('dve', 'scalar_tensor_tensor', ['mv_%d' % i, 'rstd_%d' % i], ['nb_%d' % i], out=nb[i][:], in0=mv[i][:, 0:1],
                           scalar=-1.0, in1=rstd[i][:], op0=ALU.mult, op1=ALU.mult)
                        op('act', 'activation', [xk, 'rstd_%d' % i, 'nb_%d' % i], ['xn%d' % xi], out=xn[xi][:], in_=xb[:],
                           func=AF.Identity, bias=nb[i][:], scale=rstd[i][:])

                def emit_tr(c, r):
                    uT = uTs[c % 2]
                    pt = pT0 if r % 2 == 0 else pT1
                    ptk = 'pT0' if r % 2 == 0 else 'pT1'
                    for kk in range(2):
                        k = 2 * r + kk
                        for t in range(4):
                            xi = (c % 2) * 4 + t
                            pe_raw('transpose', ['xn%d' % xi, 'ident'], [ptk], sig=(kk == 1 and t == 3), argskw=ARGS(
                                pt[:, kk * 512 + t * 128: kk * 512 + (t + 1) * 128],
                                xn[xi][:, k * 128:(k + 1) * 128], ident[:]))
                    for kk in range(2):
                        k = 2 * r + kk
                        uk = 'uT%d_%d' % (c % 2, k)
                        if r % 2 == 0:
                            op('act', 'activation', [ptk, 'sc1p', 'modT'], [uk], out=uT[:, k, :],
                               in_=pt[:, kk * 512:(kk + 1) * 512], func=AF.Identity,
                               bias=modT[:, k:k + 1], scale=sc1p[:, k:k + 1])
                        else:
                            op('dve', 'tensor_scalar', [ptk, 'sc1p', 'modT'], [uk], uT[:, k, :],
                               pt[:, kk * 512:(kk + 1) * 512], sc1p[:, k:k + 1], modT[:, k:k + 1],
                               ALU.mult, ALU.add)

                if NCH > 0:
                    emit_ln(0)
                    for r in range(4):
                        emit_tr(0, r)
                for c in range(NCH):
                    own = c >= 4
                    uT = uTs[c % 2]
                    if c + 1 < NCH:
                        emit_ln(c + 1)
                    pend_tr = list(range(4)) if c + 1 < NCH else []
                    uTr = ['uT%d_%d' % (c % 2, k) for k in range(8)]
                    cts = list(range(0, 4)) + (list(range(4, 12)) if own else []) + list(range(12, 16))
                    if 'fm' in SKIP:
                        cts = []
                    every = max(1, len(cts) // 4)
                    for ci, ct in enumerate(cts):
                        if pend_tr and ci > 0 and ci % every == 0:
                            emit_tr(c + 1, pend_tr.pop(0))
                        pp, pk = [(pA, 'pA'), (pB, 'pB'), (pO, 'pO'), (pM, 'pM')][ci % 4]
                        for k in range(8):
                            mm(pp[:, :], winb[:, k, ct * 128:(ct + 1) * 128], uT[:, k, :],
                               [winr[k], uTr[k]], [pk], k == 0, k == 7)
                        j = ct % 4
                        if ct < 4:
                            sg = stg[nstg % 2]
                            sk = 'stg%d' % (nstg % 2)
                            nstg += 1
                            op('act', 'copy', [pk], [sk], sg[:], pp[:, :])
                            P.dma('sp', xlru_s[j, :, c * 512:(c + 1) * 512], sg[:], reads=[sk], writes=['xlru_s'])
                        elif ct < 8:
                            gb = gyb[j % 2]
                            gk = 'gyb%d' % (j % 2)
                            op('act', 'copy', [pk], ['ysb'], ysb[:], pp[:, :])
                            op('pool', 'tensor_tensor', ['ysb'], ['yt'], yt[:], ysb[:], ysb[:], ALU.mult)
                            op('pool', 'tensor_scalar', ['yt'], ['yt'], yt[:], yt[:], 0.044715, 1.0, ALU.mult, ALU.add)
                            op('pool', 'tensor_tensor', ['yt', 'ysb'], ['yt'], yt[:], yt[:], ysb[:], ALU.mult)
                            op('act', 'activation', ['yt'], ['ysg'], out=ysg[:], in_=yt[:], func=AF.Sigmoid,
                               scale=1.5957691216057308)
                            op('pool', 'tensor_tensor', ['ysg', 'ysb'], [gk], gb[:], ysg[:], ysb[:], ALU.mult)
                            P.dma('sp', gy_s[j, :, (c - 4) * 512:(c - 3) * 512], gb[:], reads=[gk], writes=['gy_s'])
                        elif ct < 12:
                            oc = (c - 4) * 512
                            gb = gyb[j % 2]
                            gk = 'gyb%d' % (j % 2)
                            op('act', 'copy', [pk], [gk], gb[:], pp[:, :])
                            P.dma('sp', q_s[j, :, oc:oc + 512], gb[:], reads=[gk], writes=['q_s'])
                        else:
                            ke, km = ('act', 'copy') if j % 2 == 0 else ('dve', 'tensor_copy')
                            op(ke, km, [pk], ['kT%d' % (2 * j)], kT[2 * j][0:64, c * 512:(c + 1) * 512], pp[0:64, :])
                            op(ke, km, [pk], ['kT%d' % (2 * j + 1)],
                               kT[2 * j + 1][0:64, c * 512:(c + 1) * 512], pp[64:128, :])
                    for t in range(0 if 'v' in SKIP else 4):
                        if pend_tr:
                            emit_tr(c + 1, pend_tr.pop(0))
                        T = 4 * c + t
                        pp, pk = (pS0, 'pS0') if t % 2 == 0 else (pS1, 'pS1')
                        for k in range(8):
                            mm(pp[:, :], uT[:, k, t * 128:(t + 1) * 128], winb[:, k, 2048:2560],
                               [winr[k], uTr[k]], [pk], k == 0, k == 7)
                        op('dve' if t % 2 == 0 else 'act', 'tensor_copy' if t % 2 == 0 else 'copy', [pk], ['Vt'],
                           Vt[:, T, :].rearrange("p (h e) -> p h e", e=65)[:, :, 0:64],
                           pp[:, :].rearrange("p (h d) -> p h d", d=64))
                    while pend_tr:
                        emit_tr(c + 1, pend_tr.pop(0))
                if DBG:
                    for h in (0, 1, 7):
                        P.dma('sp', dout("d_kT%d" % h, [80, CT], BF16), kT[h][0:80, :], reads=['kT%d' % h, 'kTind%d' % h])
                    P.dma('sp', dout("d_V", [128, 32 * 520], BF16), Vt[:].rearrange("p t c -> p (t c)"), reads=['Vt'])
                P.barrier()
                if STOP == 1:
                    raise _Stop()

            P.flush()
            P.schedule = ('3' in SCHED_PH) and (os.environ.get('MK_SCHED', '1') == '1')
            with contextlib.ExitStack() as s3:
                qT = [sb(s3, "qT%d" % h, [96, NOWN], BF16) for h in range(NH)]
                for h in range(NH):
                    P.dma('sp', qT[h][0:64, :], q_s[h // 2, (h % 2) * 64:(h % 2) * 64 + 64, :], reads=['q_s'], writes=['qT%d' % h])
                    op('dve', 'memset', [], ['qTm%d' % h], qT[h][64:96, :], 0.0)
                apair = sb(s3, "apair", [128, 512], BF16)
                gmask = sb(s3, "gmask_t", [128, 16, 16], F32)
                ownhot = sb(s3, "ownhot_t", [128, 16, 16], F32)
                P.dma('sp', gmask[:].rearrange("p a b -> p (a b)"), gmask_d, writes=['gmask'])
                P.dma('sp', ownhot[:].rearrange("p a b -> p (a b)"), ownhot_d, writes=['ownhot'])
                cfar = sb(s3, "cfar", [128, 8], F32)
                biasD = sb(s3, "biasD", [128, 8, 128], F32)
                biasS = sb(s3, "biasS", [128, 8, 128], F32)
                kmf = sb(s3, "kmf", [64, NH, 16], F32)
                kmb = sb(s3, "kmb", [64, NH, 16], BF16)
                s3a = contextlib.ExitStack()
                s3a.__enter__()
                relsb = sb(s3a, "relsb", [32, 8], F32)
                Rsb = sb(s3a, "Rsb", [32, 384], F32)
                Gsb = sb(s3a, "Gsb", [8, 384], F32)
                negsb = sb(s3a, "negsb", [8, 384], F32)
                P.dma('sp', relsb[:], relb, writes=['relsb'])
                P.dma('sp', Rsb[:], Roh, writes=['Rsb'])
                P.dma('sp', negsb[:], NEGr, writes=['negsb'])
                P.dma('sp', cfar[:], relb[31:32, :].rearrange("a h -> (a h)").partition_broadcast(128), writes=['cfar'])
                mm(pA[0:8, 0:384], relsb[:, :], Rsb[:, :], ['relsb', 'Rsb'], ['pA'], True, True)
                op('dve', 'tensor_tensor', ['pA', 'negsb'], ['Gsb'], Gsb[:], pA[0:8, 0:384], negsb[:], ALU.add)
                P.dma('sp', G_s.ap(), Gsb[:], reads=['Gsb'], writes=['G_s'])
                hank = sb(s3a, "hank", [128, 16, 128], F32)
                for h in range(NH):
                    P.dma('sp', hank[:, h, :], bass.AP(G_s, h * 384 + 128, [[1, 128], [1, 128]]),
                          reads=['G_s'], writes=['hank'])
                    P.dma('sp', hank[:, 8 + h, :], bass.AP(G_s, h * 384, [[1, 128], [1, 128]]),
                          reads=['G_s'], writes=['hank'])
                for h in range(NH):
                    op('pool', 'tensor_copy', ['hank'], ['biasD'], biasD[:, h, :], hank[:, h, ::-1])
                    op('pool', 'tensor_copy', ['hank'], ['biasS'], biasS[:, h, :], hank[:, 8 + h, ::-1])
                for h in range(NH):
                    op('dve', 'tensor_reduce', ['kT%d' % h], ['kmf'], out=kmf[:, h, :],
                       in_=kT[h][0:64, :].rearrange("p (n b) -> p n b", b=BLK), axis=AX.X, op=ALU.add)
                op('dve', 'tensor_scalar_mul', ['kmf'], ['kmb'], kmb[:].rearrange("p h n -> p (h n)"),
                   kmf[:].rearrange("p h n -> p (h n)"), 1.0 / BLK)
                _sch3 = P.schedule
                s3a.__exit__(None, None, None)
                P.barrier()
                P.schedule = _sch3
                pT0f = pT0[:].bitcast(F32)
                pT1f = pT1[:].bitcast(F32)
                cw = sb(s3, "cw", [128, 16], F32)
                cb = sb(s3, "cb", [128, 4], F32)
                bA = sb(s3, "bA", [128, 4], F32)
                bX = sb(s3, "bX", [128, 4], F32)
                lamt = sb(s3, "lamt", [128, 4], F32)
                cL = sb(s3, "cL", [128, 4], F32)
                cL2 = sb(s3, "cL2", [128, 4], F32)
                flag = sb(s3, "flag_t", [128, 1], F32)
                carry = sb(s3, "carry", [128, 4], F32)
                WAb = sb(s3, "WAb", [128, 4, 128], BF16)
                WXb = sb(s3, "WXb", [128, 4, 128], BF16)
                P.dma('sp', cw[:], convw, writes=['cw'])
                P.dma('sp', cb[:], convb, writes=['cb'])
                P.dma('sp', bA[:], bga, writes=['bA'])
                P.dma('sp', bX[:], bgx, writes=['bX'])
                P.dma('sp', lamt[:], lam, writes=['lamt'])
                P.dma('sp', flag[:], flag_d, writes=['flag'])
                P.dma('pool', WAb[:], WA.rearrange("j p o -> p j o"), writes=['WAb'])
                P.dma('pool', WXb[:], WX.rearrange("j p o -> p j o"), writes=['WXb'])
                op('act', 'activation', ['lamt'], ['cL'], out=cL[:], in_=lamt[:], func=AF.Exp, scale=-1.0)
                op('act', 'activation', ['cL'], ['cL'], out=cL[:], in_=cL[:], func=AF.Ln, bias=1.0)
                op('dve', 'tensor_scalar_mul', ['cL'], ['cL2'], cL2[:], cL[:], -16.0)
                op('dve', 'tensor_scalar_mul', ['cL'], ['cL'], cL[:], cL[:], -8.0)
                op('dve', 'memset', [], ['carry'], carry[:], 0.0)
                nbA = sb(s3, "nbA", [128, 4], F32)
                nbX = sb(s3, "nbX", [128, 4], F32)
                op('dve', 'tensor_scalar_mul', ['bA'], ['nbA'], nbA[:], bA[:], -1.0)
                op('dve', 'tensor_scalar_mul', ['bX'], ['nbX'], nbX[:], bX[:], -1.0)
                W = 512
                NB2 = 2
                def mk(name, shape, dt):
                    return [sb(s3, "%s_%d" % (name, i), shape, dt) for i in range(NB2)]
                xl = mk("xl", [128, 3 + W], F32)
                xc = mk("xc", [128, W], F32)
                xcb = mk("xcb", [128, W], BF16)
                rr = mk("rr", [128, W], F32)
                ii = mk("ii", [128, W], F32)
                aa = mk("aa", [128, W], F32)
                m2 = mk("m2", [128, W], F32)
                hh = mk("hh", [128, W], F32)
                gyl = mk("gyl", [128, W], BF16)
                sq = mk("sq", [128, W], BF16)
                lop = mk("lop", [128, W], BF16)
                npc_box = [0]

                def lru_piece(j, pc):
                    b = npc_box[0] % NB2
                    npc_box[0] += 1
                    K_ = lambda n: '%s_%d' % (n, b)
                    if pc == 0:
                        op('dve', 'memset', [], [K_('xl')], xl[b][:, 0:3], 0.0)
                        P.dma('sp', xl[b][:, 3:3 + W], xlru_s[j, :, 0:W], reads=['xlru_s'], writes=[K_('xl')])
                    else:
                        P.dma('sp', xl[b][:, :], xlru_s[j, :, pc * W - 3:(pc + 1) * W], reads=['xlru_s'], writes=[K_('xl')])
                    if pc >= 4:
                        oc = (pc - 4) * W
                        P.dma('sp', gyl[b][:], gy_s[j, :, oc:oc + W], reads=['gy_s'], writes=[K_('gyl')])
                    op('dve', 'tensor_scalar', [K_('xl'), 'cw', 'cb'], [K_('xc')], xc[b][:], xl[b][:, 0:W],
                       cw[:, j * 4:j * 4 + 1], cb[:, j:j + 1], ALU.mult, ALU.add)
                    for k in range(1, 4):
                        op('dve', 'scalar_tensor_tensor', [K_('xl'), 'cw', K_('xc')], [K_('xc')], out=xc[b][:], in0=xl[b][:, k:k + W],
                           scalar=cw[:, j * 4 + k:j * 4 + k + 1], in1=xc[b][:], op0=ALU.mult, op1=ALU.add)
                    op('pool', 'tensor_copy', [K_('xc')], [K_('xcb')], xcb[b][:], xc[b][:])
                    mm(pT0f, WAb[:, j, :], xcb[b][:, :], ['WAb', K_('xcb')], ['pT0'], True, True)
                    op('act', 'activation', ['pT0', 'nbA'], [K_('rr')], out=rr[b][:, :], in_=pT0f,
                       func=AF.Exp, bias=nbA[:, j:j + 1], scale=-1.0)
                    mm(pT0f, WXb[:, j, :], xcb[b][:, :], ['WXb', K_('xcb')], ['pT0'], True, True)
                    op('act', 'activation', ['pT0', 'nbX'], [K_('ii')], out=ii[b][:, :], in_=pT0f,
                       func=AF.Exp, bias=nbX[:, j:j + 1], scale=-1.0)
                    op('act', 'activation', [K_('rr')], [K_('rr')], out=rr[b][:], in_=rr[b][:], func=AF.Ln, bias=1.0)
                    op('act', 'activation', [K_('rr')], [K_('rr')], out=rr[b][:], in_=rr[b][:], func=AF.Exp, scale=-1.0)
                    op('act', 'activation', [K_('ii')], [K_('ii')], out=ii[b][:], in_=ii[b][:], func=AF.Ln, bias=1.0)
                    op('act', 'activation', [K_('ii')], [K_('ii')], out=ii[b][:], in_=ii[b][:], func=AF.Exp, scale=-1.0)
                    op('act', 'activation', [K_('rr'), 'cL'], [K_('aa')], out=aa[b][:], in_=rr[b][:], func=AF.Exp, scale=cL[:, j:j + 1])
                    op('act', 'activation', [K_('rr'), 'cL2'], [K_('m2')], out=m2[b][:], in_=rr[b][:], func=AF.Exp, scale=cL2[:, j:j + 1])
                    op('act', 'activation', [K_('m2')], [K_('m2')], out=m2[b][:], in_=m2[b][:], func=AF.Ln, scale=-1.0, bias=1.0)
                    op('act', 'activation', [K_('m2')], [K_('m2')], out=m2[b][:], in_=m2[b][:], func=AF.Exp, scale=0.5)
                    op('pool', 'tensor_tensor', [K_('ii'), K_('xc')], [K_('ii')], ii[b][:], ii[b][:], xc[b][:], ALU.mult)
                    op('pool', 'tensor_tensor', [K_('ii'), K_('m2')], [K_('ii')], ii[b][:], ii[b][:], m2[b][:], ALU.mult)
                    if pc == 4:
                        op('dve', 'tensor_tensor', ['carry', 'flag'], ['carry'], carry[:, j:j + 1], carry[:, j:j + 1],
                           flag[:], ALU.mult)
                    op('dve', 'tensor_tensor_scan', [K_('aa'), K_('ii'), 'carry'], [K_('hh')], out=hh[b][:], data0=aa[b][:], data1=ii[b][:],
                       initial=carry[:, j:j + 1], op0=ALU.mult, op1=ALU.add)
                    op('dve', 'tensor_copy', [K_('hh')], ['carry'], carry[:, j:j + 1], hh[b][:, W - 1:W])
                    if pc >= 4:
                        oc = (pc - 4) * W
                        op('pool', 'tensor_tensor', [K_('hh'), K_('gyl')], [K_('lop')], lop[b][:], hh[b][:], gyl[b][:], ALU.mult)
                        P.dma('sp', lo_s[j, :, oc:oc + W], lop[b][:], reads=[K_('lop')], writes=['lo_s'])
                        op('pool', 'tensor_tensor', [K_('lop')], [K_('sq')], sq[b][:], lop[b][:], lop[b][:], ALU.mult)
                        for t in range(4):
                            pe_raw('matmul', [K_('sq'), 'onesb'], ['pT1'], sig=(t == 3), argskw=ARGS(pT1f[:, t:t + 1], sq[b][:, t * 128:(t + 1) * 128], onesb[:, 0:1],
                                                                start=True, stop=True))
                        t0 = (pc - 4) * 4
                        op('dve', 'tensor_tensor', ['pT1', 'ssl'], ['ssl'], ssl[:, t0:t0 + 4], ssl[:, t0:t0 + 4], pT1f[:, 0:4], ALU.add)
                lru_list = [(j, pc) for j in range(4) for pc in range(8)]
                gsb = sb(s3, "gsb", [128, NH, 16], F32)
                top8 = sb(s3, "top8", [128, NH, 8], F32)
                sel = sb(s3, "sel", [128, NH, 16], F32)
                mvb = sb(s3, "mvb", [128, NH, 16], BF16)
                for qt in range(16):
                    for h in range(NH):
                        pe_raw('matmul', ['qT%d' % h, 'kmb'], ['pM'], sig=(h == NH - 1), argskw=ARGS(pM[:, h * 16:(h + 1) * 16], qT[h][0:64, qt * 128:(qt + 1) * 128],
                                                            kmb[:, h, :], start=True, stop=True))
                    op('dve', 'tensor_tensor', ['pM', 'gmask'], ['gsb'], gsb[:],
                       pM[:, 0:128].rearrange("p (h n) -> p h n", n=16),
                       gmask[:, qt:qt + 1, :].to_broadcast([128, NH, 16]), ALU.add)
                    for h in range(NH):
                        op('dve', 'max', ['gsb'], ['top8'], out=top8[:, h, :], in_=gsb[:, h, :])
                    op('dve', 'tensor_tensor', ['gsb', 'top8'], ['sel'], sel[:], gsb[:],
                       top8[:, :, 2:3].to_broadcast([128, NH, 16]), ALU.is_ge)
                    op('dve', 'scalar_tensor_tensor', ['gsb', 'sel'], ['sel'], out=sel[:], in0=gsb[:], scalar=-1e29,
                       in1=sel[:], op0=ALU.is_gt, op1=ALU.mult)
                    op('dve', 'tensor_tensor', ['sel', 'ownhot'], ['sel'], sel[:], sel[:],
                       ownhot[:, qt:qt + 1, :].to_broadcast([128, NH, 16]), ALU.add)
                    op('dve', 'tensor_scalar', ['sel'], ['mvb'], mvb[:], sel[:], -1.0, -NEGM, ALU.add, ALU.mult)
                    for h in range(NH):
                        pe_raw('transpose', ['mvb', 'ident'], ['pT1'], sig=(h == NH - 1), argskw=ARGS(pT1[0:16, h * 128:(h + 1) * 128], mvb[:, h, :], ident[:]))
                    for h in range(NH):
                        op('act' if qt % 2 == 0 else 'dve', 'copy' if qt % 2 == 0 else 'tensor_copy', ['pT1'],
                           ['qTm%d' % h], qT[h][64:80, qt * 128:(qt + 1) * 128], pT1[0:16, h * 128:(h + 1) * 128])
                if DBG:
                    for h in (0, 1, 7):
                        P.dma('sp', dout("d_qm%d" % h, [16, NOWN], BF16), qT[h][64:80, :], reads=['qTm%d' % h])
                    P.dma('sp', dout("d_biasD", [128, 8 * 128]), biasD[:].rearrange("p h n -> p (h n)"), reads=['biasD'])
                    P.dma('sp', dout("d_biasS", [128, 8 * 128]), biasS[:].rearrange("p h n -> p (h n)"), reads=['biasS'])
                PT = [sb(s3, "PT%d" % i, [128, 512], BF16) for i in range(3)]
                tmpS = [sb(s3, "tmpS%d" % i, [128, 128], F32) for i in range(2)]
                osb = sb(s3, "osb", [65, 512], F32)
                sqa = sb(s3, "sqa", [128, 512], BF16)
                SCALE = HD ** -0.5
                npt = 0
                nts = 0
                nonlocal_nsb = [0]
                Sb = [(pS0, 'pS0'), (pS1, 'pS1'), (pA, 'pA')]
                Ob = [(pO, 'pO'), (pB, 'pB')]
                nob = 0
                for cq in range(4):
                    c = 4 + cq
                    for h in range(NH):
                        if lru_list:
                            lru_piece(*lru_list.pop(0))
                        j, s = h // 2, h % 2
                        qr = ['qT%d' % h, 'qTm%d' % h]
                        kr = ['kT%d' % h, 'kTind%d' % h]
                        nkt = 4 * c + 4
                        pOc, pOk = Ob[nob % 2]
                        nob += 1

                        def geom(kt):
                            qlo = max(kt, 4 * c)
                            n0 = (qlo - 4 * c) * 128
                            return qlo, n0

                        def issue_S(kt):
                            nonlocal_nsb[0] += 1
                            pS, pSk = Sb[nonlocal_nsb[0] % 3]
                            qlo, n0 = geom(kt)
                            mm(pS[:, n0:512], kT[h][0:96, kt * 128:(kt + 1) * 128],
                               qT[h][0:96, cq * 512 + n0: cq * 512 + 512], qr + kr, [pSk], True, True)
                            return pS, pSk

                        pendq = [issue_S(0)]
                        if nkt > 1:
                            pendq.append(issue_S(1))
                        for kt in range(nkt):
                            pS, pSk = pendq.pop(0)
                            qlo, n0 = geom(kt)
                            pt = PT[npt % 3]
                            ptk = 'PT%d' % (npt % 3)
                            npt += 1
                            col = n0
                            nearks = []
                            for qtile in range(qlo, 4 * c + 4):
                                d = qtile - kt
                                if d > 1:
                                    break
                                bt = biasD if d == 0 else biasS
                                ts_, tsk = tmpS[nts % 2], 'tmpS%d' % (nts % 2)
                                nts += 1
                                op('dve', 'scalar_tensor_tensor', [pSk, 'biasD', 'biasS'], [tsk], out=ts_[:],
                                   in0=pS[:, col:col + 128], scalar=SCALE, in1=bt[:, h, :], op0=ALU.mult, op1=ALU.add)
                                op('act', 'activation', [tsk], [ptk], out=pt[:, col:col + 128], in_=ts_[:], func=AF.Exp)
                                nearks.append(tsk)
                                col += 128
                            if col < 512:
                                op('act', 'activation', [pSk, 'cfar'] + nearks, [ptk], out=pt[:, col:512], in_=pS[:, col:512],
                                   func=AF.Exp, bias=cfar[:, h:h + 1], scale=SCALE)
                            mm(pOc[0:65, n0:512], Vt[:, kt, h * 65:(h + 1) * 65], pt[:, n0:512], ['Vt', ptk], [pOk],
                               kt == 0, kt == nkt - 1)
                            if kt + 2 < nkt:
                                pendq.append(issue_S(kt + 2))
                        op('act', 'copy', [pOk], ['osb', 'osbr'], osb[:, :], pOc[0:65, :])
                        op('dve', 'reciprocal', ['osb'], ['osbr'], osb[64:65, :], osb[64:65, :])
                        pe_raw('matmul', ['osbr', 'ones32'], ['pM'], argskw=ARGS(pM[0:64, :], ones32[64:65, 0:64], osb[64:65, :],
                                                            start=True, stop=True))
                        op('dve', 'tensor_tensor', ['osb', 'pM'], ['apair'], apair[s * 64:(s + 1) * 64, :],
                           osb[0:64, :], pM[0:64, :], ALU.mult)
                        if s == 1:
                            P.dma('sp', a_s[j, :, cq * 512:(cq + 1) * 512], apair[:], reads=['apair'], writes=['a_s'])
                            op('pool', 'tensor_tensor', ['apair'], ['sqa'], sqa[:], apair[:], apair[:], ALU.mult)
                            for t in range(4):
                                pe_raw('matmul', ['sqa', 'onesb'], ['pM'], sig=(t == 3), argskw=ARGS(pM[:, t:t + 1], sqa[:, t * 128:(t + 1) * 128], onesb[:, 0:1],
                                                                    start=True, stop=True))
                            op('dve', 'tensor_tensor', ['pM', 'ssa'], ['ssa'], ssa[:, cq * 4:cq * 4 + 4],
                               ssa[:, cq * 4:cq * 4 + 4], pM[:, 0:4], ALU.add)
                while lru_list:
                    lru_piece(*lru_list.pop(0))
                if DBG:
                    P.dma('sp', dout("d_ssa", [128, 16]), ssa[:], reads=['ssa'])
                    P.dma('sp', dout("d_ssl", [128, 16]), ssl[:], reads=['ssl'])
                P.barrier()
                if STOP == 3:
                    raise _Stop()

        P.flush()
        P.schedule = ('4' in SCHED_PH) and (os.environ.get('MK_SCHED', '1') == '1')
        lnB = sb(top, "lnB", [128, 4, D], F32)
        u2T = sb(top, "u2T", [128, 8, NOWN], BF16)
        P.dma('sp', lnB[:].rearrange("p a d -> p (a d)"),
              lnv.rearrange("a d -> (a d)").partition_broadcast(128), writes=['lnB'])
        u2r = ['u2T%d' % k for k in range(8)]
        wrb = sb(top, "wrb", [128, 8, 36], BF16)
        brB = sb(top, "brB", [128, 36], F32)
        P.dma('pool', wrb[:], w_r.rearrange("(k p) n -> p k n", p=128), writes=['wrb'])
        P.dma('sp', brB[:], b_r.rearrange("a n -> (a n)").partition_broadcast(128), writes=['brB'])
        gate = sb(top, "gate", [128, 16, NE], F32)
        lg = sb(top, "lg", [128, 36], F32)
        gmax = sb(top, "gmax", [128, 1], F32)
        ngmax = sb(top, "ngmax", [128, 1], F32)
        gex = sb(top, "gex", [128, 4], F32)
        gsum = sb(top, "gsum", [128, 1], F32)
        gtop = sb(top, "gtop", [128, 1], F32)
        goh = sb(top, "goh", [128, 4], F32)
        esel = sb(top, "esel", [128, 4, 8], F32)
        ein = sb(top, "ein", [128, 8], F32)
        et8 = sb(top, "et8", [128, 8], F32)
        nl1 = sb(top, "nl1", [128, 1], F32)
        eex = sb(top, "eex", [128, 8], F32)
        esl = sb(top, "esl", [128, 8], F32)
        eden = sb(top, "eden", [128, 1], F32)
        with contextlib.ExitStack() as s4:
            def router(tt):
                for k in range(8):
                    mm(pM[:, 0:36], u2T[:, k, tt * 128:(tt + 1) * 128], wrb[:, k, :], [u2r[k], 'wrb'], ['pM'], k == 0, k == 7)
                op('dve', 'tensor_tensor', ['pM', 'brB'], ['lg'], lg[:], pM[:, 0:36], brB[:], ALU.add)
                op('dve', 'tensor_reduce', ['lg'], ['gmax'], out=gmax[:], in_=lg[:, 0:4], axis=AX.X, op=ALU.max)
                op('dve', 'tensor_scalar_mul', ['gmax'], ['ngmax'], ngmax[:], gmax[:], -1.0)
                op('act', 'activation', ['lg', 'ngmax'], ['gex'], out=gex[:], in_=lg[:, 0:4], func=AF.Exp, bias=ngmax[:])
                op('dve', 'tensor_reduce', ['gex'], ['gsum'], out=gsum[:], in_=gex[:], axis=AX.X, op=ALU.add)
                op('dve', 'reciprocal', ['gsum'], ['gtop'], gtop[:], gsum[:])
                op('dve', 'tensor_tensor', ['lg', 'gmax'], ['goh'], goh[:], lg[:, 0:4], gmax[:].to_broadcast([128, 4]), ALU.is_ge)
                op('dve', 'tensor_tensor', ['lg', 'goh'], ['esel'], esel[:], lg[:, 4:36].rearrange("p (g e) -> p g e", e=8),
                   goh[:].unsqueeze(2).to_broadcast([128, 4, 8]), ALU.mult)
                op('dve', 'tensor_reduce', ['esel'], ['ein'], out=ein[:], in_=esel[:].rearrange("p g e -> p e g"),
                   axis=AX.X, op=ALU.add)
                op('dve', 'max', ['ein'], ['et8'], out=et8[:], in_=ein[:])
                op('dve', 'tensor_scalar_mul', ['et8'], ['nl1'], nl1[:], et8[:, 0:1], -1.0)
                op('act', 'activation', ['ein', 'nl1'], ['eex'], out=eex[:], in_=ein[:], func=AF.Exp, bias=nl1[:])
                op('dve', 'tensor_tensor', ['ein', 'et8'], ['esl'], esl[:], ein[:], et8[:, 1:2].to_broadcast([128, 8]), ALU.is_ge)
                op('dve', 'tensor_tensor', ['esl', 'eex'], ['esl'], esl[:], esl[:], eex[:], ALU.mult)
                op('dve', 'tensor_reduce', ['esl'], ['eden'], out=eden[:], in_=esl[:], axis=AX.X, op=ALU.add)
                op('dve', 'reciprocal', ['eden'], ['eden'], eden[:], eden[:])
                op('dve', 'tensor_tensor', ['eden', 'gtop'], ['eden'], eden[:], eden[:], gtop[:], ALU.mult)
                op('dve', 'tensor_scalar', ['esl', 'eden'], ['esl'], esl[:], esl[:], eden[:, 0:1], None, ALU.mult)
                op('dve', 'tensor_tensor', ['goh', 'esl'], ['gate'], gate[:, tt, :].rearrange("p (g e) -> p g e", e=8),
                   goh[:].unsqueeze(2).to_broadcast([128, 4, 8]), esl[:].unsqueeze(1).to_broadcast([128, 4, 8]), ALU.mult)
            loT = sb(s4, "loT", [128, 4, NOWN], BF16)
            aTp = sb(s4, "aTp", [128, 4, NOWN], BF16)
            for jj in range(4):
                P.dma('sp', loT[:, jj, :], lo_s[jj], reads=['lo_s'], writes=['loT%d' % jj])
                P.dma('sp', aTp[:, jj, :], a_s[jj], reads=['a_s'], writes=['aTp%d' % jj])
            if DBG:
                P.dma('sp', dout("d_loT", [128, 4 * NOWN], BF16), loT[:].rearrange("p j n -> p (j n)"),
                      reads=['loT%d' % j for j in range(4)])
                P.dma('sp', dout("d_aTp", [128, 4 * NOWN], BF16), aTp[:].rearrange("p j n -> p (j n)"),
                      reads=['aTp%d' % j for j in range(4)])
            woutb = sb(s4, "woutb", [128, 8, D], BF16)
            wo32 = [sb(s4, "wo32_%d" % i, [128, D], F32) for i in range(2)]
            gl = sb(s4, "gl", [128, 8], F32)
            P.dma('sp', gl[:, 0:4], glru, writes=['gl'])
            P.dma('sp', gl[:, 4:8], gattn, writes=['gl'])
            for k in range(8):
                wb, wk = wo32[k % 2], 'wo32_%d' % (k % 2)
                P.dma('sp', wb[:], w_out[k * 128:(k + 1) * 128, :], writes=[wk])
                op('act', 'activation', [wk, 'gl'], ['woutb%d' % k], out=woutb[:, k, :], in_=wb[:], func=AF.Identity, scale=gl[:, k:k + 1])
            NB4 = 3
            xo = [sb(s4, "xo%d" % i, [128, D], F32) for i in range(NB4)]
            mix = [sb(s4, "mix%d" % i, [128, D], F32) for i in range(NB4)]
            zz = [sb(s4, "zz%d" % i, [128, D], F32) for i in range(NB4)]
            x1 = [sb(s4, "x1_%d" % i, [128, D], F32) for i in range(NB4)]
            xn2 = [sb(s4, "xn2_%d" % i, [128, D], BF16) for i in range(NB4)]
            NST = 6
            st6 = [sb(s4, "st6b%d" % i, [128, 2, 6], F32) for i in range(NST)]
            mv = [sb(s4, "mvb%d" % i, [128, 2], F32) for i in range(NST)]
            rstd = [sb(s4, "rstdb%d" % i, [128, 1], F32) for i in range(NST)]
            nb = [sb(s4, "nbb%d" % i, [128, 1], F32) for i in range(NST)]
            ve4 = [sb(s4, "veb%d" % i, [128, 1], F32) for i in range(NST)]
            rl = sb(s4, "rl", [128, 16], F32)
            ra = sb(s4, "ra", [128, 16], F32)
            op('dve', 'tensor_scalar', ['ssl'], ['rl'], rl[:], ssl[:], 1.0 / 512, EPS, ALU.mult, ALU.add)
            op('pool', 'tensor_tensor', ['rl', 'mhalf'], ['rl'], rl[:], rl[:], mhalf[:, 0:16], ALU.pow)
            op('dve', 'tensor_scalar', ['ssa'], ['ra'], ra[:], ssa[:], 1.0 / 512, EPS, ALU.mult, ALU.add)
            op('pool', 'tensor_tensor', ['ra', 'mhalf'], ['ra'], ra[:], ra[:], mhalf[:, 0:16], ALU.pow)

            def ln_stats(src, srck, i):
                for hf in range(2):
                    op('dve', 'bn_stats', [srck], ['st6_%d' % i], out=st6[i][:, hf, :], in_=src[:, hf * 512:(hf + 1) * 512])
                op('dve', 'bn_aggr', ['st6_%d' % i], ['mv_%d' % i], out=mv[i][:], in_=st6[i][:].rearrange("p a b -> p (a b)"))
                op('dve', 'tensor_scalar_add', ['mv_%d' % i], ['ve_%d' % i], ve4[i][:], mv[i][:, 1:2], EPS)
                op('pool', 'tensor_tensor', ['ve_%d' % i, 'mhalf'], ['rstd_%d' % i], rstd[i][:], ve4[i][:], mhalf[:, 0:1], ALU.pow)
                op('dve', 'scalar_tensor_tensor', ['mv_%d' % i, 'rstd_%d' % i], ['nb_%d' % i], out=nb[i][:], in0=mv[i][:, 0:1],
                   scalar=-1.0, in1=rstd[i][:], op0=ALU.mult, op1=ALU.mult)

            for tt in range(16):
                b = tt % NB4
                sa_, sb_ = (2 * tt) % NST, (2 * tt + 1) % NST
                xok, mixk, zzk, x1k, xn2k = 'xo%d' % b, 'mix%d' % b, 'zz%d' % b, 'x1_%d' % b, 'xn2_%d' % b
                P.dma('sp', xo[b][:], xctx[OWN0 + tt * 128: OWN0 + (tt + 1) * 128, :], writes=[xok])
                for hf in range(2):
                    pa, pak, pb, pbk = (pA, 'pA', pB, 'pB') if hf == 0 else (pS0, 'pS0', pS1, 'pS1')
                    for jj in range(4):
                        mm(pa[:, :], loT[:, jj, tt * 128:(tt + 1) * 128], woutb[:, jj, hf * 512:(hf + 1) * 512],
                           ['loT%d' % jj, 'woutb%d' % jj], [pak], jj == 0, jj == 3)
                    for jj in range(4):
                        mm(pb[:, :], aTp[:, jj, tt * 128:(tt + 1) * 128], woutb[:, 4 + jj, hf * 512:(hf + 1) * 512],
                           ['aTp%d' % jj, 'woutb%d' % (4 + jj)], [pbk], jj == 0, jj == 3)
                    op('act', 'activation', [pak, 'rl'], [mixk], out=mix[b][:, hf * 512:(hf + 1) * 512], in_=pa[:, :],
                       func=AF.Identity, scale=rl[:, tt:tt + 1])
                    op('dve', 'scalar_tensor_tensor', [pbk, 'ra', mixk], [mixk], out=mix[b][:, hf * 512:(hf + 1) * 512],
                       in0=pb[:, :], scalar=ra[:, tt:tt + 1], in1=mix[b][:, hf * 512:(hf + 1) * 512], op0=ALU.mult, op1=ALU.add)
                op('pool', 'tensor_tensor', [mixk, 'g1B'], [zzk], zz[b][:], mix[b][:], g1B[:], ALU.mult)
                op('dve', 'scalar_tensor_tensor', [xok, zzk], [zzk], out=zz[b][:], in0=xo[b][:], scalar=ALPHA, in1=zz[b][:],
                   op0=ALU.mult, op1=ALU.add)
                ln_stats(zz[b], zzk, sa_)
                op('act', 'activation', [zzk, 'rstd_%d' % sa_, 'nb_%d' % sa_], [x1k], out=x1[b][:], in_=zz[b][:], func=AF.Identity,
                   bias=nb[sa_][:], scale=rstd[sa_][:])
                op('dve', 'tensor_tensor', [x1k, 'lnB'], [x1k], x1[b][:], x1[b][:], lnB[:, 0, :], ALU.mult)
                op('dve', 'tensor_tensor', [x1k, 'lnB'], [x1k], x1[b][:], x1[b][:], lnB[:, 1, :], ALU.add)
                P.dma('sp', x1_s[tt * 128:(tt + 1) * 128, :], x1[b][:], reads=[x1k], writes=['x1_s'])
                ln_stats(x1[b], x1k, sb_)
                op('act', 'activation', [x1k, 'rstd_%d' % sb_, 'nb_%d' % sb_], [xn2k], out=xn2[b][:], in_=x1[b][:], func=AF.Identity,
                   bias=nb[sb_][:], scale=rstd[sb_][:])
                pt, ptk = (pT0, 'pT0') if tt % 2 == 0 else (pT1, 'pT1')
                for k in range(8):
                    pe_raw('transpose', [xn2k, 'ident'], [ptk], sig=(k == 7), argskw=ARGS(pt[:, k * 128:(k + 1) * 128], xn2[b][:, k * 128:(k + 1) * 128], ident[:]))
                for k in range(8):
                    if tt % 2 == 0:
                        op('act', 'activation', [ptk, 'sc2p', 'modT'], ['u2T%d' % k], out=u2T[:, k, tt * 128:(tt + 1) * 128],
                           in_=pt[:, k * 128:(k + 1) * 128], func=AF.Identity, bias=modT[:, 24 + k:25 + k],
                           scale=sc2p[:, k:k + 1])
                    else:
                        op('dve', 'tensor_scalar', [ptk, 'sc2p', 'modT'], ['u2T%d' % k], u2T[:, k, tt * 128:(tt + 1) * 128],
                           pt[:, k * 128:(k + 1) * 128], sc2p[:, k:k + 1], modT[:, 24 + k:25 + k], ALU.mult, ALU.add)
                router(tt)
            if DBG:
                P.dma('sp', dout("d_u2T", [128, 8 * NOWN], BF16), u2T[:].rearrange("p k n -> p (k n)"),
                      reads=['u2T%d' % k for k in range(8)])
            P.barrier()
            if STOP == 4:
                raise _Stop()

        P.flush()
        P.schedule = ('5' in SCHED_PH) and (os.environ.get('MK_SCHED', '1') == '1')
        with contextlib.ExitStack() as s5:
            if DBG:
                P.dma('sp', dout("d_gate", [128, 16 * NE]), gate[:].rearrange("p t e -> p (t e)"), reads=['gate'])
            NS = 3
            w1b = [sb(s5, "w1b%d" % i, [128, 8, DE], BF16) for i in range(NS)]
            w3b = [sb(s5, "w3b%d" % i, [128, 8, DE], BF16) for i in range(NS)]
            w2b = [sb(s5, "w2b%d" % i, [128, 2, D], BF16) for i in range(NS)]
            yacc = sb(s5, "yacc", [128, 16, D], F32)
            ssi = [sb(s5, "ssi%d" % i, [128, 512], F32) for i in range(2)]
            hdn = [sb(s5, "hdn%d" % i, [128, 512], BF16) for i in range(4)]
            nh_ = 0
            ny = 0
            for e in range(NE):
                sl = e % NS
                P.dma('pool', w1b[sl][:], w1[e].rearrange("(k p) f -> p k f", p=128), writes=['w1b%d' % sl])
                P.dma('pool', w3b[sl][:], w3[e].rearrange("(k p) f -> p k f", p=128), writes=['w3b%d' % sl])
                P.dma('pool', w2b[sl][:], w2[e].rearrange("(c p) d -> p c d", p=128), writes=['w2b%d' % sl])
                for tc in range(4):
                    hk = []
                    for fc in range(2):
                        p1, p1k, p3, p3k = (pA, 'pA', pB, 'pB') if fc == 0 else (pS0, 'pS0', pS1, 'pS1')
                        for k in range(8):
                            mm(p1[:, :], w1b[sl][:, k, fc * 128:(fc + 1) * 128], u2T[:, k, tc * 512:(tc + 1) * 512],
                               ['w1b%d' % sl, u2r[k]], [p1k], k == 0, k == 7)
                        for k in range(8):
                            mm(p3[:, :], w3b[sl][:, k, fc * 128:(fc + 1) * 128], u2T[:, k, tc * 512:(tc + 1) * 512],
                               ['w3b%d' % sl, u2r[k]], [p3k], k == 0, k == 7)
                        si, sik = ssi[fc], 'ssi%d' % fc
                        hd, hdk = hdn[nh_ % 4], 'hdn%d' % (nh_ % 4)
                        nh_ += 1
                        op('act', 'activation', [p1k], [sik], out=si[:], in_=p1[:, :], func=AF.Silu)
                        op('dve', 'tensor_tensor', [sik, p3k], [hdk], hd[:], si[:], p3[:, :], ALU.mult)
                        hk.append((hd, hdk))
                    for t in range(4):
                        tt = tc * 4 + t
                        for hf in range(2):
                            py, pyk = (pO, 'pO') if ny % 2 == 0 else (pM, 'pM')
                            ny += 1
                            for fc in range(2):
                                mm(py[:, :], hk[fc][0][:, t * 128:(t + 1) * 128], w2b[sl][:, fc, hf * 512:(hf + 1) * 512],
                                   [hk[fc][1], 'w2b%d' % sl], [pyk], fc == 0, fc == 1)
                            if e == 0:
                                op('dve', 'tensor_scalar', [pyk, 'gate'], ['yacc%d' % tt], yacc[:, tt, hf * 512:(hf + 1) * 512],
                                   py[:, :], gate[:, tt, e:e + 1], None, ALU.mult)
                            else:
                                op('dve', 'scalar_tensor_tensor', [pyk, 'gate', 'yacc%d' % tt], ['yacc%d' % tt],
                                   out=yacc[:, tt, hf * 512:(hf + 1) * 512], in0=py[:, :], scalar=gate[:, tt, e:e + 1],
                                   in1=yacc[:, tt, hf * 512:(hf + 1) * 512], op0=ALU.mult, op1=ALU.add)
            NB5 = 3
            x1l = [sb(s5, "x1l%d" % i, [128, D], F32) for i in range(NB5)]
            zf = [sb(s5, "zf%d" % i, [128, D], F32) for i in range(NB5)]
            of = [sb(s5, "of%d" % i, [128, D], F32) for i in range(NB5)]
            st6 = [sb(s5, "st6c%d" % i, [128, 2, 6], F32) for i in range(NB5)]
            mv = [sb(s5, "mvc%d" % i, [128, 2], F32) for i in range(NB5)]
            rstd = [sb(s5, "rstdc%d" % i, [128, 1], F32) for i in range(NB5)]
            nb = [sb(s5, "nbc%d" % i, [128, 1], F32) for i in range(NB5)]
            ve5 = [sb(s5, "vec%d" % i, [128, 1], F32) for i in range(NB5)]
            for tt in range(16):
                b = tt % NB5
                x1lk, zfk, ofk = 'x1l%d' % b, 'zf%d' % b, 'of%d' % b
                P.dma('sp', x1l[b][:], x1_s[tt * 128:(tt + 1) * 128, :], reads=['x1_s'], writes=[x1lk])
                op('pool', 'tensor_tensor', ['yacc%d' % tt, 'g2B'], [zfk], zf[b][:], yacc[:, tt, :], g2B[:], ALU.mult)
                op('dve', 'scalar_tensor_tensor', [x1lk, zfk], [zfk], out=zf[b][:], in0=x1l[b][:], scalar=ALPHA, in1=zf[b][:],
                   op0=ALU.mult, op1=ALU.add)
                for hf in range(2):
                    op('dve', 'bn_stats', [zfk], ['st6f%d' % b], out=st6[b][:, hf, :], in_=zf[b][:, hf * 512:(hf + 1) * 512])
                op('dve', 'bn_aggr', ['st6f%d' % b], ['mvf%d' % b], out=mv[b][:], in_=st6[b][:].rearrange("p a b -> p (a b)"))
                op('dve', 'tensor_scalar_add', ['mvf%d' % b], ['vef%d' % b], ve5[b][:], mv[b][:, 1:2], EPS)
                op('pool', 'tensor_tensor', ['vef%d' % b, 'mhalf'], ['rstdf%d' % b], rstd[b][:], ve5[b][:], mhalf[:, 0:1], ALU.pow)
                op('dve', 'scalar_tensor_tensor', ['mvf%d' % b, 'rstdf%d' % b], ['nbf%d' % b], out=nb[b][:], in0=mv[b][:, 0:1],
                   scalar=-1.0, in1=rstd[b][:], op0=ALU.mult, op1=ALU.mult)
                op('act', 'activation', [zfk, 'rstdf%d' % b, 'nbf%d' % b], [ofk], out=of[b][:], in_=zf[b][:], func=AF.Identity,
                   bias=nb[b][:], scale=rstd[b][:])
                op('dve', 'tensor_tensor', [ofk, 'lnB'], [ofk], of[b][:], of[b][:], lnB[:, 2, :], ALU.mult)
                op('dve', 'tensor_tensor', [ofk, 'lnB'], [ofk], of[b][:], of[b][:], lnB[:, 3, :], ALU.add)
                P.dma('sp', out_d[tt * 128:(tt + 1) * 128, :], of[b][:], reads=[ofk], writes=['out'])
            P.barrier()
    except _Stop:
        P.barrier()
    P.close()
    return nc, list(dbg.keys())


def _host_inputs(inp):
    f = lambda a: np.ascontiguousarray(np.asarray(a, dtype=np.float32))
    x = f(inp['x']); c = f(inp['c'])
    per_part = lambda v: np.ascontiguousarray(v.reshape(-1, 128).T)
    shared = {
        'w_ada': f(inp['w_ada'][0]),
        'b_ada': per_part(f(inp['b_ada'][0])),
        'b_ada_row': np.ascontiguousarray(f(inp['b_ada'][0])[None, :]),
        'w_in': f(inp['w_in'][0]),
        'convw': np.ascontiguousarray(f(inp['conv_w'][0]).T.reshape(4, 128, 4).transpose(1, 0, 2).reshape(128, 16)),
        'convb': per_part(f(inp['conv_b'][0])),
        'bga': per_part(f(inp['b_gate_a'][0]).reshape(-1)),
        'bgx': per_part(f(inp['b_gate_x'][0]).reshape(-1)),
        'lam': per_part(f(inp['lru_lambda'][0])),
        'relb': f(inp['rel_bias']),
        'glru': per_part(f(inp['norm_lru_g'][0])),
        'gattn': per_part(f(inp['norm_attn_g'][0])),
        'w_out': f(inp['w_out'][0]),
        'lnv': np.ascontiguousarray(np.stack([f(inp['ln1_g'][0]), f(inp['ln1_b'][0]), f(inp['ln2_g'][0]), f(inp['ln2_b'][0])])),
        'w_r': np.ascontiguousarray(np.concatenate([f(inp['w_router_group'][0]), f(inp['w_router_expert'][0])], axis=1)),
        'b_r': np.ascontiguousarray(np.concatenate([f(inp['b_router_group'][0]), f(inp['b_router_expert'][0])])[None, :]),
        'w1': f(inp['w1'][0]), 'w3': f(inp['w3'][0]), 'w2': f(inp['w2'][0]),
    }
    for nm, src in (('WA', 'w_gate_a'), ('WX', 'w_gate_x')):
        w = f(inp[src][0])
        bd = np.zeros((4, 128, 128), np.float32)
        for j in range(4):
            for s in range(2):
                bd[j, s * 64:(s + 1) * 64, s * 64:(s + 1) * 64] = w[2 * j + s]
        shared[nm] = bd
    i = np.arange(384)
    r = 255 - i
    bk = _t5_bucket_np(r)
    Roh = np.zeros((32, 384), np.float32)
    valid = (r >= 0) & (i < 383)
    Roh[bk[valid], i[valid]] = 1.0
    NEGr = np.tile(np.where(r < 0, NEGM, 0.0).astype(np.float32)[None, :], (8, 1))
    IND = np.zeros((16, CT), np.float32)
    for n in range(16):
        IND[n, n * BLK:(n + 1) * BLK] = 1.0
    shared['Roh'] = Roh; shared['NEGr'] = np.ascontiguousarray(NEGr); shared['IND'] = IND
    maps = []
    for core in range(8):
        b, half = core // 2, core % 2
        m = dict(shared)
        if half == 1:
            m['xctx'] = np.ascontiguousarray(x[b])
        else:
            m['xctx'] = np.ascontiguousarray(np.concatenate([np.zeros((2048, D), np.float32), x[b, :2048]], axis=0))
        m['cvec'] = per_part(c[b])
        gm = np.zeros((16, 16), np.float32); oh = np.zeros((16, 16), np.float32)
        for qt in range(16):
            own = 8 + qt // 2
            for n in range(16):
                ok = (n < own) and (half == 1 or n >= 8)
                gm[qt, n] = 0.0 if ok else -1e30
            oh[qt, own] = 1.0
        m['gmask'] = np.ascontiguousarray(np.tile(gm.reshape(1, 256), (128, 1)))
        m['ownhot'] = np.ascontiguousarray(np.tile(oh.reshape(1, 256), (128, 1)))
        m['flag'] = np.full((128, 1), float(half), np.float32)
        maps.append(m)
    return maps


_NC_CACHE = {}


def kernel(**inputs):
    if 'nc' not in _NC_CACHE:
        _NC_CACHE['nc'] = build_program()
    nc, dbgnames = _NC_CACHE['nc']
    maps = _host_inputs(inputs)
    if STOP < 5:
        for m in maps:
            for k in ('w1', 'w3', 'w2'):
                m.pop(k)
    res = run_bass_kernel_spmd(nc, maps, core_ids=list(range(8)))
    out = np.zeros((4, SEQ, D), np.float32)
    for core in range(8):
        b, half = core // 2, core % 2
        out[b, half * 2048:(half + 1) * 2048] = res.results[core]['out']
    if DBG:
        kernel.dbg = [{k: res.results[core][k] for k in dbgnames} for core in range(8)]
    return out
```
